# Optimizing a Trainium2 kernel written in Bass

```python
import math
import jax, jax.numpy as jnp
from jax import lax
import numpy as np

D_MODEL = 2048
BATCH = 4
SEQ = 4096
DEPTH = 2

MIX_WIDTH = D_MODEL
S5_WIDTH = MIX_WIDTH // 4
S5_GROUP = 16
S5_GROUPS = S5_WIDTH // S5_GROUP
S5_STATE = 64
DT_MIN = 1e-3
DT_MAX = 1e-1
SB_HEAD_DIM = 128
SB_HEADS = (MIX_WIDTH - S5_WIDTH) // SB_HEAD_DIM
SB_WIDTH = SB_HEADS * SB_HEAD_DIM
Q_BLOCK = 128
AB_IN_WIDTH = S5_WIDTH + 3 * SB_WIDTH
POOL_WINDOWS = (2, 4, 8, 16)
POOL_GROUPS = len(POOL_WINDOWS)
POOL_GROUP_WIDTH = MIX_WIDTH // POOL_GROUPS
N_EXPERTS = 16
N_EXPERT_GROUPS = 4
EXPERTS_PER_GROUP = N_EXPERTS // N_EXPERT_GROUPS
TOP_K = 2
D_FF_EXPERT = D_MODEL // 2
ALPHA = (2.0 * DEPTH) ** 0.25
BETA = (8.0 * DEPTH) ** -0.25
LN_EPS = 1e-5
N_EVEN = (DEPTH + 1) // 2
N_ODD = DEPTH // 2

kernel_name = "hybrid_s5_stickbreak_pool_grouped_moe_deepnorm"


def layer_norm(x, g, b):
    xf = x.astype(jnp.float32)
    mu = jnp.mean(xf, axis=-1, keepdims=True)
    var = jnp.mean(jnp.square(xf - mu), axis=-1, keepdims=True)
    y = (xf - mu) * lax.rsqrt(var + LN_EPS) * g.astype(jnp.float32) + b.astype(jnp.float32)
    return y.astype(x.dtype)


def s5_mixer(u, lam_re, lam_im, log_dt, b_re, b_im, c_re, c_im, d, w_glu, b_glu):
    Bsz, S, _ = u.shape
    uf = u.astype(jnp.float32)
    ug = uf.reshape(Bsz, S, S5_GROUPS, S5_GROUP)
    lr = lam_re.astype(jnp.float32)
    li = lam_im.astype(jnp.float32)
    dt = jnp.exp(log_dt.astype(jnp.float32))[:, None]
    mag = jnp.exp(lr * dt)
    lbr = mag * jnp.cos(li * dt)
    lbi = mag * jnp.sin(li * dt)
    den = lr * lr + li * li
    coef_re = ((lbr - 1.0) * lr + lbi * li) / den
    coef_im = (lbi * lr - (lbr - 1.0) * li) / den
    br = b_re.astype(jnp.float32)
    bi = b_im.astype(jnp.float32)
    bbar_re = coef_re[..., None] * br - coef_im[..., None] * bi
    bbar_im = coef_re[..., None] * bi + coef_im[..., None] * br
    bu_re = jnp.einsum('bsgh,gnh->bsgn', ug, bbar_re)
    bu_im = jnp.einsum('bsgh,gnh->bsgn', ug, bbar_im)
    a_re = jnp.broadcast_to(lbr, bu_re.shape)
    a_im = jnp.broadcast_to(lbi, bu_im.shape)

    def combine(e1, e2):
        a1r, a1i, b1r, b1i = e1
        a2r, a2i, b2r, b2i = e2
        ar = a2r * a1r - a2i * a1i
        ai = a2r * a1i + a2i * a1r
        b_r = a2r * b1r - a2i * b1i + b2r
        b_i = a2r * b1i + a2i * b1r + b2i
        return (ar, ai, b_r, b_i)

    _, _, s_re, s_im = lax.associative_scan(combine, (a_re, a_im, bu_re, bu_im), axis=1)
    y = (jnp.einsum('bsgn,ghn->bsgh', s_re, c_re.astype(jnp.float32))
         - jnp.einsum('bsgn,ghn->bsgh', s_im, c_im.astype(jnp.float32)))
    y = y.reshape(Bsz, S, S5_WIDTH) + d.astype(jnp.float32) * uf
    g = jax.nn.gelu(y)
    out = g * jax.nn.sigmoid(g @ w_glu.astype(jnp.float32) + b_glu.astype(jnp.float32))
    return out.astype(u.dtype)


def stick_breaking_attention(q, k, v):
    Bsz, S, H, dh = q.shape
    nb = S // Q_BLOCK
    scale = 1.0 / math.sqrt(dh)
    qh = q.astype(jnp.float32).transpose(0, 2, 1, 3)
    kh = k.astype(jnp.float32).transpose(0, 2, 1, 3)
    vh = v.astype(jnp.float32).transpose(0, 2, 1, 3)
    qb = qh.reshape(Bsz, H, nb, Q_BLOCK, dh).transpose(2, 0, 1, 3, 4)
    k_pos = jnp.arange(S)

    def block(args):
        q_blk, blk = args
        q_pos = blk * Q_BLOCK + jnp.arange(Q_BLOCK)
        causal = k_pos[None, :] < q_pos[:, None]
        z = jnp.einsum('bhqd,bhkd->bhqk', q_blk, kh) * scale
        log_beta = jax.nn.log_sigmoid(z)
        log_keep = jnp.where(causal, jax.nn.log_sigmoid(-z), 0.0)
        tail = jnp.flip(jnp.cumsum(jnp.flip(log_keep, -1), axis=-1), -1) - log_keep
        w = jnp.where(causal, jnp.exp(log_beta + tail), 0.0)
        return jnp.einsum('bhqk,bhkd->bhqd', w, vh)

    out = lax.map(block, (qb, jnp.arange(nb)))
    out = out.transpose(1, 0, 3, 2, 4).reshape(Bsz, S, H * dh)
    return out.astype(q.dtype)


def mixer_ab(x, w_in, lam_re, lam_im, log_dt, b_re, b_im, c_re, c_im, d, w_glu, b_glu, w_out):
    Bsz, S, _ = x.shape
    h = x @ w_in
    u = h[..., :S5_WIDTH]
    q = h[..., S5_WIDTH:S5_WIDTH + SB_WIDTH].reshape(Bsz, S, SB_HEADS, SB_HEAD_DIM)
    k = h[..., S5_WIDTH + SB_WIDTH:S5_WIDTH + 2 * SB_WIDTH].reshape(Bsz, S, SB_HEADS, SB_HEAD_DIM)
    v = h[..., S5_WIDTH + 2 * SB_WIDTH:].reshape(Bsz, S, SB_HEADS, SB_HEAD_DIM)
    y_a = s5_mixer(u, lam_re, lam_im, log_dt, b_re, b_im, c_re, c_im, d, w_glu, b_glu)
    y_b = stick_breaking_attention(q, k, v)
    return jnp.concatenate([y_a, y_b], axis=-1) @ w_out


def mixer_pool(x, w_in, w_group, scale, w_out):
    Bsz, S, _ = x.shape
    h = (x @ w_in).astype(jnp.float32).reshape(Bsz, S, POOL_GROUPS, POOL_GROUP_WIDTH)
    cs = jnp.cumsum(h, axis=1)
    cs0 = jnp.concatenate([jnp.zeros_like(cs[:, :1]), cs], axis=1)
    windows = jnp.array(POOL_WINDOWS, dtype=jnp.int32)
    t = jnp.arange(S, dtype=jnp.int32)[:, None]
    lo = jnp.maximum(t + 1 - windows[None, :], 0)
    g_idx = jnp.arange(POOL_GROUPS)[None, :]
    window_sum = cs0[:, 1:] - cs0[:, lo, g_idx, :]
    count = jnp.minimum(t + 1, windows[None, :]).astype(jnp.float32)
    pooled = window_sum / count[None, :, :, None] - h
    y = jnp.einsum('bsgc,gce->bsge', pooled, w_group.astype(jnp.float32))
    y = y.reshape(Bsz, S, MIX_WIDTH) * scale.astype(jnp.float32)
    return y.astype(x.dtype) @ w_out


def grouped_moe(x, router_w, router_b, w_gate, w_up, w_down):
    Bsz, S, D = x.shape
    tok = x.reshape(-1, D)
    logits = (tok @ router_w + router_b).astype(jnp.float32)
    probs = jax.nn.softmax(logits, axis=-1)
    pg = probs.reshape(-1, N_EXPERT_GROUPS, EXPERTS_PER_GROUP)
    group_score = lax.top_k(pg, TOP_K)[0].sum(-1)
    g_sel = jnp.argmax(group_score, axis=-1)
    in_group = (jnp.arange(N_EXPERTS) // EXPERTS_PER_GROUP)[None, :] == g_sel[:, None]
    masked = jnp.where(in_group, probs, -1.0)
    top_w, top_i = lax.top_k(masked, TOP_K)
    top_w = top_w / jnp.sum(top_w, axis=-1, keepdims=True)
    gates = jnp.einsum('tk,tke->te', top_w, jax.nn.one_hot(top_i, N_EXPERTS, dtype=jnp.float32))
    gates = gates.astype(tok.dtype)
    out = jnp.zeros_like(tok)
    for e in range(N_EXPERTS):
        hid = jax.nn.silu(tok @ w_gate[e]) * (tok @ w_up[e])
        out = out + gates[:, e:e + 1] * (hid @ w_down[e])
    return out.reshape(Bsz, S, D)


def setup_inputs(seed: int = 0) -> dict:
    key = jax.random.key(seed)
    ks = jax.random.split(key, 26)
    f32 = jnp.float32
    nrm = lambda k, shape, s: jax.random.normal(k, shape, f32) * s
    x = jax.random.normal(ks[0], (BATCH, SEQ, D_MODEL), f32)
    ab_w_in = nrm(ks[1], (N_EVEN, D_MODEL, AB_IN_WIDTH), D_MODEL ** -0.5)
    n_idx = jnp.arange(S5_STATE, dtype=f32)
    ab_lambda_re = -0.5 + nrm(ks[2], (N_EVEN, S5_GROUPS, S5_STATE), 0.01)
    ab_lambda_im = math.pi * n_idx + nrm(ks[3], (N_EVEN, S5_GROUPS, S5_STATE), 0.01)
    ab_log_dt = jax.random.uniform(ks[4], (N_EVEN, S5_GROUPS), f32, math.log(DT_MIN), math.log(DT_MAX))
    ab_b_re = nrm(ks[5], (N_EVEN, S5_GROUPS, S5_STATE, S5_GROUP), (2 * S5_GROUP) ** -0.5)
    ab_b_im = nrm(ks[6], (N_EVEN, S5_GROUPS, S5_STATE, S5_GROUP), (2 * S5_GROUP) ** -0.5)
    ab_c_re = nrm(ks[7], (N_EVEN, S5_GROUPS, S5_GROUP, S5_STATE), (2 * S5_STATE) ** -0.5)
    ab_c_im = nrm(ks[8], (N_EVEN, S5_GROUPS, S5_GROUP, S5_STATE), (2 * S5_STATE) ** -0.5)
    ab_d = 1.0 + nrm(ks[9], (N_EVEN, S5_WIDTH), 0.1)
    ab_w_glu = nrm(ks[10], (N_EVEN, S5_WIDTH, S5_WIDTH), S5_WIDTH ** -0.5)
    ab_b_glu = nrm(ks[11], (N_EVEN, S5_WIDTH), 0.01)
    ab_w_out = nrm(ks[12], (N_EVEN, MIX_WIDTH, D_MODEL), BETA * MIX_WIDTH ** -0.5)
    c_w_in = nrm(ks[13], (N_ODD, D_MODEL, MIX_WIDTH), D_MODEL ** -0.5)
    c_w_group = nrm(ks[14], (N_ODD, POOL_GROUPS, POOL_GROUP_WIDTH, POOL_GROUP_WIDTH), POOL_GROUP_WIDTH ** -0.5)
    c_scale = 1.0 + nrm(ks[15], (N_ODD, MIX_WIDTH), 0.02)
    c_w_out = nrm(ks[16], (N_ODD, MIX_WIDTH, D_MODEL), BETA * MIX_WIDTH ** -0.5)
    ln_g = 1.0 + nrm(ks[17], (DEPTH, 2, D_MODEL), 0.02)
    ln_b = nrm(ks[18], (DEPTH, 2, D_MODEL), 0.02)
    router_w = nrm(ks[19], (D_MODEL, N_EXPERTS), D_MODEL ** -0.5)
    router_b = nrm(ks[20], (N_EXPERTS,), 0.01)
    moe_w_gate = nrm(ks[21], (DEPTH, N_EXPERTS, D_MODEL, D_FF_EXPERT), D_MODEL ** -0.5)
    moe_w_up = nrm(ks[22], (DEPTH, N_EXPERTS, D_MODEL, D_FF_EXPERT), D_MODEL ** -0.5)
    moe_w_down = nrm(ks[23], (DEPTH, N_EXPERTS, D_FF_EXPERT, D_MODEL), BETA * D_FF_EXPERT ** -0.5)
    return {"x": x, "ab_w_in": ab_w_in, "ab_lambda_re": ab_lambda_re, "ab_lambda_im": ab_lambda_im,
            "ab_log_dt": ab_log_dt, "ab_b_re": ab_b_re, "ab_b_im": ab_b_im, "ab_c_re": ab_c_re,
            "ab_c_im": ab_c_im, "ab_d": ab_d, "ab_w_glu": ab_w_glu, "ab_b_glu": ab_b_glu,
            "ab_w_out": ab_w_out, "c_w_in": c_w_in, "c_w_group": c_w_group, "c_scale": c_scale,
            "c_w_out": c_w_out, "ln_g": ln_g, "ln_b": ln_b, "router_w": router_w, "router_b": router_b,
            "moe_w_gate": moe_w_gate, "moe_w_up": moe_w_up, "moe_w_down": moe_w_down}


def reference(x, ab_w_in, ab_lambda_re, ab_lambda_im, ab_log_dt, ab_b_re, ab_b_im, ab_c_re, ab_c_im,
              ab_d, ab_w_glu, ab_b_glu, ab_w_out, c_w_in, c_w_group, c_scale, c_w_out, ln_g, ln_b,
              router_w, router_b, moe_w_gate, moe_w_up, moe_w_down):
    h = x
    for i in range(DEPTH):
        j = i // 2
        if i % 2 == 0:
            mix = mixer_ab(h, ab_w_in[j], ab_lambda_re[j], ab_lambda_im[j], ab_log_dt[j], ab_b_re[j],
                           ab_b_im[j], ab_c_re[j], ab_c_im[j], ab_d[j], ab_w_glu[j], ab_b_glu[j], ab_w_out[j])
        else:
            mix = mixer_pool(h, c_w_in[j], c_w_group[j], c_scale[j], c_w_out[j])
        h = layer_norm(ALPHA * h + mix, ln_g[i, 0], ln_b[i, 0])
        ffn = grouped_moe(h, router_w, router_b, moe_w_gate[i], moe_w_up[i], moe_w_down[i])
        h = layer_norm(ALPHA * h + ffn, ln_g[i, 1], ln_b[i, 1])
    return h
```

```python
from contextlib import ExitStack
import numpy as np
import concourse.bass as bass
import concourse.mybir as mybir
from concourse.bass_utils import run_bass_kernel_spmd

F32 = mybir.dt.float32
BF16 = mybir.dt.bfloat16
I32 = mybir.dt.int32
AF = mybir.ActivationFunctionType
ALU = mybir.AluOpType
AX = mybir.AxisListType

NDMA = 24
NOSELF = ("pe",)
MAXFLY = 6
ALPHA = 4.0 ** 0.25
LN_EPS = 1e-5
HIST = 1920
NEXT = 2176
LC = 256
TWO_PI = float(2 * np.pi)


class Buf:
    __slots__ = ("name", "lw", "rd")

    def __init__(self, name):
        self.name = name
        self.lw = None
        self.rd = []


class Op:
    __slots__ = ("eng", "fn", "reads", "writes", "dma", "deps", "has_dep", "ms", "sem_idx", "sem_val")

    def __init__(self, eng, fn, reads, writes, dma):
        self.eng = eng
        self.fn = fn
        self.reads = reads
        self.writes = writes
        self.dma = dma
        self.deps = ()
        self.has_dep = False
        self.ms = 0
        self.sem_idx = -1
        self.sem_val = 0


class Ctx:
    ENG = ("pe", "act", "dve", "pool", "sp")

    def __init__(self, nc):
        self.nc = nc
        self.e = {"pe": nc.tensor, "act": nc.scalar, "dve": nc.vector, "pool": nc.gpsimd, "sp": nc.sync}
        self.ops = []
        self.bufs = []
        self.dma_sems = [nc.alloc_semaphore(f"dmas{i}") for i in range(NDMA)]
        self.dma_counts = [0] * NDMA
        self.dma_rr = 0
        self.bar = nc.alloc_semaphore("bar")
        self.nbar = 0
        self.waited = {e: {} for e in self.ENG}
        self.n_inst = 0
        self.inflight = {}

    def buf(self, name):
        b = Buf(name)
        self.bufs.append(b)
        return b

    def op(self, eng, fn, reads=(), writes=()):
        self.ops.append(Op(eng, fn, tuple(reads), tuple(writes), False))

    def dma(self, eng, fn, reads=(), writes=()):
        self.ops.append(Op(eng, fn, tuple(reads), tuple(writes), True))

    def _wait(self, E, key, sem, val):
        w = self.waited[E]
        if w.get(key, 0) < val:
            self.e[E].wait_ge(sem, val)
            w[key] = val
            self.n_inst += 1

    def flush(self):
        nc = self.nc
        ops = self.ops
        if not ops:
            return
        for b in self.bufs:
            b.lw = None
            b.rd = []
        last_on = {}
        for i, op in enumerate(ops):
            deps = set()
            for b in op.reads:
                if b.lw is not None:
                    deps.add(b.lw)
            for b in op.writes:
                if b.lw is not None:
                    deps.add(b.lw)
                deps.update(b.rd)
            deps.discard(i)
            if op.eng in NOSELF and not op.dma:
                deps = {d for d in deps if ops[d].dma or ops[d].eng != op.eng}
            op.deps = sorted(deps)
            for d in op.deps:
                ops[d].has_dep = True
            for b in op.reads:
                if not op.dma:
                    b.rd = [r for r in b.rd if ops[r].dma or ops[r].eng != op.eng]
                b.rd.append(i)
            for b in op.writes:
                b.lw = i
                b.rd = []
            if not op.dma:
                last_on[op.eng] = i
        for e, i in last_on.items():
            ops[i].has_dep = True
        sem = {e: nc.alloc_semaphore(f"ph{self.nbar}_{e}") for e in self.ENG if e != "sp"}
        cnt = {e: 0 for e in self.ENG}
        dma_used = set()
        for op in ops:
            E = op.eng
            for d in op.deps:
                D = ops[d]
                if D.dma:
                    self._wait(E, ("d", D.sem_idx), self.dma_sems[D.sem_idx], D.sem_val)
                else:
                    self._wait(E, ("e", D.eng), sem[D.eng], D.ms)
            if op.dma:
                fl = self.inflight.setdefault(E, [])
                if len(fl) >= MAXFLY:
                    pk, pv = fl[len(fl) - MAXFLY]
                    self._wait(E, ("d", pk), self.dma_sems[pk], pv)
                k = self.dma_rr
                self.dma_rr = (self.dma_rr + 1) % NDMA
                if self.dma_counts[k] > 0:
                    self._wait(E, ("d", k), self.dma_sems[k], self.dma_counts[k])
                self.dma_counts[k] += 16
                op.sem_idx = k
                op.sem_val = self.dma_counts[k]
                dma_used.add(k)
                fl.append((k, op.sem_val))
                ins = op.fn(self.e[E])
                ins.then_inc(self.dma_sems[k], 16)
            else:
                ins = op.fn(self.e[E])
                if op.has_dep:
                    cnt[E] += 1
                    op.ms = cnt[E]
                    ins.then_inc(sem[E], 1)
            self.n_inst += 1
        for k in sorted(dma_used):
            self._wait("sp", ("d", k), self.dma_sems[k], self.dma_counts[k])
        for e in self.ENG:
            if e != "sp" and cnt[e] > 0:
                self._wait("sp", ("e", e), sem[e], cnt[e])
        self.nbar += 1
        self.e["sp"].sem_inc(self.bar, 1)
        for e in self.ENG:
            if e != "sp":
                self.e[e].wait_ge(self.bar, self.nbar)
        self.ops = []
        self.waited = {e: {k: v for k, v in self.waited[e].items() if k[0] == "d"} for e in self.ENG}


class TB:
    __slots__ = ("t", "b")

    def __init__(self, t, b):
        self.t = t
        self.b = b


class K:
    def __init__(self, nc, debug):
        self.nc = nc
        self.c = Ctx(nc)
        self.debug = debug
        self.ev = 0
        self.uid = 0
        self.act_copy_ok = False

    def tile(self, st, name, shape, dt):
        self.uid += 1
        t = st.enter_context(self.nc.sbuf_tensor(f"{name}_{self.uid}", shape, dt))
        return TB(t, self.c.buf(name))

    def gtile(self, name, shape, dt):
        t = self.nc.alloc_sbuf_tensor(name, shape, dt)
        return TB(t, self.c.buf(name))

    def evac(self, out, in_, reads, writes, eng=None):
        if eng is None:
            eng = "act" if (self.ev % 2 == 0 and self.act_copy_ok) else "dve"
            self.ev += 1
        if eng == "act":
            self.c.op("act", lambda e: e.activation(out=out, in_=in_, func=AF.Copy), reads=reads, writes=writes)
        else:
            self.c.op(eng, lambda e: e.tensor_copy(out=out, in_=in_), reads=reads, writes=writes)


def tok_tiles(n, w=512):
    out = []
    t = 0
    while t < n:
        m = min(w, n - t)
        out.append((t, m))
        t += m
    return out


def setup_consts(k):
    nc, c = k.nc, k.c
    k.ident = k.gtile("ident", [128, 128], F32)
    k.identb = k.gtile("identb", [128, 128], BF16)
    k.triu = k.gtile("triu", [128, 128], BF16)
    k.strl = k.gtile("strl", [128, 128], BF16)
    k.cmask = k.gtile("cmask", [128, 4, 512], BF16)
    k.kb_sb = k.gtile("kb_sb", [128, 32], F32)
    k.eps = k.gtile("eps", [128, 1], F32)
    k.gates = [k.gtile(f"gates{i}", [128, 17, 16], F32) for i in range(2)]
    k.ps = [TB(nc.alloc_psum_tensor(f"ps{i}", [128, 512], F32), c.buf(f"ps{i}")) for i in range(8)]
    with ExitStack() as st:
        tmp = k.tile(st, "ctmp", [128, 512], F32)
        c.op("pool", lambda e: e.memset(k.ident.t[:], 0.0), writes=[k.ident.b])
        c.op("pool", lambda e: e.memset(k.eps.t[:], LN_EPS), writes=[k.eps.b])
        c.op("pool", lambda e: e.affine_select(out=k.ident.t[:], in_=k.ident.t[:], pattern=[[-1, 128]], compare_op=ALU.not_equal, fill=1.0, base=0, channel_multiplier=1), reads=[k.ident.b], writes=[k.ident.b])
        c.op("dve", lambda e: e.tensor_copy(out=k.identb.t[:], in_=k.ident.t[:]), reads=[k.ident.b], writes=[k.identb.b])
        c.op("pool", lambda e: e.memset(tmp.t[:, 0:128], 1.0), writes=[tmp.b])
        c.op("pool", lambda e: e.affine_select(out=tmp.t[:, 0:128], in_=tmp.t[:, 0:128], pattern=[[-1, 128]], compare_op=ALU.is_ge, fill=0.0, base=0, channel_multiplier=1), reads=[tmp.b], writes=[tmp.b])
        c.op("dve", lambda e: e.tensor_copy(out=k.triu.t[:], in_=tmp.t[:, 0:128]), reads=[tmp.b], writes=[k.triu.b])
        c.op("dve", lambda e: e.tensor_scalar(out=k.strl.t[:], in0=tmp.t[:, 0:128], scalar1=-1.0, scalar2=1.0, op0=ALU.mult, op1=ALU.add), reads=[tmp.b], writes=[k.strl.b])
        for m in range(4):
            c.op("pool", lambda e: e.memset(tmp.t[:], 1.0), reads=[tmp.b], writes=[tmp.b])
            c.op("pool", lambda e, m=m: e.affine_select(out=tmp.t[:], in_=tmp.t[:], pattern=[[1, 512]], compare_op=ALU.is_gt, fill=0.0, base=-128 * m, channel_multiplier=-1), reads=[tmp.b], writes=[tmp.b])
            c.op("dve", lambda e, m=m: e.tensor_copy(out=k.cmask.t[:, m, :], in_=tmp.t[:]), reads=[tmp.b], writes=[k.cmask.b])
        c.dma("sp", lambda e: e.dma_start(out=k.kb_sb.t[:], in_=k.kbias.rearrange("kb p -> p kb"), allow_slow_non_contiguous=True), writes=[k.kb_sb.b])
        c.flush()


def phase_inproj0(k):
    nc, c = k.nc, k.c
    with ExitStack() as st:
        xT = k.tile(st, "xT", [128, 16, NEXT], BF16)
        xs = [k.tile(st, f"xs{i}", [128, 2048], F32) for i in range(2)]
        Wb = [k.tile(st, f"Wb{i}", [128, 16, 512], BF16) for i in range(2)]
        stg = [k.tile(st, f"stg{i}", [128, NEXT], BF16) for i in range(2)]
        vst = [k.tile(st, f"vst{i}", [128, 512], BF16) for i in range(3)]
        wi = si = vi = pi = 0
        for (tok0, ntok, blocks) in ((0, HIST, [0, 4, 5, 6, 7, 8, 9]), (HIST, NEXT, list(range(10)))):
            for tt in range(ntok // 128):
                x_ = xs[tt % 2]
                r0 = tok0 + tt * 128
                c.dma("sp", lambda e, x_=x_, r0=r0: e.dma_start(out=x_.t[:], in_=k.x_loc[r0:r0 + 128, :]), writes=[x_.b])
                for g in range(4):
                    pb = k.ps[g]
                    for j in range(4):
                        kc = 4 * g + j
                        c.op("pe", lambda e, pb=pb, j=j, kc=kc, x_=x_: e.transpose(pb.t[:, j * 128:(j + 1) * 128], x_.t[:, kc * 128:(kc + 1) * 128], k.ident.t[:]), reads=[x_.b, k.ident.b], writes=[pb.b])
                    k.evac(xT.t[:, 4 * g:4 * g + 4, tt * 128:(tt + 1) * 128], pb.t[:].rearrange("p (a b) -> p a b", a=4), reads=[pb.b], writes=[xT.b])
            for blk in blocks:
                W_ = Wb[wi % 2]
                wi += 1
                c.dma("pool", lambda e, W_=W_, blk=blk: e.dma_start(out=W_.t[:], in_=k.w_in.rearrange("(kc p) n -> p kc n", p=128)[:, :, blk * 512:(blk + 1) * 512]), writes=[W_.b])
                if blk < 7:
                    for sub in range(4):
                        s_ = stg[si % 2]
                        si += 1
                        for (t0, n) in tok_tiles(ntok):
                            pb = k.ps[4 + pi % 4]
                            pi += 1
                            for kc in range(16):
                                c.op("pe", lambda e, pb=pb, kc=kc, W_=W_, sub=sub, t0=t0, n=n: e.matmul(pb.t[:, 0:n], lhsT=W_.t[:, kc, sub * 128:(sub + 1) * 128], rhs=xT.t[:, kc, t0:t0 + n], start=(kc == 0), stop=(kc == 15)), reads=[W_.b, xT.b], writes=[pb.b])
                            k.evac(s_.t[:, t0:t0 + n], pb.t[:, 0:n], reads=[pb.b], writes=[s_.b])
                        if blk == 0:
                            dst = k.uT[sub][:, tok0:tok0 + ntok]
                        elif blk < 4:
                            dst = k.qT[(blk - 1) * 4 + sub][:, 0:ntok]
                        else:
                            dst = k.kT[(blk - 4) * 4 + sub][:, tok0:tok0 + ntok]
                        c.dma("sp", lambda e, dst=dst, s_=s_, ntok=ntok: e.dma_start(out=dst, in_=s_.t[:, 0:ntok]), reads=[s_.b])
                else:
                    for tt in range(ntok // 128):
                        pb = k.ps[4 + pi % 4]
                        pi += 1
                        v_ = vst[vi % 3]
                        vi += 1
                        for kc in range(16):
                            c.op("pe", lambda e, pb=pb, kc=kc, W_=W_, tt=tt: e.matmul(pb.t[:], lhsT=xT.t[:, kc, tt * 128:(tt + 1) * 128], rhs=W_.t[:, kc, :], start=(kc == 0), stop=(kc == 15)), reads=[W_.b, xT.b], writes=[pb.b])
                        k.evac(v_.t[:], pb.t[:], reads=[pb.b], writes=[v_.b])
                        r0 = tok0 + tt * 128
                        c.dma("sp", lambda e, v_=v_, r0=r0, blk=blk: e.dma_start(out=k.vS[r0:r0 + 128, (blk - 7) * 512:(blk - 6) * 512], in_=v_.t[:]), reads=[v_.b])
        c.flush()


def range_reduce_sin(k, out, ang, tmp_i, tmp_f, bufs_r, bufs_w, eng="dve"):
    c = k.c
    c.op(eng, lambda e: e.tensor_scalar(out=tmp_i, in0=ang, scalar1=1.0 / TWO_PI, scalar2=None, op0=ALU.mult), reads=bufs_r, writes=bufs_w)
    c.op(eng, lambda e: e.tensor_copy(out=tmp_f, in_=tmp_i), reads=bufs_w, writes=bufs_w)
    c.op(eng, lambda e: e.scalar_tensor_tensor(out=tmp_f, in0=tmp_f, scalar=-TWO_PI, in1=ang, op0=ALU.mult, op1=ALU.add), reads=list(bufs_r) + list(bufs_w), writes=bufs_w)
    c.op("act", lambda e: e.activation(out=out, in_=tmp_f, func=AF.Sin), reads=bufs_w, writes=bufs_w)


def phase_s5(k):
    nc, c = k.nc, k.c
    NCH = 4096 // LC
    first_ext_chunk = HIST // LC
    with ExitStack() as st:
        uT = k.tile(st, "s5uT", [128, 4, 4096], BF16)
        cosT = k.tile(st, "cosT", [128, 16, LC + 1], F32)
        sinT = k.tile(st, "sinT", [128, 16, LC + 1], F32)
        BT = k.tile(st, "BT", [128, 32, 128], BF16)
        CT = k.tile(st, "CT", [128, 32, 128], BF16)
        par = k.tile(st, "s5par", [128, 12, 16], F32)
        zin = [k.tile(st, f"zin{i}", [128, 2], F32) for i in range(16)]
        dcol = k.tile(st, "dcol", [128, 4], F32)
        bglu = k.tile(st, "bglu", [128, 4], F32)
        wglu = k.tile(st, "wglu", [128, 4, 512], BF16)
        iot = k.tile(st, "iot", [128, LC + 1], F32)
        LR, LI, DT, TH, MAG, LBR, LBI, CR, CI, T0, T1, T2 = range(12)
        c.dma("sp", lambda e: e.dma_start(out=par.t[:, LR, :], in_=k.lam_re.rearrange("(gp gl) n -> (gl n) gp", gl=2), allow_slow_non_contiguous=True), writes=[par.b])
        c.dma("sp", lambda e: e.dma_start(out=par.t[:, LI, :], in_=k.lam_im.rearrange("(gp gl) n -> (gl n) gp", gl=2), allow_slow_non_contiguous=True), writes=[par.b])
        for gl in range(2):
            c.dma("sp", lambda e, gl=gl: e.dma_start(out=par.t[gl * 64:(gl + 1) * 64, DT, :], in_=k.log_dt.rearrange("(gp gl) -> gp gl", gl=2)[:, gl].partition_broadcast(64)), writes=[par.b])
        c.dma("sp", lambda e: e.dma_start(out=dcol.t[:], in_=k.ab_d.rearrange("(ct p) -> p ct", p=128), allow_slow_non_contiguous=True), writes=[dcol.b])
        c.dma("sp", lambda e: e.dma_start(out=bglu.t[:], in_=k.b_glu.rearrange("(ct p) -> p ct", p=128), allow_slow_non_contiguous=True), writes=[bglu.b])
        c.dma("pool", lambda e: e.dma_start(out=wglu.t[:], in_=k.w_glu.rearrange("(kc p) n -> p kc n", p=128)), writes=[wglu.b])
        c.dma("sp", lambda e: e.dma_start(out=uT.t[:], in_=k.uT_all.rearrange("ct p t -> p ct t")), writes=[uT.b])
        ioti = k.tile(st, "ioti", [128, LC + 1], I32)
        c.op("pool", lambda e: e.iota(ioti.t[:], pattern=[[1, LC + 1]], base=0, channel_multiplier=0), writes=[ioti.b])
        c.op("dve", lambda e: e.tensor_copy(out=iot.t[:], in_=ioti.t[:]), reads=[ioti.b], writes=[iot.b])
        for z_ in zin:
            c.op("pool", lambda e, z_=z_: e.memset(z_.t[:], 0.0), writes=[z_.b])
        P = lambda i: par.t[:, i, :]
        c.op("act", lambda e: e.activation(out=P(DT), in_=P(DT), func=AF.Exp), reads=[par.b], writes=[par.b])
        c.op("dve", lambda e: e.tensor_tensor(out=P(TH), in0=P(LI), in1=P(DT), op=ALU.mult), reads=[par.b], writes=[par.b])
        c.op("dve", lambda e: e.tensor_tensor(out=P(T0), in0=P(LR), in1=P(DT), op=ALU.mult), reads=[par.b], writes=[par.b])
        c.op("act", lambda e: e.activation(out=P(MAG), in_=P(T0), func=AF.Exp), reads=[par.b], writes=[par.b])
        import os
        s5stop = int(os.environ.get("S5STOP", "9"))
        if s5stop == 1:
            c.flush()
            return
        with ExitStack() as st2:
            ang = k.tile(st2, "ang", [128, LC + 1], F32)
            ti = k.tile(st2, "ti", [128, LC + 1], I32)
            tf = k.tile(st2, "tf", [128, LC + 1], F32)
            for gp in range(16):
                c.op("dve", lambda e, gp=gp: e.tensor_scalar(out=ang.t[:], in0=iot.t[:], scalar1=par.t[:, TH, gp:gp + 1], scalar2=None, op0=ALU.mult), reads=[iot.b, par.b], writes=[ang.b])
                range_reduce_sin(k, sinT.t[:, gp, :], ang.t[:], ti.t[:], tf.t[:], [ang.b], [ti.b, tf.b, sinT.b])
                c.op("dve", lambda e: e.tensor_scalar(out=ang.t[:], in0=ang.t[:], scalar1=float(np.pi / 2), scalar2=None, op0=ALU.add), reads=[ang.b, ti.b, tf.b], writes=[ang.b])
                range_reduce_sin(k, cosT.t[:, gp, :], ang.t[:], ti.t[:], tf.t[:], [ang.b], [ti.b, tf.b, cosT.b])
            if s5stop == 2:
                c.flush()
                return
            c.op("dve", lambda e: e.tensor_tensor(out=P(LBR), in0=P(MAG), in1=cosT.t[:, :, 1], op=ALU.mult), reads=[par.b, cosT.b], writes=[par.b])
            c.op("dve", lambda e: e.tensor_tensor(out=P(LBI), in0=P(MAG), in1=sinT.t[:, :, 1], op=ALU.mult), reads=[par.b, sinT.b], writes=[par.b])
            c.op("dve", lambda e: e.tensor_tensor(out=P(T0), in0=P(LR), in1=P(LR), op=ALU.mult), reads=[par.b], writes=[par.b])
            c.op("dve", lambda e: e.tensor_tensor(out=P(T1), in0=P(LI), in1=P(LI), op=ALU.mult), reads=[par.b], writes=[par.b])
            c.op("dve", lambda e: e.tensor_tensor(out=P(T0), in0=P(T0), in1=P(T1), op=ALU.add), reads=[par.b], writes=[par.b])
            c.op("dve", lambda e: e.reciprocal(out=P(T2), in_=P(T0)), reads=[par.b], writes=[par.b])
            c.op("dve", lambda e: e.tensor_scalar(out=P(LBR), in0=P(LBR), scalar1=-1.0, scalar2=None, op0=ALU.add), reads=[par.b], writes=[par.b])
            c.op("dve", lambda e: e.tensor_tensor(out=P(T0), in0=P(LBR), in1=P(LR), op=ALU.mult), reads=[par.b], writes=[par.b])
            c.op("dve", lambda e: e.tensor_tensor(out=P(T1), in0=P(LBI), in1=P(LI), op=ALU.mult), reads=[par.b], writes=[par.b])
            c.op("dve", lambda e: e.tensor_tensor(out=P(T0), in0=P(T0), in1=P(T1), op=ALU.add), reads=[par.b], writes=[par.b])
            c.op("dve", lambda e: e.tensor_tensor(out=P(CR), in0=P(T0), in1=P(T2), op=ALU.mult), reads=[par.b], writes=[par.b])
            c.op("dve", lambda e: e.tensor_tensor(out=P(T0), in0=P(LBI), in1=P(LR), op=ALU.mult), reads=[par.b], writes=[par.b])
            c.op("dve", lambda e: e.tensor_tensor(out=P(T1), in0=P(LBR), in1=P(LI), op=ALU.mult), reads=[par.b], writes=[par.b])
            c.op("dve", lambda e: e.tensor_tensor(out=P(T0), in0=P(T0), in1=P(T1), op=ALU.subtract), reads=[par.b], writes=[par.b])
            c.op("dve", lambda e: e.tensor_tensor(out=P(CI), in0=P(T0), in1=P(T2), op=ALU.mult), reads=[par.b], writes=[par.b])
            if s5stop == 3:
                c.flush()
                return
            zr = [k.tile(st2, f"zr{i}", [128, 128], F32) for i in range(2)]
            zi = [k.tile(st2, f"zi{i}", [128, 128], F32) for i in range(2)]
            yr = [k.tile(st2, f"yr{i}", [128, 128], F32) for i in range(2)]
            yi = [k.tile(st2, f"yi{i}", [128, 128], F32) for i in range(2)]
            bb = [k.tile(st2, f"bb{i}", [128, 2, 128], F32) for i in range(2)]
            for gp in range(16):
                i = gp % 2
                Zr, Zi, Yr, Yi, Bb = zr[i], zi[i], yr[i], yi[i], bb[i]
                for T_ in (Zr, Zi, Yr, Yi):
                    c.op("pool", lambda e, T_=T_: e.memset(T_.t[:], 0.0), writes=[T_.b])
                for gl in range(2):
                    g = 2 * gp + gl
                    c0 = (g % 8) * 16
                    c.dma("sp", lambda e, Zr=Zr, g=g, gl=gl, c0=c0: e.dma_start(out=Zr.t[gl * 64:(gl + 1) * 64, c0:c0 + 16], in_=k.b_re[g]), reads=[Zr.b], writes=[Zr.b])
                    c.dma("sp", lambda e, Zi=Zi, g=g, gl=gl, c0=c0: e.dma_start(out=Zi.t[gl * 64:(gl + 1) * 64, c0:c0 + 16], in_=k.b_im[g]), reads=[Zi.b], writes=[Zi.b])
                    c.dma("sp", lambda e, Yr=Yr, g=g, gl=gl, c0=c0: e.dma_start(out=Yr.t[c0:c0 + 16, gl * 64:(gl + 1) * 64], in_=k.c_re[g]), reads=[Yr.b], writes=[Yr.b])
                    c.dma("sp", lambda e, Yi=Yi, g=g, gl=gl, c0=c0: e.dma_start(out=Yi.t[c0:c0 + 16, gl * 64:(gl + 1) * 64], in_=k.c_im[g]), reads=[Yi.b], writes=[Yi.b])
                cr = par.t[:, CR, gp:gp + 1]
                ci = par.t[:, CI, gp:gp + 1]
                c.op("dve", lambda e, Bb=Bb, Zi=Zi, ci=ci: e.tensor_scalar(out=Bb.t[:, 0, :], in0=Zi.t[:], scalar1=ci, scalar2=None, op0=ALU.mult), reads=[Zi.b, par.b], writes=[Bb.b])
                c.op("dve", lambda e, Bb=Bb, Zr=Zr, cr=cr: e.scalar_tensor_tensor(out=Bb.t[:, 0, :], in0=Zr.t[:], scalar=cr, in1=Bb.t[:, 0, :], op0=ALU.mult, op1=ALU.subtract), reads=[Zr.b, par.b, Bb.b], writes=[Bb.b])
                c.op("dve", lambda e, Bb=Bb, Zr=Zr, ci=ci: e.tensor_scalar(out=Bb.t[:, 1, :], in0=Zr.t[:], scalar1=ci, scalar2=None, op0=ALU.mult), reads=[Zr.b, par.b], writes=[Bb.b])
                c.op("dve", lambda e, Bb=Bb, Zi=Zi, cr=cr: e.scalar_tensor_tensor(out=Bb.t[:, 1, :], in0=Zi.t[:], scalar=cr, in1=Bb.t[:, 1, :], op0=ALU.mult, op1=ALU.add), reads=[Zi.b, par.b, Bb.b], writes=[Bb.b])
                if s5stop == 5:
                    continue
                pb = k.ps[gp % 2]
                for part in range(2):
                    c.op("pe", lambda e, pb=pb, Bb=Bb, part=part: e.transpose(pb.t[:, part * 128:(part + 1) * 128], Bb.t[:, part, :], k.ident.t[:]), reads=[Bb.b, k.ident.b], writes=[pb.b])
                c.op("pe", lambda e, pb=pb, Yr=Yr: e.transpose(pb.t[:, 256:384], Yr.t[:], k.ident.t[:]), reads=[Yr.b, k.ident.b], writes=[pb.b])
                c.op("pe", lambda e, pb=pb, Yi=Yi: e.transpose(pb.t[:, 384:512], Yi.t[:], k.ident.t[:]), reads=[Yi.b, k.ident.b], writes=[pb.b])
                if s5stop == 6:
                    continue
                c.op("dve", lambda e, pb=pb, gp=gp: e.tensor_copy(out=BT.t[:, 2 * gp:2 * gp + 2, :], in_=pb.t[:, 0:256].rearrange("p (a b) -> p a b", a=2)), reads=[pb.b], writes=[BT.b])
                if s5stop == 7:
                    continue
                c.op("dve", lambda e, pb=pb, gp=gp: e.tensor_copy(out=CT.t[:, 2 * gp, :], in_=pb.t[:, 256:384]), reads=[pb.b], writes=[CT.b])
                c.op("dve", lambda e, pb=pb, gp=gp: e.tensor_scalar(out=CT.t[:, 2 * gp + 1, :], in0=pb.t[:, 384:512], scalar1=-1.0, scalar2=None, op0=ALU.mult), reads=[pb.b], writes=[CT.b])
            c.flush()
        if s5stop in (4, 5, 6, 7):
            return
        with ExitStack() as st3:
            NW = 2
            cre = [k.tile(st3, f"cre{i}", [128, LC], F32) for i in range(NW)]
            cim = [k.tile(st3, f"cim{i}", [128, LC], F32) for i in range(NW)]
            t1 = [k.tile(st3, f"t1_{i}", [128, LC], F32) for i in range(NW)]
            t2 = [k.tile(st3, f"t2_{i}", [128, LC], F32) for i in range(NW)]
            zre = [k.tile(st3, f"zre{i}", [128, LC], F32) for i in range(NW)]
            zim = [k.tile(st3, f"zim{i}", [128, LC], F32) for i in range(NW)]
            sre = [k.tile(st3, f"sre{i}", [128, LC], BF16) for i in range(8)]
            sim = [k.tile(st3, f"sim{i}", [128, LC], BF16) for i in range(8)]
            ctmp = [k.tile(st3, f"cz{i}", [128, 2], F32) for i in range(NW)]
            vv = [k.tile(st3, f"vv{i}", [128, LC], F32) for i in range(2)]
            gt = [k.tile(st3, f"gt{i}", [128, 4, LC], BF16) for i in range(2)]
            sg = [k.tile(st3, f"sg{i}", [128, LC], F32) for i in range(2)]
            ya = [k.tile(st3, f"ya{i}", [128, LC], BF16) for i in range(4)]
            py7 = TB(k.ps[7].t, c.buf("ps7a"))
            pg7 = TB(k.ps[7].t, c.buf("ps7b"))

            def s5_gen():
                wk = 0
                for ch in range(NCH):
                    t0 = ch * LC
                    need_y = ch >= first_ext_chunk
                    G_ = gt[ch % 2]
                    for ct in range(4):
                        py = py7
                        for q in range(4):
                            gp = 4 * ct + q
                            yield
                            w = wk % NW
                            wk += 1
                            eA = "dve" if gp % 2 == 0 else "pool"
                            pb = k.ps[6]
                            for part in range(2):
                                c.op("pe", lambda e, pb=pb, gp=gp, part=part, ct=ct, t0=t0: e.matmul(pb.t[:, part * LC:(part + 1) * LC], lhsT=BT.t[:, 2 * gp + part, :], rhs=uT.t[:, ct, t0:t0 + LC], start=True, stop=True), reads=[BT.b, uT.b], writes=[pb.b])
                            bre = pb.t[:, 0:LC]
                            bim = pb.t[:, LC:2 * LC]
                            cs = cosT.t[:, gp, 0:LC]
                            sn = sinT.t[:, gp, 0:LC]
                            c.op("dve", lambda e, w=w, bre=bre, cs=cs: e.tensor_tensor(out=t1[w].t[:], in0=bre, in1=cs, op=ALU.mult), reads=[pb.b, cosT.b], writes=[t1[w].b])
                            c.op("dve", lambda e, w=w, bim=bim, sn=sn: e.tensor_tensor(out=t2[w].t[:], in0=bim, in1=sn, op=ALU.mult), reads=[pb.b, sinT.b], writes=[t2[w].b])
                            c.op(eA, lambda e, w=w: e.tensor_tensor(out=cre[w].t[:], in0=t1[w].t[:], in1=t2[w].t[:], op=ALU.add), reads=[t1[w].b, t2[w].b], writes=[cre[w].b])
                            c.op("dve", lambda e, w=w, bim=bim, cs=cs: e.tensor_tensor(out=t1[w].t[:], in0=bim, in1=cs, op=ALU.mult), reads=[pb.b, cosT.b, cre[w].b], writes=[t1[w].b])
                            c.op("dve", lambda e, w=w, bre=bre, sn=sn: e.tensor_tensor(out=t2[w].t[:], in0=bre, in1=sn, op=ALU.mult), reads=[pb.b, sinT.b, cre[w].b], writes=[t2[w].b])
                            c.op(eA, lambda e, w=w: e.tensor_tensor(out=cim[w].t[:], in0=t1[w].t[:], in1=t2[w].t[:], op=ALU.subtract), reads=[t1[w].b, t2[w].b], writes=[cim[w].b])
                            c.op("dve", lambda e, w=w, gp=gp: e.tensor_tensor_scan(out=zre[w].t[:], data0=par.t[:, MAG, gp:gp + 1].broadcast_to([128, LC]), data1=cre[w].t[:], initial=zin[gp].t[:, 0:1], op0=ALU.mult, op1=ALU.add), reads=[par.b, cre[w].b, zin[gp].b], writes=[zre[w].b])
                            c.op("dve", lambda e, w=w, gp=gp: e.tensor_tensor_scan(out=zim[w].t[:], data0=par.t[:, MAG, gp:gp + 1].broadcast_to([128, LC]), data1=cim[w].t[:], initial=zin[gp].t[:, 1:2], op0=ALU.mult, op1=ALU.add), reads=[par.b, cim[w].b, zin[gp].b], writes=[zim[w].b])
                            cL = cosT.t[:, gp, LC:LC + 1]
                            sL = sinT.t[:, gp, LC:LC + 1]
                            c.op(eA, lambda e, w=w, sL=sL: e.tensor_scalar(out=ctmp[w].t[:, 0:1], in0=zim[w].t[:, LC - 1:LC], scalar1=sL, scalar2=None, op0=ALU.mult), reads=[zim[w].b, sinT.b], writes=[ctmp[w].b])
                            c.op(eA, lambda e, w=w, sL=sL: e.tensor_scalar(out=ctmp[w].t[:, 1:2], in0=zre[w].t[:, LC - 1:LC], scalar1=sL, scalar2=None, op0=ALU.mult), reads=[zre[w].b, sinT.b], writes=[ctmp[w].b])
                            c.op("dve", lambda e, w=w, cL=cL, gp=gp: e.scalar_tensor_tensor(out=zin[gp].t[:, 0:1], in0=zre[w].t[:, LC - 1:LC], scalar=cL, in1=ctmp[w].t[:, 0:1], op0=ALU.mult, op1=ALU.subtract), reads=[zre[w].b, cosT.b, ctmp[w].b], writes=[zin[gp].b])
                            c.op("dve", lambda e, w=w, cL=cL, gp=gp: e.scalar_tensor_tensor(out=zin[gp].t[:, 1:2], in0=zim[w].t[:, LC - 1:LC], scalar=cL, in1=ctmp[w].t[:, 1:2], op0=ALU.mult, op1=ALU.add), reads=[zim[w].b, cosT.b, ctmp[w].b], writes=[zin[gp].b])
                            if not need_y:
                                continue
                            s8 = (4 * ct + q) % 8
                            eB = "pool" if gp % 2 == 0 else "dve"
                            c.op(eB, lambda e, w=w, cs=cs: e.tensor_tensor(out=t1[w].t[:], in0=zre[w].t[:], in1=cs, op=ALU.mult), reads=[zre[w].b, cosT.b, cim[w].b], writes=[t1[w].b])
                            c.op(eB, lambda e, w=w, sn=sn: e.tensor_tensor(out=t2[w].t[:], in0=zim[w].t[:], in1=sn, op=ALU.mult), reads=[zim[w].b, sinT.b, cim[w].b], writes=[t2[w].b])
                            c.op(eB, lambda e, w=w, s8=s8: e.tensor_tensor(out=sre[s8].t[:], in0=t1[w].t[:], in1=t2[w].t[:], op=ALU.subtract), reads=[t1[w].b, t2[w].b], writes=[sre[s8].b])
                            c.op(eB, lambda e, w=w, sn=sn: e.tensor_tensor(out=t1[w].t[:], in0=zre[w].t[:], in1=sn, op=ALU.mult), reads=[zre[w].b, sinT.b, sre[s8].b], writes=[t1[w].b])
                            c.op(eB, lambda e, w=w, cs=cs: e.tensor_tensor(out=t2[w].t[:], in0=zim[w].t[:], in1=cs, op=ALU.mult), reads=[zim[w].b, cosT.b, sre[s8].b], writes=[t2[w].b])
                            c.op(eB, lambda e, w=w, s8=s8: e.tensor_tensor(out=sim[s8].t[:], in0=t1[w].t[:], in1=t2[w].t[:], op=ALU.add), reads=[t1[w].b, t2[w].b], writes=[sim[s8].b])
                            c.op("pe", lambda e, py=py, gp=gp, s8=s8, q=q: e.matmul(py.t[:, 0:LC], lhsT=CT.t[:, 2 * gp, :], rhs=sre[s8].t[:], start=(q == 0), stop=False), reads=[CT.b, sre[s8].b], writes=[py.b])
                            c.op("pe", lambda e, py=py, gp=gp, s8=s8, q=q: e.matmul(py.t[:, 0:LC], lhsT=CT.t[:, 2 * gp + 1, :], rhs=sim[s8].t[:], start=False, stop=(q == 3)), reads=[CT.b, sim[s8].b], writes=[py.b])
                        if need_y:
                            V_ = vv[ct % 2]
                            c.op("dve", lambda e, V_=V_, py=py, ct=ct, t0=t0: e.scalar_tensor_tensor(out=V_.t[:], in0=uT.t[:, ct, t0:t0 + LC], scalar=dcol.t[:, ct:ct + 1], in1=py.t[:, 0:LC], op0=ALU.mult, op1=ALU.add), reads=[uT.b, dcol.b, py.b], writes=[V_.b])
                            c.op("act", lambda e, V_=V_, G_=G_, ct=ct: e.activation(out=G_.t[:, ct, :], in_=V_.t[:], func=AF.Gelu), reads=[V_.b], writes=[G_.b])
                    if need_y:
                        lo = max(t0, HIST)
                        for ot in range(4):
                            pg = pg7
                            for kc in range(4):
                                c.op("pe", lambda e, pg=pg, kc=kc, ot=ot, G_=G_: e.matmul(pg.t[:, LC:2 * LC], lhsT=wglu.t[:, kc, ot * 128:(ot + 1) * 128], rhs=G_.t[:, kc, :], start=(kc == 0), stop=(kc == 3)), reads=[wglu.b, G_.b], writes=[pg.b])
                            S_ = sg[ot % 2]
                            Y_ = ya[ot]
                            c.op("act", lambda e, S_=S_, pg=pg, ot=ot: e.activation(out=S_.t[:], in_=pg.t[:, LC:2 * LC], func=AF.Sigmoid, bias=bglu.t[:, ot:ot + 1]), reads=[pg.b, bglu.b], writes=[S_.b])
                            c.op("dve", lambda e, S_=S_, Y_=Y_, G_=G_, ot=ot: e.tensor_tensor(out=Y_.t[:], in0=S_.t[:], in1=G_.t[:, ot, :], op=ALU.mult), reads=[S_.b, G_.b], writes=[Y_.b])
                            c.dma("sp", lambda e, Y_=Y_, ot=ot, lo=lo, t0=t0: e.dma_start(out=k.yT[ot][:, lo - HIST:t0 + LC - HIST], in_=Y_.t[:, lo - t0:LC]), reads=[Y_.b])

            scale = 1.0 / float(np.sqrt(128.0))
            TA = [attn_alloc(k, st3, "a"), attn_alloc(k, st3, "b")]
            gens = [attn_stream(k, list(range(0, 6)), [k.ps[0], k.ps[1], k.ps[2]], TA[0], scale),
                    attn_stream(k, list(range(6, 12)), [k.ps[3], k.ps[4], k.ps[5]], TA[1], scale),
                    s5_gen()]
            alive = [True, True, True]
            step = 0
            while any(alive):
                for gi, g in enumerate(gens):
                    if not alive[gi]:
                        continue
                    if gi == 2 and step % 3 != 0 and (alive[0] or alive[1]):
                        continue
                    try:
                        next(g)
                    except StopIteration:
                        alive[gi] = False
                step += 1
            c.flush()


def attn_alloc(k, st, sfx):
    T = {}
    T["kT"] = k.tile(st, f"kTs{sfx}", [128, 4096], BF16)
    T["qT"] = k.tile(st, f"qTs{sfx}", [128, NEXT], BF16)
    T["V"] = k.tile(st, f"Vs{sfx}", [128, 32, 128], BF16)
    T["e"] = [k.tile(st, f"e_sb{sfx}{i}", [128, 512], F32) for i in range(3)]
    T["L"] = [k.tile(st, f"L_sb{sfx}{i}", [128, 512], BF16) for i in range(3)]
    T["g"] = [k.tile(st, f"g_sb{sfx}{i}", [128, 512], F32) for i in range(2)]
    T["w"] = [k.tile(st, f"w_sb{sfx}{i}", [128, 512], BF16) for i in range(3)]
    T["yb"] = k.tile(st, f"yb{sfx}", [128, 512], BF16)
    return T


def attn_qtile_gen(k, h, q0e, nq, banks, T, scale):
    c = k.c
    kT_, qT_, V_ = T["kT"], T["qT"], T["V"]
    e_sb, L_sb, g_sb, w_sb, Y_ = T["e"], T["L"], T["g"], T["w"], T["yb"]
    ps_s, ps_cs, ps_o = banks
    q0 = HIST + q0e
    kb_max = (q0 + nq) // 128 - 1
    kbs = list(range(kb_max, -1, -1))
    n = len(kbs)

    def diag_m(kb):
        return (kb * 128 - q0) // 128 if kb * 128 >= q0 else None

    def emit_S(i):
        kb = kbs[i]
        E_ = e_sb[i % len(e_sb)]
        c.op("pe", lambda e: e.matmul(ps_s.t[:, 0:nq], lhsT=kT_.t[:, kb * 128:(kb + 1) * 128], rhs=qT_.t[:, q0e:q0e + nq], start=True, stop=True), reads=[kT_.b, qT_.b], writes=[ps_s.b])
        c.op("act", lambda e: e.activation(out=E_.t[:, 0:nq], in_=ps_s.t[:, 0:nq], func=AF.Exp, scale=scale, bias=k.kb_sb.t[:, kb:kb + 1]), reads=[ps_s.b, k.kb_sb.b], writes=[E_.b])

    def emit_L(i):
        kb = kbs[i]
        E_, L_ = e_sb[i % len(e_sb)], L_sb[i % len(L_sb)]
        c.op("act", lambda e: e.activation(out=L_.t[:, 0:nq], in_=E_.t[:, 0:nq], func=AF.Ln, bias=1.0), reads=[E_.b], writes=[L_.b])
        m = diag_m(kb)
        if m is not None:
            c.op("pool", lambda e: e.tensor_tensor(out=L_.t[:, 0:nq], in0=L_.t[:, 0:nq], in1=k.cmask.t[:, m, 0:nq], op=ALU.mult), reads=[L_.b, k.cmask.b], writes=[L_.b])

    def emit_WV(i):
        kb = kbs[i]
        W_ = w_sb[i % len(w_sb)]
        c.op("pe", lambda e: e.matmul(ps_o.t[:, 0:nq], lhsT=V_.t[:, kb, :], rhs=W_.t[:, 0:nq], start=(i == 0), stop=(i == n - 1)), reads=[V_.b, W_.b], writes=[ps_o.b])

    def emit_strict(i):
        L_ = L_sb[i % len(L_sb)]
        c.op("pe", lambda e: e.matmul(ps_cs.t[:, 0:nq], lhsT=k.strl.t[:], rhs=L_.t[:, 0:nq], start=False, stop=True, skip_group_check=True), reads=[k.strl.b, L_.b], writes=[ps_cs.b])

    emit_S(0)
    emit_L(0)
    for i in range(n):
        kb = kbs[i]
        if i + 1 < n:
            emit_S(i + 1)
        if i > 0:
            emit_strict(i - 1)
        E_, L_, G_, W_ = e_sb[i % len(e_sb)], L_sb[i % len(L_sb)], g_sb[i % len(g_sb)], w_sb[i % len(w_sb)]
        c.op("pe", lambda e, L_=L_, i=i: e.matmul(ps_cs.t[:, 0:nq], lhsT=k.triu.t[:], rhs=L_.t[:, 0:nq], start=(i == 0), stop=True, skip_group_check=True), reads=[k.triu.b, L_.b], writes=[ps_cs.b])
        c.op("act", lambda e, G_=G_: e.activation(out=G_.t[:, 0:nq], in_=ps_cs.t[:, 0:nq], func=AF.Exp, scale=-1.0), reads=[ps_cs.b], writes=[G_.b])
        if i + 1 < n:
            emit_L(i + 1)
        if i > 0:
            emit_WV(i - 1)
        c.op("dve", lambda e, E_=E_, G_=G_, W_=W_: e.tensor_tensor(out=W_.t[:, 0:nq], in0=E_.t[:, 0:nq], in1=G_.t[:, 0:nq], op=ALU.mult), reads=[E_.b, G_.b], writes=[W_.b])
        m = diag_m(kb)
        if m is not None:
            c.op("pool", lambda e, W_=W_, m=m: e.tensor_tensor(out=W_.t[:, 0:nq], in0=W_.t[:, 0:nq], in1=k.cmask.t[:, m, 0:nq], op=ALU.mult), reads=[W_.b, k.cmask.b], writes=[W_.b])
        yield
    emit_WV(n - 1)
    k.evac(Y_.t[:, 0:nq], ps_o.t[:, 0:nq], reads=[ps_o.b], writes=[Y_.b], eng="dve")
    c.dma("sp", lambda e: e.dma_start(out=k.yT[4 + h][:, q0e:q0e + nq], in_=Y_.t[:, 0:nq]), reads=[Y_.b])


def attn_stream(k, heads, banks, T, scale):
    c = k.c
    QT = [(0, 128), (128, 512), (640, 512), (1152, 512), (1664, 512)]
    kT_, qT_, V_ = T["kT"], T["qT"], T["V"]
    for h in heads:
        c.dma("sp", lambda e, h=h: e.dma_start(out=kT_.t[:], in_=k.kT[h]), writes=[kT_.b])
        c.dma("sp", lambda e, h=h: e.dma_start(out=qT_.t[:], in_=k.qT[h]), writes=[qT_.b])
        c.dma("sp", lambda e, h=h: e.dma_start(out=V_.t[:], in_=k.vS[:, h * 128:(h + 1) * 128].rearrange("(kb p) d -> p kb d", p=128)), writes=[V_.b])
        for (q0e, nq) in QT:
            yield from attn_qtile_gen(k, h, q0e, nq, banks, T, scale)


def ln_tile(k, r, lnp, S, sbk, tt, res_out_dram, hT_stage, hT32, want_router, gates_sb, rw, rb):
    nc, c = k.nc, k.c
    gB, bB = lnp
    stats, mv, sm = S["stats"], S["mv"], S.get("sm")
    for j in range(4):
        c.op("dve", lambda e, j=j: e.bn_stats(out=stats.t[:, j, :], in_=r.t[:, j * 512:(j + 1) * 512]), reads=[r.b], writes=[stats.b])
    c.op("dve", lambda e: e.bn_aggr(out=mv.t[:], in_=stats.t[:].rearrange("p a b -> p (a b)")), reads=[stats.b], writes=[mv.b])
    c.op("act", lambda e: e.activation(out=mv.t[:, 1:2], in_=mv.t[:, 1:2], func=AF.Sqrt, bias=k.eps.t[:, 0:1]), reads=[mv.b, k.eps.b], writes=[mv.b])
    c.op("dve", lambda e: e.reciprocal(out=mv.t[:, 1:2], in_=mv.t[:, 1:2]), reads=[mv.b], writes=[mv.b])
    c.op("dve", lambda e: e.tensor_scalar(out=r.t[:], in0=r.t[:], scalar1=mv.t[:, 0:1], scalar2=mv.t[:, 1:2], op0=ALU.subtract, op1=ALU.mult), reads=[r.b, mv.b], writes=[r.b])
    c.op("pool", lambda e: e.tensor_tensor(out=r.t[:], in0=r.t[:], in1=gB.t[:], op=ALU.mult), reads=[r.b, gB.b], writes=[r.b])
    c.op("dve", lambda e: e.tensor_tensor(out=r.t[:], in0=r.t[:], in1=bB.t[:], op=ALU.add), reads=[r.b, bB.b], writes=[r.b])
    c.dma("sp", lambda e: e.dma_start(out=res_out_dram, in_=r.t[:]), reads=[r.b])
    cb = sbk * 128
    for g in range(4):
        pb = k.ps[g]
        for j in range(4):
            kc = 4 * g + j
            c.op("pe", lambda e, pb=pb, j=j, kc=kc: e.transpose(pb.t[:, j * 128:(j + 1) * 128], r.t[:, kc * 128:(kc + 1) * 128], k.ident.t[:]), reads=[r.b, k.ident.b], writes=[pb.b])
        src = pb.t[:].rearrange("p (a b) -> p a b", a=4)
        c.op("dve", lambda e, g=g, src=src: e.tensor_copy(out=hT_stage.t[:, 4 * g:4 * g + 4, cb:cb + 128], in_=src), reads=[pb.b], writes=[hT_stage.b])
        if want_router:
            c.op("dve", lambda e, g=g, src=src: e.tensor_copy(out=hT32.t[:, 4 * g:4 * g + 4, :], in_=src), reads=[pb.b], writes=[hT32.b])
    if not want_router:
        return
    pl = k.ps[7]
    hhi, hlo = S["hhi"], S["hlo"]
    rwhi, rwlo = rw
    c.op("dve", lambda e: e.tensor_copy(out=hhi.t[:], in_=hT32.t[:]), reads=[hT32.b], writes=[hhi.b])
    c.op("dve", lambda e: e.tensor_tensor(out=hT32.t[:], in0=hT32.t[:], in1=hhi.t[:], op=ALU.subtract), reads=[hT32.b, hhi.b], writes=[hT32.b])
    c.op("dve", lambda e: e.tensor_copy(out=hlo.t[:], in_=hT32.t[:]), reads=[hT32.b], writes=[hlo.b])
    trip = [(hhi, rwhi), (hlo, rwhi), (hhi, rwlo)]
    for pi3, (ha, wa) in enumerate(trip):
        for kc in range(16):
            c.op("pe", lambda e, kc=kc, ha=ha, wa=wa, pi3=pi3: e.matmul(pl.t[:, 0:16], lhsT=ha.t[:, kc, :], rhs=wa.t[:, kc, :], start=(pi3 == 0 and kc == 0), stop=(pi3 == 2 and kc == 15)), reads=[ha.b, wa.b], writes=[pl.b])
    class _V:
        def __init__(self, t):
            self.t = t
        def __getitem__(self, key):
            a, sl = key
            return self.t[:, sl]
    lg = _V(sm.t)
    LG, PR, MXi, GS, T4, IG, PM, OH1, PM2, OH2, GT = [slice(i * 16, (i + 1) * 16) for i in range(11)]
    mx = lambda a, b: sm.t[:, 32 + a:32 + b]
    smb = [sm.b]
    o = lambda fn, extra=(): c.op("dve", fn, reads=smb + list(extra), writes=smb)
    o(lambda e: e.tensor_tensor(out=lg[:, LG], in0=pl.t[:, 0:16], in1=rb.t[:], op=ALU.add), extra=[pl.b, rb.b])
    o(lambda e: e.tensor_reduce(out=mx(0, 1), in_=lg[:, LG], axis=AX.X, op=ALU.max))
    o(lambda e: e.tensor_scalar(out=lg[:, PR], in0=lg[:, LG], scalar1=mx(0, 1), scalar2=None, op0=ALU.subtract))
    c.op("act", lambda e: e.activation(out=lg[:, PR], in_=lg[:, PR], func=AF.Exp), reads=smb, writes=smb)
    p4 = lg[:, PR].rearrange("p (g e) -> p g e", e=4)
    t4 = lg[:, T4].rearrange("p (g e) -> p g e", e=4)
    gs = lg[:, GS]
    pairs = [(0, 1), (0, 2), (0, 3), (1, 2), (1, 3), (2, 3)]
    for pi_, (a, b) in enumerate(pairs):
        if pi_ == 0:
            o(lambda e, a=a, b=b: e.tensor_tensor(out=gs[:, 0:4], in0=p4[:, :, a], in1=p4[:, :, b], op=ALU.add))
        else:
            o(lambda e, a=a, b=b: e.tensor_tensor(out=gs[:, 4:8], in0=p4[:, :, a], in1=p4[:, :, b], op=ALU.add))
            o(lambda e: e.tensor_tensor(out=gs[:, 0:4], in0=gs[:, 0:4], in1=gs[:, 4:8], op=ALU.max))
    o(lambda e: e.tensor_reduce(out=gs[:, 8:9], in_=gs[:, 0:4], axis=AX.X, op=ALU.max))
    o(lambda e: e.tensor_scalar(out=gs[:, 12:16], in0=gs[:, 0:4], scalar1=gs[:, 8:9], scalar2=None, op0=ALU.is_ge))
    ig = lg[:, IG].rearrange("p (g e) -> p g e", e=4)
    for e4 in range(4):
        o(lambda e, e4=e4: e.tensor_copy(out=ig[:, :, e4], in_=gs[:, 12:16]))
    o(lambda e: e.tensor_tensor(out=lg[:, PM], in0=lg[:, PR], in1=lg[:, IG], op=ALU.mult))
    o(lambda e: e.tensor_tensor(out=lg[:, PM], in0=lg[:, PM], in1=lg[:, IG], op=ALU.add))
    o(lambda e: e.tensor_scalar(out=lg[:, PM], in0=lg[:, PM], scalar1=-1.0, scalar2=None, op0=ALU.add))
    o(lambda e: e.tensor_reduce(out=mx(1, 2), in_=lg[:, PM], axis=AX.X, op=ALU.max))
    o(lambda e: e.tensor_scalar(out=lg[:, OH1], in0=lg[:, PM], scalar1=mx(1, 2), scalar2=None, op0=ALU.is_ge))
    o(lambda e: e.scalar_tensor_tensor(out=lg[:, PM2], in0=lg[:, OH1], scalar=-4.0, in1=lg[:, PM], op0=ALU.mult, op1=ALU.add))
    o(lambda e: e.tensor_reduce(out=mx(2, 3), in_=lg[:, PM2], axis=AX.X, op=ALU.max))
    o(lambda e: e.tensor_scalar(out=lg[:, OH2], in0=lg[:, PM2], scalar1=mx(2, 3), scalar2=None, op0=ALU.is_ge))
    o(lambda e: e.tensor_tensor(out=mx(3, 4), in0=mx(1, 2), in1=mx(2, 3), op=ALU.add))
    o(lambda e: e.reciprocal(out=mx(3, 4), in_=mx(3, 4)))
    o(lambda e: e.tensor_scalar(out=mx(4, 6), in0=mx(1, 3), scalar1=mx(3, 4), scalar2=None, op0=ALU.mult))
    o(lambda e: e.tensor_scalar(out=lg[:, GT], in0=lg[:, OH1], scalar1=mx(4, 5), scalar2=None, op0=ALU.mult))
    c.op("dve", lambda e: e.scalar_tensor_tensor(out=gates_sb.t[:, tt, :], in0=lg[:, OH2], scalar=mx(5, 6), in1=lg[:, GT], op0=ALU.mult, op1=ALU.add), reads=smb, writes=smb + [gates_sb.b])


def load_ln_params(k, st, idx_l, idx_j):
    c = k.c
    gB = k.tile(st, "gB", [128, 2048], F32)
    bB = k.tile(st, "bB", [128, 2048], F32)
    c.dma("sp", lambda e: e.dma_start(out=gB.t[:], in_=k.ln_g[idx_l, idx_j].partition_broadcast(128)), writes=[gB.b])
    c.dma("sp", lambda e: e.dma_start(out=bB.t[:], in_=k.ln_b[idx_l, idx_j].partition_broadcast(128)), writes=[bB.b])
    return gB, bB


def ln_scratch(k, st, router=True):
    if not router:
        return {"stats": k.tile(st, "stats", [128, 4, 6], F32), "mv": k.tile(st, "mv", [128, 2], F32)}
    return {"stats": k.tile(st, "stats", [128, 4, 6], F32), "mv": k.tile(st, "mv", [128, 2], F32), "sm": k.tile(st, "sm", [128, 11 * 16], F32),
            "hhi": k.tile(st, "hhi", [128, 16, 128], BF16), "hlo": k.tile(st, "hlo", [128, 16, 128], BF16)}


def load_router(k, st):
    c = k.c
    rw = k.tile(st, "rw", [128, 16, 16], F32)
    rb = k.tile(st, "rb", [128, 16], F32)
    c.dma("sp", lambda e: e.dma_start(out=rw.t[:], in_=k.router_w.rearrange("(kc p) n -> p kc n", p=128)), writes=[rw.b])
    c.dma("sp", lambda e: e.dma_start(out=rb.t[:], in_=k.router_b.partition_broadcast(128)), writes=[rb.b])
    rwhi = k.tile(st, "rwhi", [128, 16, 16], BF16)
    rwlo = k.tile(st, "rwlo", [128, 16, 16], BF16)
    c.op("dve", lambda e: e.tensor_copy(out=rwhi.t[:], in_=rw.t[:]), reads=[rw.b], writes=[rwhi.b])
    c.op("dve", lambda e: e.tensor_tensor(out=rw.t[:], in0=rw.t[:], in1=rwhi.t[:], op=ALU.subtract), reads=[rw.b, rwhi.b], writes=[rw.b])
    c.op("dve", lambda e: e.tensor_copy(out=rwlo.t[:], in_=rw.t[:]), reads=[rw.b], writes=[rwlo.b])
    return (rwhi, rwlo), rb


def phase_outproj_ln(k, layer, ntok, yT_dram, w_out, res_dram, res_row0, h1_dram, h1T_dram):
    nc, c = k.nc, k.c
    ntt = ntok // 128
    with ExitStack() as st:
        Wo = k.tile(st, "Wo", [128, 16, 2048], BF16)
        yTs = [k.tile(st, f"yTs{i}", [128, 16, 512], BF16) for i in range(2)]
        xr = [k.tile(st, f"xr{i}", [128, 2048], F32) for i in range(2)]
        rr = [k.tile(st, f"rr{i}", [128, 2048], F32) for i in range(2)]
        hst = [k.tile(st, f"hst{i}", [128, 16, 512], BF16) for i in range(2)]
        hT32 = k.tile(st, "hT32", [128, 16, 128], F32)
        lnp = load_ln_params(k, st, layer, 0)
        S = ln_scratch(k, st)
        rw, rb = load_router(k, st)
        for half in range(2):
            c.dma("pool", lambda e, half=half: e.dma_start(out=Wo.t[:, :, half * 1024:(half + 1) * 1024], in_=w_out.rearrange("(kc p) n -> p kc n", p=128)[:, :, half * 1024:(half + 1) * 1024]), reads=[Wo.b], writes=[Wo.b])
        groups = []
        t = 0
        if ntt % 4 == 1:
            groups.append((0, 1))
            t = 1
        while t < ntt:
            groups.append((t, 4))
            t += 4
        for gi, (tt0, ng) in enumerate(groups):
            Y_ = yTs[gi % 2]
            H_ = hst[gi % 2]
            c.dma("sp", lambda e, Y_=Y_, tt0=tt0, ng=ng: e.dma_start(out=Y_.t[:, :, 0:ng * 128], in_=yT_dram[:, :, tt0 * 128:(tt0 + ng) * 128].rearrange("kc p t -> p kc t")), writes=[Y_.b])
            for ti in range(ng):
                tt = tt0 + ti
                X_ = xr[tt % 2]
                R_ = rr[tt % 2]
                r0 = res_row0 + tt * 128
                c.dma("sp", lambda e, X_=X_, r0=r0: e.dma_start(out=X_.t[:], in_=res_dram[r0:r0 + 128, :]), writes=[X_.b])
                for j in range(4):
                    pb = k.ps[4 + j % 2]
                    for kc in range(16):
                        c.op("pe", lambda e, pb=pb, kc=kc, j=j, Y_=Y_, ti=ti: e.matmul(pb.t[:], lhsT=Y_.t[:, kc, ti * 128:(ti + 1) * 128], rhs=Wo.t[:, kc, j * 512:(j + 1) * 512], start=(kc == 0), stop=(kc == 15)), reads=[Y_.b, Wo.b], writes=[pb.b])
                    c.op("dve", lambda e, pb=pb, j=j, X_=X_, R_=R_: e.scalar_tensor_tensor(out=R_.t[:, j * 512:(j + 1) * 512], in0=X_.t[:, j * 512:(j + 1) * 512], scalar=ALPHA, in1=pb.t[:], op0=ALU.mult, op1=ALU.add), reads=[X_.b, pb.b], writes=[R_.b])
                ln_tile(k, R_, lnp, S, ti, tt, h1_dram[tt * 128:(tt + 1) * 128, :], H_, hT32, True, k.gates[layer], rw, rb)
            c.dma("sp", lambda e, H_=H_, tt0=tt0, ng=ng: e.dma_start(out=h1T_dram[:, :, tt0 * 128:(tt0 + ng) * 128].rearrange("kc p t -> p kc t"), in_=H_.t[:, :, 0:ng * 128]), reads=[H_.b])
        c.flush()


def phase_moe(k, layer, supers, hT_dram, h1_dram, out_dram, out_row_of_tile, outT_dram):
    nc, c = k.nc, k.c
    wg, wu, wd = k.moe_wg[layer], k.moe_wu[layer], k.moe_wd[layer]
    gates_sb = k.gates[layer]
    SMAX = max(len(s) for s in supers) * 128
    with ExitStack() as st:
        hT = k.tile(st, "mhT", [128, 16, SMAX], BF16)
        acc = k.tile(st, "macc", [128, SMAX // 128, 2048], F32)
        hid = k.tile(st, "mhid", [128, 8, SMAX], BF16)
        NR = 4 if SMAX <= 768 else 2
        Wg = [k.tile(st, f"mWg{i}", [128, 16, 128], BF16) for i in range(NR)]
        Wu = [k.tile(st, f"mWu{i}", [128, 16, 128], BF16) for i in range(NR)]
        Wd = [k.tile(st, f"mWd{i}", [128, 2, 2048], BF16) for i in range(4)]
        sgt = [k.tile(st, f"msg{i}", [128, 512], BF16) for i in range(2)]
        xr = [k.tile(st, f"mxr{i}", [128, 2048], F32) for i in range(1)]
        hst = k.tile(st, "mhst", [128, 16, 128], BF16)
        lnp = load_ln_params(k, st, layer, 1)
        S = ln_scratch(k, st, router=False)
        wq = 0
        pi = 0
        for tiles in supers:
            ns = len(tiles) * 128
            tt0 = tiles[0]
            c.dma("sp", lambda e, tt0=tt0, ns=ns: e.dma_start(out=hT.t[:, :, 0:ns], in_=hT_dram[:, :, tt0 * 128:tt0 * 128 + ns].rearrange("kc p t -> p kc t")), writes=[hT.b])
            ttl = tok_tiles(ns)
            for ex in range(16):
                for fc in range(8):
                    G_, U_ = Wg[wq % NR], Wu[wq % NR]
                    wq += 1
                    c.dma("pool", lambda e, G_=G_, ex=ex, fc=fc: e.dma_start(out=G_.t[:], in_=wg[ex].rearrange("(kc p) f -> p kc f", p=128)[:, :, fc * 128:(fc + 1) * 128]), writes=[G_.b])
                    c.dma("pool", lambda e, U_=U_, ex=ex, fc=fc: e.dma_start(out=U_.t[:], in_=wu[ex].rearrange("(kc p) f -> p kc f", p=128)[:, :, fc * 128:(fc + 1) * 128]), writes=[U_.b])
                    for (t0, n) in ttl:
                        pg = k.ps[(pi % 2) * 2]
                        pu = k.ps[(pi % 2) * 2 + 1]
                        sg_ = sgt[pi % 2]
                        pi += 1
                        for kc in range(16):
                            c.op("pe", lambda e, pg=pg, G_=G_, kc=kc, t0=t0, n=n: e.matmul(pg.t[:, 0:n], lhsT=G_.t[:, kc, :], rhs=hT.t[:, kc, t0:t0 + n], start=(kc == 0), stop=(kc == 15)), reads=[G_.b, hT.b], writes=[pg.b])
                        for kc in range(16):
                            c.op("pe", lambda e, pu=pu, U_=U_, kc=kc, t0=t0, n=n: e.matmul(pu.t[:, 0:n], lhsT=U_.t[:, kc, :], rhs=hT.t[:, kc, t0:t0 + n], start=(kc == 0), stop=(kc == 15)), reads=[U_.b, hT.b], writes=[pu.b])
                        c.op("act", lambda e, pg=pg, sg_=sg_, n=n: e.activation(out=sg_.t[:, 0:n], in_=pg.t[:, 0:n], func=AF.Silu), reads=[pg.b], writes=[sg_.b])
                        c.op("dve", lambda e, pu=pu, sg_=sg_, fc=fc, t0=t0, n=n: e.tensor_tensor(out=hid.t[:, fc, t0:t0 + n], in0=sg_.t[:, 0:n], in1=pu.t[:, 0:n], op=ALU.mult), reads=[sg_.b, pu.b], writes=[hid.b])
                for pr in range(4):
                    D_ = Wd[pr]
                    c.dma("pool", lambda e, D_=D_, ex=ex, pr=pr: e.dma_start(out=D_.t[:], in_=wd[ex][pr * 256:(pr + 1) * 256, :].rearrange("(fc p) n -> p fc n", p=128)), writes=[D_.b])
                for lt in range(len(tiles)):
                    tt = tiles[lt]
                    for j in range(4):
                        po = k.ps[4 + (lt * 4 + j) % 4]
                        for fc in range(8):
                            D_ = Wd[fc // 2]
                            c.op("pe", lambda e, po=po, fc=fc, D_=D_, lt=lt, j=j: e.matmul(po.t[:], lhsT=hid.t[:, fc, lt * 128:(lt + 1) * 128], rhs=D_.t[:, fc % 2, j * 512:(j + 1) * 512], start=(fc == 0), stop=(fc == 7)), reads=[hid.b, D_.b], writes=[po.b])
                        if ex == 0:
                            c.op("dve", lambda e, po=po, lt=lt, j=j, tt=tt, ex=ex: e.tensor_scalar(out=acc.t[:, lt, j * 512:(j + 1) * 512], in0=po.t[:], scalar1=gates_sb.t[:, tt, ex:ex + 1], scalar2=None, op0=ALU.mult), reads=[po.b, gates_sb.b, acc.b], writes=[acc.b])
                        else:
                            c.op("dve", lambda e, po=po, lt=lt, j=j, tt=tt, ex=ex: e.scalar_tensor_tensor(out=acc.t[:, lt, j * 512:(j + 1) * 512], in0=po.t[:], scalar=gates_sb.t[:, tt, ex:ex + 1], in1=acc.t[:, lt, j * 512:(j + 1) * 512], op0=ALU.mult, op1=ALU.add), reads=[po.b, gates_sb.b, acc.b], writes=[acc.b])
            for lt in range(len(tiles)):
                tt = tiles[lt]
                X_ = xr[0]
                c.dma("sp", lambda e, X_=X_, tt=tt: e.dma_start(out=X_.t[:], in_=h1_dram[tt * 128:(tt + 1) * 128, :]), writes=[X_.b])
                c.op("dve", lambda e, X_=X_, lt=lt: e.scalar_tensor_tensor(out=X_.t[:], in0=X_.t[:], scalar=ALPHA, in1=acc.t[:, lt, :], op0=ALU.mult, op1=ALU.add), reads=[X_.b, acc.b], writes=[X_.b])
                orow = out_row_of_tile(tt)
                dst = out_dram[orow:orow + 128, :] if orow is not None else k.dummy_out[0:128, :]
                ln_tile(k, X_, lnp, S, 0, tt, dst, hst, None, False, None, None, None)
                if outT_dram is not None:
                    c.dma("sp", lambda e, tt=tt: e.dma_start(out=outT_dram[:, :, tt * 128:(tt + 1) * 128].rearrange("kc p t -> p kc t"), in_=hst.t[:, :, 0:128]), reads=[hst.b])
        c.flush()


def phase_pool(k):
    nc, c = k.nc, k.c
    with ExitStack() as st:
        hT = k.tile(st, "phT", [128, 16, NEXT], BF16)
        Wi = [k.tile(st, f"pWi{i}", [128, 16, 512], BF16) for i in range(2)]
        Wgp = [k.tile(st, f"pWg{i}", [128, 4, 512], BF16) for i in range(2)]
        hp = [k.tile(st, f"php{i}", [128, NEXT], F32) for i in range(2)]
        sa = [k.tile(st, f"psa{i}", [128, NEXT], F32) for i in range(2)]
        sb_ = [k.tile(st, f"psb{i}", [128, NEXT], F32) for i in range(2)]
        pl = k.tile(st, "ppl", [128, 4, 2048], BF16)
        ysg = [k.tile(st, f"pys{i}", [128, 2048], BF16) for i in range(2)]
        hm = k.tile(st, "phm", [128, 1], F32)
        ic = k.tile(st, "pic", [128, 64], F32)
        sc = k.tile(st, "psc", [128, 16], F32)
        c.dma("sp", lambda e: e.dma_start(out=hT.t[:], in_=k.h2T.rearrange("kc p t -> p kc t")), writes=[hT.b])
        c.dma("sp", lambda e: e.dma_start(out=hm.t[:], in_=k.halo_mask), writes=[hm.b])
        c.dma("sp", lambda e: e.dma_start(out=ic.t[:], in_=k.invcnt), writes=[ic.b])
        c.dma("sp", lambda e: e.dma_start(out=sc.t[:], in_=k.c_scale.rearrange("(cc p) -> p cc", p=128), allow_slow_non_contiguous=True), writes=[sc.b])
        pi = 0
        for g in range(4):
            w = (2, 4, 8, 16)[g]
            W_ = Wi[g % 2]
            Wg_ = Wgp[g % 2]
            c.dma("pool", lambda e, W_=W_, g=g: e.dma_start(out=W_.t[:], in_=k.c_w_in.rearrange("(kc p) n -> p kc n", p=128)[:, :, g * 512:(g + 1) * 512]), writes=[W_.b])
            c.dma("pool", lambda e, Wg_=Wg_, g=g: e.dma_start(out=Wg_.t[:], in_=k.c_w_group[g].rearrange("(kc p) n -> p kc n", p=128)), writes=[Wg_.b])
            for sub in range(4):
                H_ = hp[sub % 2]
                for (t0, n) in tok_tiles(NEXT):
                    pb = k.ps[pi % 4]
                    pi += 1
                    for kc in range(16):
                        c.op("pe", lambda e, pb=pb, kc=kc, W_=W_, sub=sub, t0=t0, n=n: e.matmul(pb.t[:, 0:n], lhsT=W_.t[:, kc, sub * 128:(sub + 1) * 128], rhs=hT.t[:, kc, t0:t0 + n], start=(kc == 0), stop=(kc == 15)), reads=[W_.b, hT.b], writes=[pb.b])
                    k.evac(H_.t[:, t0:t0 + n], pb.t[:, 0:n], reads=[pb.b], writes=[H_.b])
                c.op("dve", lambda e, H_=H_: e.tensor_scalar(out=H_.t[:, 0:128], in0=H_.t[:, 0:128], scalar1=hm.t[:, 0:1], scalar2=None, op0=ALU.mult), reads=[H_.b, hm.b], writes=[H_.b])
                cur = H_
                step = 1
                pp = [sa[sub % 2], sb_[sub % 2]]
                ii = 0
                while step < w:
                    nxt = pp[ii % 2]
                    ii += 1
                    eng = "pool" if ii % 2 == 0 else "dve"
                    c.op(eng, lambda e, cur=cur, nxt=nxt, step=step: e.tensor_copy(out=nxt.t[:, 0:step], in_=cur.t[:, 0:step]), reads=[cur.b], writes=[nxt.b])
                    c.op(eng, lambda e, cur=cur, nxt=nxt, step=step: e.tensor_tensor(out=nxt.t[:, step:NEXT], in0=cur.t[:, step:NEXT], in1=cur.t[:, 0:NEXT - step], op=ALU.add), reads=[cur.b, nxt.b], writes=[nxt.b])
                    cur = nxt
                    step *= 2
                c.op("dve", lambda e, cur=cur, H_=H_, sub=sub, w=w: e.scalar_tensor_tensor(out=pl.t[:, sub, :], in0=cur.t[:, 128:NEXT], scalar=1.0 / w, in1=H_.t[:, 128:NEXT], op0=ALU.mult, op1=ALU.subtract), reads=[cur.b, H_.b], writes=[pl.b])
                c.op("dve", lambda e, cur=cur, g=g: e.tensor_tensor(out=cur.t[:, 128:144], in0=cur.t[:, 128:144], in1=ic.t[:, g * 16:(g + 1) * 16], op=ALU.mult), reads=[cur.b, ic.b, pl.b], writes=[cur.b])
                c.op("dve", lambda e, cur=cur, H_=H_, sub=sub: e.tensor_tensor(out=pl.t[:, sub, 0:16], in0=cur.t[:, 128:144], in1=H_.t[:, 128:144], op=ALU.subtract), reads=[cur.b, H_.b], writes=[pl.b])
            for oc in range(4):
                Y_ = ysg[oc % 2]
                cc = g * 4 + oc
                for (t0, n) in tok_tiles(2048):
                    pb = k.ps[4 + pi % 4]
                    pi += 1
                    for kc in range(4):
                        c.op("pe", lambda e, pb=pb, kc=kc, Wg_=Wg_, oc=oc, t0=t0, n=n: e.matmul(pb.t[:, 0:n], lhsT=Wg_.t[:, kc, oc * 128:(oc + 1) * 128], rhs=pl.t[:, kc, t0:t0 + n], start=(kc == 0), stop=(kc == 3)), reads=[Wg_.b, pl.b], writes=[pb.b])
                    c.op("dve", lambda e, pb=pb, Y_=Y_, cc=cc, t0=t0, n=n: e.tensor_scalar(out=Y_.t[:, t0:t0 + n], in0=pb.t[:, 0:n], scalar1=sc.t[:, cc:cc + 1], scalar2=None, op0=ALU.mult), reads=[pb.b, sc.b], writes=[Y_.b])
                c.dma("sp", lambda e, Y_=Y_, cc=cc: e.dma_start(out=k.y1T[cc], in_=Y_.t[:]), reads=[Y_.b])
        c.flush()


def build(debug=False, stop_after=None, small_moe=False):
    nc = bass.Bass("TRN2", target_bir_lowering=False)
    k = K(nc, debug)

    def din(name, shape):
        return nc.dram_tensor(name, shape, F32, kind="ExternalInput").ap()

    def scr(name, shape, dt):
        kind = "ExternalOutput" if (debug and name in DEBUG_OUTS) else "Internal"
        return nc.dram_tensor(name, shape, dt, kind=kind).ap()

    k.x_loc = din("x_loc", [4096, 2048])
    k.kbias = din("kbias", [32, 128])
    k.halo_mask = din("halo_mask", [128, 1])
    k.invcnt = din("invcnt", [128, 64])
    k.w_in = din("ab_w_in", [2048, 5120])
    k.lam_re = din("ab_lambda_re", [32, 64])
    k.lam_im = din("ab_lambda_im", [32, 64])
    k.log_dt = din("ab_log_dt", [32])
    k.b_re = din("ab_b_re", [32, 64, 16])
    k.b_im = din("ab_b_im", [32, 64, 16])
    k.c_re = din("ab_c_re", [32, 16, 64])
    k.c_im = din("ab_c_im", [32, 16, 64])
    k.ab_d = din("ab_d", [512])
    k.w_glu = din("ab_w_glu", [512, 512])
    k.b_glu = din("ab_b_glu", [512])
    k.ab_w_out = din("ab_w_out", [2048, 2048])
    k.c_w_in = din("c_w_in", [2048, 2048])
    k.c_w_group = din("c_w_group", [4, 512, 512])
    k.c_scale = din("c_scale", [2048])
    k.c_w_out = din("c_w_out", [2048, 2048])
    k.ln_g = din("ln_g", [2, 2, 2048])
    k.ln_b = din("ln_b", [2, 2, 2048])
    k.router_w = din("router_w", [2048, 16])
    k.router_b = din("router_b", [16])
    if not small_moe:
        k.moe_wg = din("moe_w_gate", [2, 16, 2048, 1024])
        k.moe_wu = din("moe_w_up", [2, 16, 2048, 1024])
        k.moe_wd = din("moe_w_down", [2, 16, 1024, 2048])
    out = nc.dram_tensor("out", [2048, 2048], F32, kind="ExternalOutput").ap()

    k.uT_all = scr("uT", [4, 128, 4096], BF16)
    k.uT = [k.uT_all[i] for i in range(4)]
    qT_all = scr("qT", [12, 128, NEXT], BF16)
    k.qT = [qT_all[i] for i in range(12)]
    kT_all = scr("kT", [12, 128, 4096], BF16)
    k.kT = [kT_all[i] for i in range(12)]
    k.vS = scr("vS", [4096, 1536], BF16)
    k.yT_all = scr("yT", [16, 128, NEXT], BF16)
    k.yT = [k.yT_all[i] for i in range(16)]
    k.h1 = scr("h1", [NEXT, 2048], F32)
    k.h1T = scr("h1T", [16, 128, NEXT], BF16)
    k.h2 = scr("h2", [NEXT, 2048], F32)
    k.h2T = scr("h2T", [16, 128, NEXT], BF16)
    k.y1T_all = scr("y1T", [16, 128, 2048], BF16)
    k.y1T = [k.y1T_all[i] for i in range(16)]
    k.h3 = scr("h3", [2048, 2048], F32)
    k.h3T = scr("h3T", [16, 128, 2048], BF16)
    k.dummy_out = scr("dummy_o", [128, 2048], F32)

    with nc.allow_low_precision("bf16 matmul operands, fp32 accumulation"), nc.allow_non_contiguous_dma(reason="small parameter gathers"):
        setup_consts(k)
        phases = [
            ("inproj0", lambda: (setattr(k, "act_copy_ok", True), phase_inproj0(k), setattr(k, "act_copy_ok", False))),
            ("s5", lambda: phase_s5(k)),
            ("ln0", lambda: phase_outproj_ln(k, 0, NEXT, k.yT_all, k.ab_w_out, k.x_loc, HIST, k.h1, k.h1T)),
            ("moe0", lambda: phase_moe(k, 0, [[0, 1, 2, 3, 4], list(range(5, 11)), list(range(11, 17))], k.h1T, k.h1, k.h2, lambda tt: tt * 128, k.h2T)),
            ("pool", lambda: phase_pool(k)),
            ("ln1", lambda: phase_outproj_ln(k, 1, 2048, k.y1T_all, k.c_w_out, k.h2, 128, k.h3, k.h3T)),
            ("moe1", lambda: phase_moe(k, 1, [list(range(0, 8)), list(range(8, 16))], k.h3T, k.h3, out, lambda tt: tt * 128, None)),
        ]
        for name, fn in phases:
            if stop_after == 'consts':
                break
            fn()
            if stop_after == name:
                break
        k.c.flush()
    return nc, k


DEBUG_OUTS = set()


def make_in_maps(inputs):
    x = np.ascontiguousarray(inputs["x"], dtype=np.float32)
    sq = lambda a: np.ascontiguousarray(np.asarray(a, dtype=np.float32)[0])
    shared = {
        "ab_w_in": sq(inputs["ab_w_in"]), "ab_lambda_re": sq(inputs["ab_lambda_re"]), "ab_lambda_im": sq(inputs["ab_lambda_im"]),
        "ab_log_dt": sq(inputs["ab_log_dt"]), "ab_b_re": sq(inputs["ab_b_re"]), "ab_b_im": sq(inputs["ab_b_im"]),
        "ab_c_re": sq(inputs["ab_c_re"]), "ab_c_im": sq(inputs["ab_c_im"]), "ab_d": sq(inputs["ab_d"]),
        "ab_w_glu": sq(inputs["ab_w_glu"]), "ab_b_glu": sq(inputs["ab_b_glu"]), "ab_w_out": sq(inputs["ab_w_out"]),
        "c_w_in": sq(inputs["c_w_in"]), "c_w_group": sq(inputs["c_w_group"]), "c_scale": sq(inputs["c_scale"]),
        "c_w_out": sq(inputs["c_w_out"]),
        "ln_g": np.ascontiguousarray(inputs["ln_g"], dtype=np.float32), "ln_b": np.ascontiguousarray(inputs["ln_b"], dtype=np.float32),
        "router_w": np.ascontiguousarray(inputs["router_w"], dtype=np.float32), "router_b": np.ascontiguousarray(inputs["router_b"], dtype=np.float32),
        "moe_w_gate": np.ascontiguousarray(inputs["moe_w_gate"], dtype=np.float32),
        "moe_w_up": np.ascontiguousarray(inputs["moe_w_up"], dtype=np.float32),
        "moe_w_down": np.ascontiguousarray(inputs["moe_w_down"], dtype=np.float32),
    }
    windows = (2, 4, 8, 16)
    in_maps = []
    for core in range(8):
        b, p = core // 2, core % 2
        m = dict(shared)
        if p == 1:
            m["x_loc"] = x[b]
            kb = np.zeros((32, 128), np.float32)
            hm = np.ones((128, 1), np.float32)
            ic = np.stack([np.full(16, 1.0 / w, np.float32) for w in windows])
        else:
            xl = np.zeros((4096, 2048), np.float32)
            xl[2048:] = x[b, :2048]
            m["x_loc"] = xl
            kb = np.zeros((32, 128), np.float32)
            kb[:16] = -30000.0
            hm = np.zeros((128, 1), np.float32)
            ic = np.stack([1.0 / np.minimum(np.arange(16) + 1, w).astype(np.float32) for w in windows])
        m["kbias"] = kb
        m["halo_mask"] = hm
        m["invcnt"] = np.ascontiguousarray(np.broadcast_to(ic.reshape(1, 64), (128, 64)), dtype=np.float32)
        in_maps.append(m)
    return in_maps


def kernel(**inputs):
    nc, _ = build()
    in_maps = make_in_maps(inputs)
    res = run_bass_kernel_spmd(nc, in_maps, core_ids=list(range(8)))
    outp = np.empty((4, 4096, 2048), np.float32)
    for core in range(8):
        b, p = core // 2, core % 2
        outp[b, p * 2048:(p + 1) * 2048] = res.results[core]["out"]
    return outp
```

```python
from contextlib import ExitStack
import numpy as np
import concourse.bass as bass
import concourse.mybir as mybir
from concourse.bass_utils import run_bass_kernel_spmd

F32 = mybir.dt.float32
BF16 = mybir.dt.bfloat16
I32 = mybir.dt.int32
AF = mybir.ActivationFunctionType
ALU = mybir.AluOpType
AX = mybir.AxisListType

NDMA = 24
NOSELF = ("pe",)
MAXFLY = 6
ALPHA = 4.0 ** 0.25
LN_EPS = 1e-5
HIST = 1920
NEXT = 2176
LC = 256
TWO_PI = float(2 * np.pi)


class Buf:
    __slots__ = ("name", "lw", "rd")

    def __init__(self, name):
        self.name = name
        self.lw = None
        self.rd = []


class Op:
    __slots__ = ("eng", "fn", "reads", "writes", "dma", "deps", "has_dep", "ms", "sem_idx", "sem_val")

    def __init__(self, eng, fn, reads, writes, dma):
        self.eng = eng
        self.fn = fn
        self.reads = reads
        self.writes = writes
        self.dma = dma
        self.deps = ()
        self.has_dep = False
        self.ms = 0
        self.sem_idx = -1
        self.sem_val = 0


class Ctx:
    ENG = ("pe", "act", "dve", "pool", "sp")

    def __init__(self, nc):
        self.nc = nc
        self.e = {"pe": nc.tensor, "act": nc.scalar, "dve": nc.vector, "pool": nc.gpsimd, "sp": nc.sync}
        self.ops = []
        self.bufs = []
        self.dma_sems = [nc.alloc_semaphore(f"dmas{i}") for i in range(NDMA)]
        self.dma_counts = [0] * NDMA
        self.dma_rr = 0
        self.bar = nc.alloc_semaphore("bar")
        self.nbar = 0
        self.waited = {e: {} for e in self.ENG}
        self.n_inst = 0
        self.inflight = {}

    def buf(self, name):
        b = Buf(name)
        self.bufs.append(b)
        return b

    def op(self, eng, fn, reads=(), writes=()):
        self.ops.append(Op(eng, fn, tuple(reads), tuple(writes), False))

    def dma(self, eng, fn, reads=(), writes=()):
        self.ops.append(Op(eng, fn, tuple(reads), tuple(writes), True))

    def _wait(self, E, key, sem, val):
        w = self.waited[E]
        if w.get(key, 0) < val:
            self.e[E].wait_ge(sem, val)
            w[key] = val
            self.n_inst += 1

    def flush(self):
        nc = self.nc
        ops = self.ops
        if not ops:
            return
        for b in self.bufs:
            b.lw = None
            b.rd = []
        last_on = {}
        for i, op in enumerate(ops):
            deps = set()
            for b in op.reads:
                if b.lw is not None:
                    deps.add(b.lw)
            for b in op.writes:
                if b.lw is not None:
                    deps.add(b.lw)
                deps.update(b.rd)
            deps.discard(i)
            if op.eng in NOSELF and not op.dma:
                deps = {d for d in deps if ops[d].dma or ops[d].eng != op.eng}
            op.deps = sorted(deps)
            for d in op.deps:
                ops[d].has_dep = True
            for b in op.reads:
                if not op.dma:
                    b.rd = [r for r in b.rd if ops[r].dma or ops[r].eng != op.eng]
                b.rd.append(i)
            for b in op.writes:
                b.lw = i
                b.rd = []
            if not op.dma:
                last_on[op.eng] = i
        for e, i in last_on.items():
            ops[i].has_dep = True
        sem = {e: nc.alloc_semaphore(f"ph{self.nbar}_{e}") for e in self.ENG if e != "sp"}
        cnt = {e: 0 for e in self.ENG}
        dma_used = set()
        for op in ops:
            E = op.eng
            for d in op.deps:
                D = ops[d]
                if D.dma:
                    self._wait(E, ("d", D.sem_idx), self.dma_sems[D.sem_idx], D.sem_val)
                else:
                    self._wait(E, ("e", D.eng), sem[D.eng], D.ms)
            if op.dma:
                fl = self.inflight.setdefault(E, [])
                if len(fl) >= MAXFLY:
                    pk, pv = fl[len(fl) - MAXFLY]
                    self._wait(E, ("d", pk), self.dma_sems[pk], pv)
                k = self.dma_rr
                self.dma_rr = (self.dma_rr + 1) % NDMA
                if self.dma_counts[k] > 0:
                    self._wait(E, ("d", k), self.dma_sems[k], self.dma_counts[k])
                self.dma_counts[k] += 16
                op.sem_idx = k
                op.sem_val = self.dma_counts[k]
                dma_used.add(k)
                fl.append((k, op.sem_val))
                ins = op.fn(self.e[E])
                ins.then_inc(self.dma_sems[k], 16)
            else:
                ins = op.fn(self.e[E])
                if op.has_dep:
                    cnt[E] += 1
                    op.ms = cnt[E]
                    ins.then_inc(sem[E], 1)
            self.n_inst += 1
        for k in sorted(dma_used):
            self._wait("sp", ("d", k), self.dma_sems[k], self.dma_counts[k])
        for e in self.ENG:
            if e != "sp" and cnt[e] > 0:
                self._wait("sp", ("e", e), sem[e], cnt[e])
        self.nbar += 1
        self.e["sp"].sem_inc(self.bar, 1)
        for e in self.ENG:
            if e != "sp":
                self.e[e].wait_ge(self.bar, self.nbar)
        self.ops = []
        self.waited = {e: {k: v for k, v in self.waited[e].items() if k[0] == "d"} for e in self.ENG}


class TB:
    __slots__ = ("t", "b")

    def __init__(self, t, b):
        self.t = t
        self.b = b


class K:
    def __init__(self, nc, debug):
        self.nc = nc
        self.c = Ctx(nc)
        self.debug = debug
        self.ev = 0
        self.uid = 0
        self.act_copy_ok = False

    def tile(self, st, name, shape, dt):
        self.uid += 1
        t = st.enter_context(self.nc.sbuf_tensor(f"{name}_{self.uid}", shape, dt))
        return TB(t, self.c.buf(name))

    def gtile(self, name, shape, dt):
        t = self.nc.alloc_sbuf_tensor(name, shape, dt)
        return TB(t, self.c.buf(name))

    def evac(self, out, in_, reads, writes, eng=None):
        if eng is None:
            eng = "act" if (self.ev % 2 == 0 and self.act_copy_ok) else "dve"
            self.ev += 1
        if eng == "act":
            self.c.op("act", lambda e: e.activation(out=out, in_=in_, func=AF.Copy), reads=reads, writes=writes)
        else:
            self.c.op(eng, lambda e: e.tensor_copy(out=out, in_=in_), reads=reads, writes=writes)


def tok_tiles(n, w=512):
    out = []
    t = 0
    while t < n:
        m = min(w, n - t)
        out.append((t, m))
        t += m
    return out


def setup_consts(k):
    nc, c = k.nc, k.c
    k.ident = k.gtile("ident", [128, 128], F32)
    k.identb = k.gtile("identb", [128, 128], BF16)
    k.triu = k.gtile("triu", [128, 128], BF16)
    k.strl = k.gtile("strl", [128, 128], BF16)
    k.cmask = k.gtile("cmask", [128, 4, 512], BF16)
    k.kb_sb = k.gtile("kb_sb", [128, 32], F32)
    k.eps = k.gtile("eps", [128, 1], F32)
    k.gates = [k.gtile(f"gates{i}", [128, 17, 16], F32) for i in range(2)]
    k.ps = [TB(nc.alloc_psum_tensor(f"ps{i}", [128, 512], F32), c.buf(f"ps{i}")) for i in range(8)]
    with ExitStack() as st:
        tmp = k.tile(st, "ctmp", [128, 512], F32)
        c.op("pool", lambda e: e.memset(k.ident.t[:], 0.0), writes=[k.ident.b])
        c.op("pool", lambda e: e.memset(k.eps.t[:], LN_EPS), writes=[k.eps.b])
        c.op("pool", lambda e: e.affine_select(out=k.ident.t[:], in_=k.ident.t[:], pattern=[[-1, 128]], compare_op=ALU.not_equal, fill=1.0, base=0, channel_multiplier=1), reads=[k.ident.b], writes=[k.ident.b])
        c.op("dve", lambda e: e.tensor_copy(out=k.identb.t[:], in_=k.ident.t[:]), reads=[k.ident.b], writes=[k.identb.b])
        c.op("pool", lambda e: e.memset(tmp.t[:, 0:128], 1.0), writes=[tmp.b])
        c.op("pool", lambda e: e.affine_select(out=tmp.t[:, 0:128], in_=tmp.t[:, 0:128], pattern=[[-1, 128]], compare_op=ALU.is_ge, fill=0.0, base=0, channel_multiplier=1), reads=[tmp.b], writes=[tmp.b])
        c.op("dve", lambda e: e.tensor_copy(out=k.triu.t[:], in_=tmp.t[:, 0:128]), reads=[tmp.b], writes=[k.triu.b])
        c.op("dve", lambda e: e.tensor_scalar(out=k.strl.t[:], in0=tmp.t[:, 0:128], scalar1=-1.0, scalar2=1.0, op0=ALU.mult, op1=ALU.add), reads=[tmp.b], writes=[k.strl.b])
        for m in range(4):
            c.op("pool", lambda e: e.memset(tmp.t[:], 1.0), reads=[tmp.b], writes=[tmp.b])
            c.op("pool", lambda e, m=m: e.affine_select(out=tmp.t[:], in_=tmp.t[:], pattern=[[1, 512]], compare_op=ALU.is_gt, fill=0.0, base=-128 * m, channel_multiplier=-1), reads=[tmp.b], writes=[tmp.b])
            c.op("dve", lambda e, m=m: e.tensor_copy(out=k.cmask.t[:, m, :], in_=tmp.t[:]), reads=[tmp.b], writes=[k.cmask.b])
        c.dma("sp", lambda e: e.dma_start(out=k.kb_sb.t[:], in_=k.kbias.rearrange("kb p -> p kb"), allow_slow_non_contiguous=True), writes=[k.kb_sb.b])
        c.flush()


def phase_inproj0(k):
    nc, c = k.nc, k.c
    with ExitStack() as st:
        xT = k.tile(st, "xT", [128, 16, NEXT], BF16)
        xs = [k.tile(st, f"xs{i}", [128, 2048], F32) for i in range(2)]
        Wb = [k.tile(st, f"Wb{i}", [128, 16, 512], BF16) for i in range(2)]
        stg = [k.tile(st, f"stg{i}", [128, NEXT], BF16) for i in range(2)]
        vst = [k.tile(st, f"vst{i}", [128, 512], BF16) for i in range(3)]
        wi = si = vi = pi = 0
        for (tok0, ntok, blocks) in ((0, HIST, [0, 4, 5, 6, 7, 8, 9]), (HIST, NEXT, list(range(10)))):
            for tt in range(ntok // 128):
                x_ = xs[tt % 2]
                r0 = tok0 + tt * 128
                c.dma("sp", lambda e, x_=x_, r0=r0: e.dma_start(out=x_.t[:], in_=k.x_loc[r0:r0 + 128, :]), writes=[x_.b])
                for g in range(4):
                    pb = k.ps[g]
                    for j in range(4):
                        kc = 4 * g + j
                        c.op("pe", lambda e, pb=pb, j=j, kc=kc, x_=x_: e.transpose(pb.t[:, j * 128:(j + 1) * 128], x_.t[:, kc * 128:(kc + 1) * 128], k.ident.t[:]), reads=[x_.b, k.ident.b], writes=[pb.b])
                    k.evac(xT.t[:, 4 * g:4 * g + 4, tt * 128:(tt + 1) * 128], pb.t[:].rearrange("p (a b) -> p a b", a=4), reads=[pb.b], writes=[xT.b])
            for blk in blocks:
                W_ = Wb[wi % 2]
                wi += 1
                c.dma("pool", lambda e, W_=W_, blk=blk: e.dma_start(out=W_.t[:], in_=k.w_in.rearrange("(kc p) n -> p kc n", p=128)[:, :, blk * 512:(blk + 1) * 512]), writes=[W_.b])
                if blk < 7:
                    for sub in range(4):
                        s_ = stg[si % 2]
                        si += 1
                        for (t0, n) in tok_tiles(ntok):
                            pb = k.ps[4 + pi % 4]
                            pi += 1
                            for kc in range(16):
                                c.op("pe", lambda e, pb=pb, kc=kc, W_=W_, sub=sub, t0=t0, n=n: e.matmul(pb.t[:, 0:n], lhsT=W_.t[:, kc, sub * 128:(sub + 1) * 128], rhs=xT.t[:, kc, t0:t0 + n], start=(kc == 0), stop=(kc == 15)), reads=[W_.b, xT.b], writes=[pb.b])
                            k.evac(s_.t[:, t0:t0 + n], pb.t[:, 0:n], reads=[pb.b], writes=[s_.b])
                        if blk == 0:
                            dst = k.uT[sub][:, tok0:tok0 + ntok]
                        elif blk < 4:
                            dst = k.qT[(blk - 1) * 4 + sub][:, 0:ntok]
                        else:
                            dst = k.kT[(blk - 4) * 4 + sub][:, tok0:tok0 + ntok]
                        c.dma("sp", lambda e, dst=dst, s_=s_, ntok=ntok: e.dma_start(out=dst, in_=s_.t[:, 0:ntok]), reads=[s_.b])
                else:
                    for tt in range(ntok // 128):
                        pb = k.ps[4 + pi % 4]
                        pi += 1
                        v_ = vst[vi % 3]
                        vi += 1
                        for kc in range(16):
                            c.op("pe", lambda e, pb=pb, kc=kc, W_=W_, tt=tt: e.matmul(pb.t[:], lhsT=xT.t[:, kc, tt * 128:(tt + 1) * 128], rhs=W_.t[:, kc, :], start=(kc == 0), stop=(kc == 15)), reads=[W_.b, xT.b], writes=[pb.b])
                        k.evac(v_.t[:], pb.t[:], reads=[pb.b], writes=[v_.b])
                        r0 = tok0 + tt * 128
                        c.dma("sp", lambda e, v_=v_, r0=r0, blk=blk: e.dma_start(out=k.vS[r0:r0 + 128, (blk - 7) * 512:(blk - 6) * 512], in_=v_.t[:]), reads=[v_.b])
        c.flush()


def range_reduce_sin(k, out, ang, tmp_i, tmp_f, bufs_r, bufs_w, eng="dve"):
    c = k.c
    c.op(eng, lambda e: e.tensor_scalar(out=tmp_i, in0=ang, scalar1=1.0 / TWO_PI, scalar2=None, op0=ALU.mult), reads=bufs_r, writes=bufs_w)
    c.op(eng, lambda e: e.tensor_copy(out=tmp_f, in_=tmp_i), reads=bufs_w, writes=bufs_w)
    c.op(eng, lambda e: e.scalar_tensor_tensor(out=tmp_f, in0=tmp_f, scalar=-TWO_PI, in1=ang, op0=ALU.mult, op1=ALU.add), reads=list(bufs_r) + list(bufs_w), writes=bufs_w)
    c.op("act", lambda e: e.activation(out=out, in_=tmp_f, func=AF.Sin), reads=bufs_w, writes=bufs_w)


def phase_s5(k):
    nc, c = k.nc, k.c
    NCH = 4096 // LC
    first_ext_chunk = HIST // LC
    with ExitStack() as st:
        uT = k.tile(st, "s5uT", [128, 4, 4096], BF16)
        cosT = k.tile(st, "cosT", [128, 16, LC + 1], F32)
        sinT = k.tile(st, "sinT", [128, 16, LC + 1], F32)
        BT = k.tile(st, "BT", [128, 32, 128], BF16)
        CT = k.tile(st, "CT", [128, 32, 128], BF16)
        par = k.tile(st, "s5par", [128, 12, 16], F32)
        zin = [k.tile(st, f"zin{i}", [128, 2], F32) for i in range(16)]
        dcol = k.tile(st, "dcol", [128, 4], F32)
        bglu = k.tile(st, "bglu", [128, 4], F32)
        wglu = k.tile(st, "wglu", [128, 4, 512], BF16)
        iot = k.tile(st, "iot", [128, LC + 1], F32)
        LR, LI, DT, TH, MAG, LBR, LBI, CR, CI, T0, T1, T2 = range(12)
        c.dma("sp", lambda e: e.dma_start(out=par.t[:, LR, :], in_=k.lam_re.rearrange("(gp gl) n -> (gl n) gp", gl=2), allow_slow_non_contiguous=True), writes=[par.b])
        c.dma("sp", lambda e: e.dma_start(out=par.t[:, LI, :], in_=k.lam_im.rearrange("(gp gl) n -> (gl n) gp", gl=2), allow_slow_non_contiguous=True), writes=[par.b])
        for gl in range(2):
            c.dma("sp", lambda e, gl=gl: e.dma_start(out=par.t[gl * 64:(gl + 1) * 64, DT, :], in_=k.log_dt.rearrange("(gp gl) -> gp gl", gl=2)[:, gl].partition_broadcast(64)), writes=[par.b])
        c.dma("sp", lambda e: e.dma_start(out=dcol.t[:], in_=k.ab_d.rearrange("(ct p) -> p ct", p=128), allow_slow_non_contiguous=True), writes=[dcol.b])
        c.dma("sp", lambda e: e.dma_start(out=bglu.t[:], in_=k.b_glu.rearrange("(ct p) -> p ct", p=128), allow_slow_non_contiguous=True), writes=[bglu.b])
        c.dma("pool", lambda e: e.dma_start(out=wglu.t[:], in_=k.w_glu.rearrange("(kc p) n -> p kc n", p=128)), writes=[wglu.b])
        c.dma("sp", lambda e: e.dma_start(out=uT.t[:], in_=k.uT_all.rearrange("ct p t -> p ct t")), writes=[uT.b])
        ioti = k.tile(st, "ioti", [128, LC + 1], I32)
        c.op("pool", lambda e: e.iota(ioti.t[:], pattern=[[1, LC + 1]], base=0, channel_multiplier=0), writes=[ioti.b])
        c.op("dve", lambda e: e.tensor_copy(out=iot.t[:], in_=ioti.t[:]), reads=[ioti.b], writes=[iot.b])
        for z_ in zin:
            c.op("pool", lambda e, z_=z_: e.memset(z_.t[:], 0.0), writes=[z_.b])
        P = lambda i: par.t[:, i, :]
        c.op("act", lambda e: e.activation(out=P(DT), in_=P(DT), func=AF.Exp), reads=[par.b], writes=[par.b])
        c.op("dve", lambda e: e.tensor_tensor(out=P(TH), in0=P(LI), in1=P(DT), op=ALU.mult), reads=[par.b], writes=[par.b])
        c.op("dve", lambda e: e.tensor_tensor(out=P(T0), in0=P(LR), in1=P(DT), op=ALU.mult), reads=[par.b], writes=[par.b])
        c.op("act", lambda e: e.activation(out=P(MAG), in_=P(T0), func=AF.Exp), reads=[par.b], writes=[par.b])
        import os
        s5stop = int(os.environ.get("S5STOP", "9"))
        if s5stop == 1:
            c.flush()
            return
        with ExitStack() as st2:
            ang = k.tile(st2, "ang", [128, LC + 1], F32)
            ti = k.tile(st2, "ti", [128, LC + 1], I32)
            tf = k.tile(st2, "tf", [128, LC + 1], F32)
            for gp in range(16):
                c.op("dve", lambda e, gp=gp: e.tensor_scalar(out=ang.t[:], in0=iot.t[:], scalar1=par.t[:, TH, gp:gp + 1], scalar2=None, op0=ALU.mult), reads=[iot.b, par.b], writes=[ang.b])
                range_reduce_sin(k, sinT.t[:, gp, :], ang.t[:], ti.t[:], tf.t[:], [ang.b], [ti.b, tf.b, sinT.b])
                c.op("dve", lambda e: e.tensor_scalar(out=ang.t[:], in0=ang.t[:], scalar1=float(np.pi / 2), scalar2=None, op0=ALU.add), reads=[ang.b, ti.b, tf.b], writes=[ang.b])
                range_reduce_sin(k, cosT.t[:, gp, :], ang.t[:], ti.t[:], tf.t[:], [ang.b], [ti.b, tf.b, cosT.b])
            if s5stop == 2:
                c.flush()
                return
            c.op("dve", lambda e: e.tensor_tensor(out=P(LBR), in0=P(MAG), in1=cosT.t[:, :, 1], op=ALU.mult), reads=[par.b, cosT.b], writes=[par.b])
            c.op("dve", lambda e: e.tensor_tensor(out=P(LBI), in0=P(MAG), in1=sinT.t[:, :, 1], op=ALU.mult), reads=[par.b, sinT.b], writes=[par.b])
            c.op("dve", lambda e: e.tensor_tensor(out=P(T0), in0=P(LR), in1=P(LR), op=ALU.mult), reads=[par.b], writes=[par.b])
            c.op("dve", lambda e: e.tensor_tensor(out=P(T1), in0=P(LI), in1=P(LI), op=ALU.mult), reads=[par.b], writes=[par.b])
            c.op("dve", lambda e: e.tensor_tensor(out=P(T0), in0=P(T0), in1=P(T1), op=ALU.add), reads=[par.b], writes=[par.b])
            c.op("dve", lambda e: e.reciprocal(out=P(T2), in_=P(T0)), reads=[par.b], writes=[par.b])
            c.op("dve", lambda e: e.tensor_scalar(out=P(LBR), in0=P(LBR), scalar1=-1.0, scalar2=None, op0=ALU.add), reads=[par.b], writes=[par.b])
            c.op("dve", lambda e: e.tensor_tensor(out=P(T0), in0=P(LBR), in1=P(LR), op=ALU.mult), reads=[par.b], writes=[par.b])
            c.op("dve", lambda e: e.tensor_tensor(out=P(T1), in0=P(LBI), in1=P(LI), op=ALU.mult), reads=[par.b], writes=[par.b])
            c.op("dve", lambda e: e.tensor_tensor(out=P(T0), in0=P(T0), in1=P(T1), op=ALU.add), reads=[par.b], writes=[par.b])
            c.op("dve", lambda e: e.tensor_tensor(out=P(CR), in0=P(T0), in1=P(T2), op=ALU.mult), reads=[par.b], writes=[par.b])
            c.op("dve", lambda e: e.tensor_tensor(out=P(T0), in0=P(LBI), in1=P(LR), op=ALU.mult), reads=[par.b], writes=[par.b])
            c.op("dve", lambda e: e.tensor_tensor(out=P(T1), in0=P(LBR), in1=P(LI), op=ALU.mult), reads=[par.b], writes=[par.b])
            c.op("dve", lambda e: e.tensor_tensor(out=P(T0), in0=P(T0), in1=P(T1), op=ALU.subtract), reads=[par.b], writes=[par.b])
            c.op("dve", lambda e: e.tensor_tensor(out=P(CI), in0=P(T0), in1=P(T2), op=ALU.mult), reads=[par.b], writes=[par.b])
            if s5stop == 3:
                c.flush()
                return
            zr = [k.tile(st2, f"zr{i}", [128, 128], F32) for i in range(2)]
            zi = [k.tile(st2, f"zi{i}", [128, 128], F32) for i in range(2)]
            yr = [k.tile(st2, f"yr{i}", [128, 128], F32) for i in range(2)]
            yi = [k.tile(st2, f"yi{i}", [128, 128], F32) for i in range(2)]
            bb = [k.tile(st2, f"bb{i}", [128, 2, 128], F32) for i in range(2)]
            for gp in range(16):
                i = gp % 2
                Zr, Zi, Yr, Yi, Bb = zr[i], zi[i], yr[i], yi[i], bb[i]
                for T_ in (Zr, Zi, Yr, Yi):
                    c.op("pool", lambda e, T_=T_: e.memset(T_.t[:], 0.0), writes=[T_.b])
                for gl in range(2):
                    g = 2 * gp + gl
                    c0 = (g % 8) * 16
                    c.dma("sp", lambda e, Zr=Zr, g=g, gl=gl, c0=c0: e.dma_start(out=Zr.t[gl * 64:(gl + 1) * 64, c0:c0 + 16], in_=k.b_re[g]), reads=[Zr.b], writes=[Zr.b])
                    c.dma("sp", lambda e, Zi=Zi, g=g, gl=gl, c0=c0: e.dma_start(out=Zi.t[gl * 64:(gl + 1) * 64, c0:c0 + 16], in_=k.b_im[g]), reads=[Zi.b], writes=[Zi.b])
                    c.dma("sp", lambda e, Yr=Yr, g=g, gl=gl, c0=c0: e.dma_start(out=Yr.t[c0:c0 + 16, gl * 64:(gl + 1) * 64], in_=k.c_re[g]), reads=[Yr.b], writes=[Yr.b])
                    c.dma("sp", lambda e, Yi=Yi, g=g, gl=gl, c0=c0: e.dma_start(out=Yi.t[c0:c0 + 16, gl * 64:(gl + 1) * 64], in_=k.c_im[g]), reads=[Yi.b], writes=[Yi.b])
                cr = par.t[:, CR, gp:gp + 1]
                ci = par.t[:, CI, gp:gp + 1]
                c.op("dve", lambda e, Bb=Bb, Zi=Zi, ci=ci: e.tensor_scalar(out=Bb.t[:, 0, :], in0=Zi.t[:], scalar1=ci, scalar2=None, op0=ALU.mult), reads=[Zi.b, par.b], writes=[Bb.b])
                c.op("dve", lambda e, Bb=Bb, Zr=Zr, cr=cr: e.scalar_tensor_tensor(out=Bb.t[:, 0, :], in0=Zr.t[:], scalar=cr, in1=Bb.t[:, 0, :], op0=ALU.mult, op1=ALU.subtract), reads=[Zr.b, par.b, Bb.b], writes=[Bb.b])
                c.op("dve", lambda e, Bb=Bb, Zr=Zr, ci=ci: e.tensor_scalar(out=Bb.t[:, 1, :], in0=Zr.t[:], scalar1=ci, scalar2=None, op0=ALU.mult), reads=[Zr.b, par.b], writes=[Bb.b])
                c.op("dve", lambda e, Bb=Bb, Zi=Zi, cr=cr: e.scalar_tensor_tensor(out=Bb.t[:, 1, :], in0=Zi.t[:], scalar=cr, in1=Bb.t[:, 1, :], op0=ALU.mult, op1=ALU.add), reads=[Zi.b, par.b, Bb.b], writes=[Bb.b])
                if s5stop == 5:
                    continue
                pb = k.ps[gp % 2]
                for part in range(2):
                    c.op("pe", lambda e, pb=pb, Bb=Bb, part=part: e.transpose(pb.t[:, part * 128:(part + 1) * 128], Bb.t[:, part, :], k.ident.t[:]), reads=[Bb.b, k.ident.b], writes=[pb.b])
                c.op("pe", lambda e, pb=pb, Yr=Yr: e.transpose(pb.t[:, 256:384], Yr.t[:], k.ident.t[:]), reads=[Yr.b, k.ident.b], writes=[pb.b])
                c.op("pe", lambda e, pb=pb, Yi=Yi: e.transpose(pb.t[:, 384:512], Yi.t[:], k.ident.t[:]), reads=[Yi.b, k.ident.b], writes=[pb.b])
                if s5stop == 6:
                    continue
                c.op("dve", lambda e, pb=pb, gp=gp: e.tensor_copy(out=BT.t[:, 2 * gp:2 * gp + 2, :], in_=pb.t[:, 0:256].rearrange("p (a b) -> p a b", a=2)), reads=[pb.b], writes=[BT.b])
                if s5stop == 7:
                    continue
                c.op("dve", lambda e, pb=pb, gp=gp: e.tensor_copy(out=CT.t[:, 2 * gp, :], in_=pb.t[:, 256:384]), reads=[pb.b], writes=[CT.b])
                c.op("dve", lambda e, pb=pb, gp=gp: e.tensor_scalar(out=CT.t[:, 2 * gp + 1, :], in0=pb.t[:, 384:512], scalar1=-1.0, scalar2=None, op0=ALU.mult), reads=[pb.b], writes=[CT.b])
            c.flush()
        if s5stop in (4, 5, 6, 7):
            return
        with ExitStack() as st3:
            NW = 2
            cre = [k.tile(st3, f"cre{i}", [128, LC], F32) for i in range(NW)]
            cim = [k.tile(st3, f"cim{i}", [128, LC], F32) for i in range(NW)]
            t1 = [k.tile(st3, f"t1_{i}", [128, LC], F32) for i in range(NW)]
            t2 = [k.tile(st3, f"t2_{i}", [128, LC], F32) for i in range(NW)]
            zre = [k.tile(st3, f"zre{i}", [128, LC], F32) for i in range(NW)]
            zim = [k.tile(st3, f"zim{i}", [128, LC], F32) for i in range(NW)]
            sre = [k.tile(st3, f"sre{i}", [128, LC], BF16) for i in range(8)]
            sim = [k.tile(st3, f"sim{i}", [128, LC], BF16) for i in range(8)]
            ctmp = [k.tile(st3, f"cz{i}", [128, 2], F32) for i in range(NW)]
            vv = [k.tile(st3, f"vv{i}", [128, LC], F32) for i in range(4)]
            py7 = TB(k.ps[7].t, c.buf("ps7a"))
            pg7 = TB(k.ps[7].t, c.buf("ps7b"))

            def s5_gen():
                wk = 0
                for ch in range(NCH):
                    t0 = ch * LC
                    need_y = ch >= first_ext_chunk
                    for ct in range(4):
                        py = py7
                        for q in range(4):
                            gp = 4 * ct + q
                            yield
                            w = wk % NW
                            wk += 1
                            eA = "dve"
                            pb = k.ps[6]
                            for part in range(2):
                                c.op("pe", lambda e, pb=pb, gp=gp, part=part, ct=ct, t0=t0: e.matmul(pb.t[:, part * LC:(part + 1) * LC], lhsT=BT.t[:, 2 * gp + part, :], rhs=uT.t[:, ct, t0:t0 + LC], start=True, stop=True), reads=[BT.b, uT.b], writes=[pb.b])
                            bre = pb.t[:, 0:LC]
                            bim = pb.t[:, LC:2 * LC]
                            cs = cosT.t[:, gp, 0:LC]
                            sn = sinT.t[:, gp, 0:LC]
                            c.op("dve", lambda e, w=w, bre=bre, cs=cs: e.tensor_tensor(out=t1[w].t[:], in0=bre, in1=cs, op=ALU.mult), reads=[pb.b, cosT.b], writes=[t1[w].b])
                            c.op("dve", lambda e, w=w, bim=bim, sn=sn: e.tensor_tensor(out=t2[w].t[:], in0=bim, in1=sn, op=ALU.mult), reads=[pb.b, sinT.b], writes=[t2[w].b])
                            c.op(eA, lambda e, w=w: e.tensor_tensor(out=cre[w].t[:], in0=t1[w].t[:], in1=t2[w].t[:], op=ALU.add), reads=[t1[w].b, t2[w].b], writes=[cre[w].b])
                            c.op("dve", lambda e, w=w, bim=bim, cs=cs: e.tensor_tensor(out=t1[w].t[:], in0=bim, in1=cs, op=ALU.mult), reads=[pb.b, cosT.b, cre[w].b], writes=[t1[w].b])
                            c.op("dve", lambda e, w=w, bre=bre, sn=sn: e.tensor_tensor(out=t2[w].t[:], in0=bre, in1=sn, op=ALU.mult), reads=[pb.b, sinT.b, cre[w].b], writes=[t2[w].b])
                            c.op(eA, lambda e, w=w: e.tensor_tensor(out=cim[w].t[:], in0=t1[w].t[:], in1=t2[w].t[:], op=ALU.subtract), reads=[t1[w].b, t2[w].b], writes=[cim[w].b])
                            c.op("dve", lambda e, w=w, gp=gp: e.tensor_tensor_scan(out=zre[w].t[:], data0=par.t[:, MAG, gp:gp + 1].broadcast_to([128, LC]), data1=cre[w].t[:], initial=zin[gp].t[:, 0:1], op0=ALU.mult, op1=ALU.add), reads=[par.b, cre[w].b, zin[gp].b], writes=[zre[w].b])
                            c.op("dve", lambda e, w=w, gp=gp: e.tensor_tensor_scan(out=zim[w].t[:], data0=par.t[:, MAG, gp:gp + 1].broadcast_to([128, LC]), data1=cim[w].t[:], initial=zin[gp].t[:, 1:2], op0=ALU.mult, op1=ALU.add), reads=[par.b, cim[w].b, zin[gp].b], writes=[zim[w].b])
                            cL = cosT.t[:, gp, LC:LC + 1]
                            sL = sinT.t[:, gp, LC:LC + 1]
                            c.op(eA, lambda e, w=w, sL=sL: e.tensor_scalar(out=ctmp[w].t[:, 0:1], in0=zim[w].t[:, LC - 1:LC], scalar1=sL, scalar2=None, op0=ALU.mult), reads=[zim[w].b, sinT.b], writes=[ctmp[w].b])
                            c.op(eA, lambda e, w=w, sL=sL: e.tensor_scalar(out=ctmp[w].t[:, 1:2], in0=zre[w].t[:, LC - 1:LC], scalar1=sL, scalar2=None, op0=ALU.mult), reads=[zre[w].b, sinT.b], writes=[ctmp[w].b])
                            c.op("dve", lambda e, w=w, cL=cL, gp=gp: e.scalar_tensor_tensor(out=zin[gp].t[:, 0:1], in0=zre[w].t[:, LC - 1:LC], scalar=cL, in1=ctmp[w].t[:, 0:1], op0=ALU.mult, op1=ALU.subtract), reads=[zre[w].b, cosT.b, ctmp[w].b], writes=[zin[gp].b])
                            c.op("dve", lambda e, w=w, cL=cL, gp=gp: e.scalar_tensor_tensor(out=zin[gp].t[:, 1:2], in0=zim[w].t[:, LC - 1:LC], scalar=cL, in1=ctmp[w].t[:, 1:2], op0=ALU.mult, op1=ALU.add), reads=[zim[w].b, cosT.b, ctmp[w].b], writes=[zin[gp].b])
                            if not need_y:
                                continue
                            s8 = (4 * ct + q) % 8
                            eB = "dve"
                            c.op(eB, lambda e, w=w, cs=cs: e.tensor_tensor(out=t1[w].t[:], in0=zre[w].t[:], in1=cs, op=ALU.mult), reads=[zre[w].b, cosT.b, cim[w].b], writes=[t1[w].b])
                            c.op(eB, lambda e, w=w, sn=sn: e.tensor_tensor(out=t2[w].t[:], in0=zim[w].t[:], in1=sn, op=ALU.mult), reads=[zim[w].b, sinT.b, cim[w].b], writes=[t2[w].b])
                            c.op(eB, lambda e, w=w, s8=s8: e.tensor_tensor(out=sre[s8].t[:], in0=t1[w].t[:], in1=t2[w].t[:], op=ALU.subtract), reads=[t1[w].b, t2[w].b], writes=[sre[s8].b])
                            c.op(eB, lambda e, w=w, sn=sn: e.tensor_tensor(out=t1[w].t[:], in0=zre[w].t[:], in1=sn, op=ALU.mult), reads=[zre[w].b, sinT.b, sre[s8].b], writes=[t1[w].b])
                            c.op(eB, lambda e, w=w, cs=cs: e.tensor_tensor(out=t2[w].t[:], in0=zim[w].t[:], in1=cs, op=ALU.mult), reads=[zim[w].b, cosT.b, sre[s8].b], writes=[t2[w].b])
                            c.op(eB, lambda e, w=w, s8=s8: e.tensor_tensor(out=sim[s8].t[:], in0=t1[w].t[:], in1=t2[w].t[:], op=ALU.add), reads=[t1[w].b, t2[w].b], writes=[sim[s8].b])
                            c.op("pe", lambda e, py=py, gp=gp, s8=s8, q=q: e.matmul(py.t[:, 0:LC], lhsT=CT.t[:, 2 * gp, :], rhs=sre[s8].t[:], start=(q == 0), stop=False), reads=[CT.b, sre[s8].b], writes=[py.b])
                            c.op("pe", lambda e, py=py, gp=gp, s8=s8, q=q: e.matmul(py.t[:, 0:LC], lhsT=CT.t[:, 2 * gp + 1, :], rhs=sim[s8].t[:], start=False, stop=(q == 3)), reads=[CT.b, sim[s8].b], writes=[py.b])
                        if need_y:
                            V_ = vv[(ch * 4 + ct) % len(vv)]
                            lo = max(t0, HIST)
                            c.op("dve", lambda e, V_=V_, py=py, ct=ct, t0=t0: e.scalar_tensor_tensor(out=V_.t[:], in0=uT.t[:, ct, t0:t0 + LC], scalar=dcol.t[:, ct:ct + 1], in1=py.t[:, 0:LC], op0=ALU.mult, op1=ALU.add), reads=[uT.b, dcol.b, py.b], writes=[V_.b])
                            c.dma("sp", lambda e, V_=V_, ct=ct, lo=lo, t0=t0: e.dma_start(out=k.vT[ct][:, lo - HIST:t0 + LC - HIST], in_=V_.t[:, lo - t0:LC]), reads=[V_.b])

            scale = 1.0 / float(np.sqrt(128.0))
            TA = [attn_alloc(k, st3, "a"), attn_alloc(k, st3, "b")]
            gens = [attn_stream(k, list(range(0, 6)), [k.ps[0], k.ps[1], k.ps[2]], TA[0], scale),
                    attn_stream(k, list(range(6, 12)), [k.ps[3], k.ps[4], k.ps[5]], TA[1], scale),
                    s5_gen()]
            alive = [True, True, True]
            step = 0
            while any(alive):
                for gi, g in enumerate(gens):
                    if not alive[gi]:
                        continue
                    if gi == 2 and step % 3 != 0 and (alive[0] or alive[1]):
                        continue
                    try:
                        next(g)
                    except StopIteration:
                        alive[gi] = False
                step += 1
            c.flush()
        with ExitStack() as st4:
            vts = k.tile(st4, "vts", [128, 4, NEXT], F32)
            gtb = k.tile(st4, "gtb", [128, 4, NEXT], BF16)
            sgs = [k.tile(st4, f"sgs{i}", [128, 512], F32) for i in range(2)]
            yos = [k.tile(st4, f"yos{i}", [128, NEXT], BF16) for i in range(2)]
            c.dma("sp", lambda e: e.dma_start(out=vts.t[:], in_=k.vT.rearrange("ct p t -> p ct t")), writes=[vts.b])
            for ct in range(4):
                c.op("act", lambda e, ct=ct: e.activation(out=gtb.t[:, ct, :], in_=vts.t[:, ct, :], func=AF.Gelu), reads=[vts.b], writes=[gtb.b])
            pj = 0
            for ot in range(4):
                Y_ = yos[ot % 2]
                for (t0, n) in tok_tiles(NEXT):
                    pg = k.ps[pj % 4]
                    S_ = sgs[pj % 2]
                    pj += 1
                    for kc in range(4):
                        c.op("pe", lambda e, pg=pg, kc=kc, ot=ot, t0=t0, n=n: e.matmul(pg.t[:, 0:n], lhsT=wglu.t[:, kc, ot * 128:(ot + 1) * 128], rhs=gtb.t[:, kc, t0:t0 + n], start=(kc == 0), stop=(kc == 3)), reads=[wglu.b, gtb.b], writes=[pg.b])
                    c.op("act", lambda e, S_=S_, pg=pg, ot=ot, n=n: e.activation(out=S_.t[:, 0:n], in_=pg.t[:, 0:n], func=AF.Sigmoid, bias=bglu.t[:, ot:ot + 1]), reads=[pg.b, bglu.b], writes=[S_.b])
                    c.op("dve", lambda e, S_=S_, Y_=Y_, ot=ot, t0=t0, n=n: e.tensor_tensor(out=Y_.t[:, t0:t0 + n], in0=S_.t[:, 0:n], in1=gtb.t[:, ot, t0:t0 + n], op=ALU.mult), reads=[S_.b, gtb.b], writes=[Y_.b])
                c.dma("sp", lambda e, Y_=Y_, ot=ot: e.dma_start(out=k.yT[ot], in_=Y_.t[:]), reads=[Y_.b])
            c.flush()


def attn_alloc(k, st, sfx):
    T = {}
    T["kT"] = k.tile(st, f"kTs{sfx}", [128, 4096], BF16)
    T["qT"] = k.tile(st, f"qTs{sfx}", [128, NEXT], BF16)
    T["V"] = k.tile(st, f"Vs{sfx}", [128, 32, 128], BF16)
    T["e"] = [k.tile(st, f"e_sb{sfx}{i}", [128, 512], F32) for i in range(3)]
    T["L"] = [k.tile(st, f"L_sb{sfx}{i}", [128, 512], BF16) for i in range(3)]
    T["g"] = [k.tile(st, f"g_sb{sfx}{i}", [128, 512], F32) for i in range(2)]
    T["w"] = [k.tile(st, f"w_sb{sfx}{i}", [128, 512], BF16) for i in range(3)]
    T["yb"] = k.tile(st, f"yb{sfx}", [128, 512], BF16)
    return T


def attn_qtile_gen(k, h, q0e, nq, banks, T, scale):
    c = k.c
    kT_, qT_, V_ = T["kT"], T["qT"], T["V"]
    e_sb, L_sb, g_sb, w_sb, Y_ = T["e"], T["L"], T["g"], T["w"], T["yb"]
    ps_s, ps_cs, ps_o = banks
    q0 = HIST + q0e
    kb_max = (q0 + nq) // 128 - 1
    kbs = list(range(kb_max, -1, -1))
    n = len(kbs)

    def diag_m(kb):
        return (kb * 128 - q0) // 128 if kb * 128 >= q0 else None

    def emit_S(i):
        kb = kbs[i]
        E_ = e_sb[i % len(e_sb)]
        c.op("pe", lambda e: e.matmul(ps_s.t[:, 0:nq], lhsT=kT_.t[:, kb * 128:(kb + 1) * 128], rhs=qT_.t[:, q0e:q0e + nq], start=True, stop=True), reads=[kT_.b, qT_.b], writes=[ps_s.b])
        c.op("act", lambda e: e.activation(out=E_.t[:, 0:nq], in_=ps_s.t[:, 0:nq], func=AF.Exp, scale=scale, bias=k.kb_sb.t[:, kb:kb + 1]), reads=[ps_s.b, k.kb_sb.b], writes=[E_.b])

    def emit_L(i):
        kb = kbs[i]
        E_, L_ = e_sb[i % len(e_sb)], L_sb[i % len(L_sb)]
        c.op("act", lambda e: e.activation(out=L_.t[:, 0:nq], in_=E_.t[:, 0:nq], func=AF.Ln, bias=1.0), reads=[E_.b], writes=[L_.b])
        m = diag_m(kb)
        if m is not None:
            c.op("pool", lambda e: e.tensor_tensor(out=L_.t[:, 0:nq], in0=L_.t[:, 0:nq], in1=k.cmask.t[:, m, 0:nq], op=ALU.mult), reads=[L_.b, k.cmask.b], writes=[L_.b])

    def emit_WV(i):
        kb = kbs[i]
        W_ = w_sb[i % len(w_sb)]
        c.op("pe", lambda e: e.matmul(ps_o.t[:, 0:nq], lhsT=V_.t[:, kb, :], rhs=W_.t[:, 0:nq], start=(i == 0), stop=(i == n - 1)), reads=[V_.b, W_.b], writes=[ps_o.b])

    def emit_strict(i):
        L_ = L_sb[i % len(L_sb)]
        c.op("pe", lambda e: e.matmul(ps_cs.t[:, 0:nq], lhsT=k.strl.t[:], rhs=L_.t[:, 0:nq], start=False, stop=True, skip_group_check=True), reads=[k.strl.b, L_.b], writes=[ps_cs.b])

    emit_S(0)
    emit_L(0)
    for i in range(n):
        kb = kbs[i]
        if i + 1 < n:
            emit_S(i + 1)
        if i > 0:
            emit_strict(i - 1)
        E_, L_, G_, W_ = e_sb[i % len(e_sb)], L_sb[i % len(L_sb)], g_sb[i % len(g_sb)], w_sb[i % len(w_sb)]
        c.op("pe", lambda e, L_=L_, i=i: e.matmul(ps_cs.t[:, 0:nq], lhsT=k.triu.t[:], rhs=L_.t[:, 0:nq], start=(i == 0), stop=True, skip_group_check=True), reads=[k.triu.b, L_.b], writes=[ps_cs.b])
        c.op("act", lambda e, G_=G_: e.activation(out=G_.t[:, 0:nq], in_=ps_cs.t[:, 0:nq], func=AF.Exp, scale=-1.0), reads=[ps_cs.b], writes=[G_.b])
        if i + 1 < n:
            emit_L(i + 1)
        if i > 0:
            emit_WV(i - 1)
        c.op("pool", lambda e, E_=E_, G_=G_, W_=W_: e.tensor_tensor(out=W_.t[:, 0:nq], in0=E_.t[:, 0:nq], in1=G_.t[:, 0:nq], op=ALU.mult), reads=[E_.b, G_.b], writes=[W_.b])
        m = diag_m(kb)
        if m is not None:
            c.op("pool", lambda e, W_=W_, m=m: e.tensor_tensor(out=W_.t[:, 0:nq], in0=W_.t[:, 0:nq], in1=k.cmask.t[:, m, 0:nq], op=ALU.mult), reads=[W_.b, k.cmask.b], writes=[W_.b])
        yield
    emit_WV(n - 1)
    k.evac(Y_.t[:, 0:nq], ps_o.t[:, 0:nq], reads=[ps_o.b], writes=[Y_.b], eng="dve")
    c.dma("sp", lambda e: e.dma_start(out=k.yT[4 + h][:, q0e:q0e + nq], in_=Y_.t[:, 0:nq]), reads=[Y_.b])


def attn_stream(k, heads, banks, T, scale):
    c = k.c
    QT = [(0, 128), (128, 512), (640, 512), (1152, 512), (1664, 512)]
    kT_, qT_, V_ = T["kT"], T["qT"], T["V"]
    for h in heads:
        c.dma("sp", lambda e, h=h: e.dma_start(out=kT_.t[:], in_=k.kT[h]), writes=[kT_.b])
        c.dma("sp", lambda e, h=h: e.dma_start(out=qT_.t[:], in_=k.qT[h]), writes=[qT_.b])
        c.dma("sp", lambda e, h=h: e.dma_start(out=V_.t[:], in_=k.vS[:, h * 128:(h + 1) * 128].rearrange("(kb p) d -> p kb d", p=128)), writes=[V_.b])
        for (q0e, nq) in QT:
            yield from attn_qtile_gen(k, h, q0e, nq, banks, T, scale)


def ln_tile(k, r, lnp, S, sbk, tt, res_out_dram, hT_stage, hT32, want_router, gates_sb, rw, rb):
    nc, c = k.nc, k.c
    gB, bB = lnp
    stats, mv, sm = S["stats"], S["mv"], S.get("sm")
    for j in range(4):
        c.op("dve", lambda e, j=j: e.bn_stats(out=stats.t[:, j, :], in_=r.t[:, j * 512:(j + 1) * 512]), reads=[r.b], writes=[stats.b])
    c.op("dve", lambda e: e.bn_aggr(out=mv.t[:], in_=stats.t[:].rearrange("p a b -> p (a b)")), reads=[stats.b], writes=[mv.b])
    c.op("act", lambda e: e.activation(out=mv.t[:, 1:2], in_=mv.t[:, 1:2], func=AF.Sqrt, bias=k.eps.t[:, 0:1]), reads=[mv.b, k.eps.b], writes=[mv.b])
    c.op("dve", lambda e: e.reciprocal(out=mv.t[:, 1:2], in_=mv.t[:, 1:2]), reads=[mv.b], writes=[mv.b])
    c.op("dve", lambda e: e.tensor_scalar(out=r.t[:], in0=r.t[:], scalar1=mv.t[:, 0:1], scalar2=mv.t[:, 1:2], op0=ALU.subtract, op1=ALU.mult), reads=[r.b, mv.b], writes=[r.b])
    c.op("pool", lambda e: e.tensor_tensor(out=r.t[:], in0=r.t[:], in1=gB.t[:], op=ALU.mult), reads=[r.b, gB.b], writes=[r.b])
    c.op("dve", lambda e: e.tensor_tensor(out=r.t[:], in0=r.t[:], in1=bB.t[:], op=ALU.add), reads=[r.b, bB.b], writes=[r.b])
    c.dma("sp", lambda e: e.dma_start(out=res_out_dram, in_=r.t[:]), reads=[r.b])
    cb = sbk * 128
    for g in range(4):
        pb = k.ps[g]
        for j in range(4):
            kc = 4 * g + j
            c.op("pe", lambda e, pb=pb, j=j, kc=kc: e.transpose(pb.t[:, j * 128:(j + 1) * 128], r.t[:, kc * 128:(kc + 1) * 128], k.ident.t[:]), reads=[r.b, k.ident.b], writes=[pb.b])
        src = pb.t[:].rearrange("p (a b) -> p a b", a=4)
        c.op("dve", lambda e, g=g, src=src: e.tensor_copy(out=hT_stage.t[:, 4 * g:4 * g + 4, cb:cb + 128], in_=src), reads=[pb.b], writes=[hT_stage.b])
        if want_router:
            c.op("dve", lambda e, g=g, src=src: e.tensor_copy(out=hT32.t[:, 4 * g:4 * g + 4, :], in_=src), reads=[pb.b], writes=[hT32.b])
    if not want_router:
        return
    pl = k.ps[7]
    hhi, hlo = S["hhi"], S["hlo"]
    rwhi, rwlo = rw
    c.op("dve", lambda e: e.tensor_copy(out=hhi.t[:], in_=hT32.t[:]), reads=[hT32.b], writes=[hhi.b])
    c.op("dve", lambda e: e.tensor_tensor(out=hT32.t[:], in0=hT32.t[:], in1=hhi.t[:], op=ALU.subtract), reads=[hT32.b, hhi.b], writes=[hT32.b])
    c.op("dve", lambda e: e.tensor_copy(out=hlo.t[:], in_=hT32.t[:]), reads=[hT32.b], writes=[hlo.b])
    trip = [(hhi, rwhi), (hlo, rwhi), (hhi, rwlo)]
    for pi3, (ha, wa) in enumerate(trip):
        for kc in range(16):
            c.op("pe", lambda e, kc=kc, ha=ha, wa=wa, pi3=pi3: e.matmul(pl.t[:, 0:16], lhsT=ha.t[:, kc, :], rhs=wa.t[:, kc, :], start=(pi3 == 0 and kc == 0), stop=(pi3 == 2 and kc == 15)), reads=[ha.b, wa.b], writes=[pl.b])
    class _V:
        def __init__(self, t):
            self.t = t
        def __getitem__(self, key):
            a, sl = key
            return self.t[:, sl]
    lg = _V(sm.t)
    LG, PR, MXi, GS, T4, IG, PM, OH1, PM2, OH2, GT = [slice(i * 16, (i + 1) * 16) for i in range(11)]
    mx = lambda a, b: sm.t[:, 32 + a:32 + b]
    smb = [sm.b]
    o = lambda fn, extra=(): c.op("dve", fn, reads=smb + list(extra), writes=smb)
    o(lambda e: e.tensor_tensor(out=lg[:, LG], in0=pl.t[:, 0:16], in1=rb.t[:], op=ALU.add), extra=[pl.b, rb.b])
    o(lambda e: e.tensor_reduce(out=mx(0, 1), in_=lg[:, LG], axis=AX.X, op=ALU.max))
    o(lambda e: e.tensor_scalar(out=lg[:, PR], in0=lg[:, LG], scalar1=mx(0, 1), scalar2=None, op0=ALU.subtract))
    c.op("act", lambda e: e.activation(out=lg[:, PR], in_=lg[:, PR], func=AF.Exp), reads=smb, writes=smb)
    p4 = lg[:, PR].rearrange("p (g e) -> p g e", e=4)
    t4 = lg[:, T4].rearrange("p (g e) -> p g e", e=4)
    gs = lg[:, GS]
    pairs = [(0, 1), (0, 2), (0, 3), (1, 2), (1, 3), (2, 3)]
    for pi_, (a, b) in enumerate(pairs):
        if pi_ == 0:
            o(lambda e, a=a, b=b: e.tensor_tensor(out=gs[:, 0:4], in0=p4[:, :, a], in1=p4[:, :, b], op=ALU.add))
        else:
            o(lambda e, a=a, b=b: e.tensor_tensor(out=gs[:, 4:8], in0=p4[:, :, a], in1=p4[:, :, b], op=ALU.add))
            o(lambda e: e.tensor_tensor(out=gs[:, 0:4], in0=gs[:, 0:4], in1=gs[:, 4:8], op=ALU.max))
    o(lambda e: e.tensor_reduce(out=gs[:, 8:9], in_=gs[:, 0:4], axis=AX.X, op=ALU.max))
    o(lambda e: e.tensor_scalar(out=gs[:, 12:16], in0=gs[:, 0:4], scalar1=gs[:, 8:9], scalar2=None, op0=ALU.is_ge))
    ig = lg[:, IG].rearrange("p (g e) -> p g e", e=4)
    for e4 in range(4):
        o(lambda e, e4=e4: e.tensor_copy(out=ig[:, :, e4], in_=gs[:, 12:16]))
    o(lambda e: e.tensor_tensor(out=lg[:, PM], in0=lg[:, PR], in1=lg[:, IG], op=ALU.mult))
    o(lambda e: e.tensor_tensor(out=lg[:, PM], in0=lg[:, PM], in1=lg[:, IG], op=ALU.add))
    o(lambda e: e.tensor_scalar(out=lg[:, PM], in0=lg[:, PM], scalar1=-1.0, scalar2=None, op0=ALU.add))
    o(lambda e: e.tensor_reduce(out=mx(1, 2), in_=lg[:, PM], axis=AX.X, op=ALU.max))
    o(lambda e: e.tensor_scalar(out=lg[:, OH1], in0=lg[:, PM], scalar1=mx(1, 2), scalar2=None, op0=ALU.is_ge))
    o(lambda e: e.scalar_tensor_tensor(out=lg[:, PM2], in0=lg[:, OH1], scalar=-4.0, in1=lg[:, PM], op0=ALU.mult, op1=ALU.add))
    o(lambda e: e.tensor_reduce(out=mx(2, 3), in_=lg[:, PM2], axis=AX.X, op=ALU.max))
    o(lambda e: e.tensor_scalar(out=lg[:, OH2], in0=lg[:, PM2], scalar1=mx(2, 3), scalar2=None, op0=ALU.is_ge))
    o(lambda e: e.tensor_tensor(out=mx(3, 4), in0=mx(1, 2), in1=mx(2, 3), op=ALU.add))
    o(lambda e: e.reciprocal(out=mx(3, 4), in_=mx(3, 4)))
    o(lambda e: e.tensor_scalar(out=mx(4, 6), in0=mx(1, 3), scalar1=mx(3, 4), scalar2=None, op0=ALU.mult))
    o(lambda e: e.tensor_scalar(out=lg[:, GT], in0=lg[:, OH1], scalar1=mx(4, 5), scalar2=None, op0=ALU.mult))
    c.op("dve", lambda e: e.scalar_tensor_tensor(out=gates_sb.t[:, tt, :], in0=lg[:, OH2], scalar=mx(5, 6), in1=lg[:, GT], op0=ALU.mult, op1=ALU.add), reads=smb, writes=smb + [gates_sb.b])


def load_ln_params(k, st, idx_l, idx_j):
    c = k.c
    gB = k.tile(st, "gB", [128, 2048], F32)
    bB = k.tile(st, "bB", [128, 2048], F32)
    c.dma("sp", lambda e: e.dma_start(out=gB.t[:], in_=k.ln_g[idx_l, idx_j].partition_broadcast(128)), writes=[gB.b])
    c.dma("sp", lambda e: e.dma_start(out=bB.t[:], in_=k.ln_b[idx_l, idx_j].partition_broadcast(128)), writes=[bB.b])
    return gB, bB


def ln_scratch(k, st, router=True):
    if not router:
        return {"stats": k.tile(st, "stats", [128, 4, 6], F32), "mv": k.tile(st, "mv", [128, 2], F32)}
    return {"stats": k.tile(st, "stats", [128, 4, 6], F32), "mv": k.tile(st, "mv", [128, 2], F32), "sm": k.tile(st, "sm", [128, 11 * 16], F32),
            "hhi": k.tile(st, "hhi", [128, 16, 128], BF16), "hlo": k.tile(st, "hlo", [128, 16, 128], BF16)}


def load_router(k, st):
    c = k.c
    rw = k.tile(st, "rw", [128, 16, 16], F32)
    rb = k.tile(st, "rb", [128, 16], F32)
    c.dma("sp", lambda e: e.dma_start(out=rw.t[:], in_=k.router_w.rearrange("(kc p) n -> p kc n", p=128)), writes=[rw.b])
    c.dma("sp", lambda e: e.dma_start(out=rb.t[:], in_=k.router_b.partition_broadcast(128)), writes=[rb.b])
    rwhi = k.tile(st, "rwhi", [128, 16, 16], BF16)
    rwlo = k.tile(st, "rwlo", [128, 16, 16], BF16)
    c.op("dve", lambda e: e.tensor_copy(out=rwhi.t[:], in_=rw.t[:]), reads=[rw.b], writes=[rwhi.b])
    c.op("dve", lambda e: e.tensor_tensor(out=rw.t[:], in0=rw.t[:], in1=rwhi.t[:], op=ALU.subtract), reads=[rw.b, rwhi.b], writes=[rw.b])
    c.op("dve", lambda e: e.tensor_copy(out=rwlo.t[:], in_=rw.t[:]), reads=[rw.b], writes=[rwlo.b])
    return (rwhi, rwlo), rb


def phase_outproj_ln(k, layer, ntok, yT_dram, w_out, res_dram, res_row0, h1_dram, h1T_dram):
    nc, c = k.nc, k.c
    ntt = ntok // 128
    with ExitStack() as st:
        Wo = k.tile(st, "Wo", [128, 16, 2048], BF16)
        yTs = [k.tile(st, f"yTs{i}", [128, 16, 512], BF16) for i in range(2)]
        xr = [k.tile(st, f"xr{i}", [128, 2048], F32) for i in range(2)]
        rr = [k.tile(st, f"rr{i}", [128, 2048], F32) for i in range(2)]
        hst = [k.tile(st, f"hst{i}", [128, 16, 512], BF16) for i in range(2)]
        hT32 = k.tile(st, "hT32", [128, 16, 128], F32)
        lnp = load_ln_params(k, st, layer, 0)
        S = ln_scratch(k, st)
        rw, rb = load_router(k, st)
        for half in range(2):
            c.dma("pool", lambda e, half=half: e.dma_start(out=Wo.t[:, :, half * 1024:(half + 1) * 1024], in_=w_out.rearrange("(kc p) n -> p kc n", p=128)[:, :, half * 1024:(half + 1) * 1024]), reads=[Wo.b], writes=[Wo.b])
        groups = []
        t = 0
        if ntt % 4 == 1:
            groups.append((0, 1))
            t = 1
        while t < ntt:
            groups.append((t, 4))
            t += 4
        for gi, (tt0, ng) in enumerate(groups):
            Y_ = yTs[gi % 2]
            H_ = hst[gi % 2]
            c.dma("sp", lambda e, Y_=Y_, tt0=tt0, ng=ng: e.dma_start(out=Y_.t[:, :, 0:ng * 128], in_=yT_dram[:, :, tt0 * 128:(tt0 + ng) * 128].rearrange("kc p t -> p kc t")), writes=[Y_.b])
            for ti in range(ng):
                tt = tt0 + ti
                X_ = xr[tt % 2]
                R_ = rr[tt % 2]
                r0 = res_row0 + tt * 128
                c.dma("sp", lambda e, X_=X_, r0=r0: e.dma_start(out=X_.t[:], in_=res_dram[r0:r0 + 128, :]), writes=[X_.b])
                for j in range(4):
                    pb = k.ps[4 + j % 2]
                    for kc in range(16):
                        c.op("pe", lambda e, pb=pb, kc=kc, j=j, Y_=Y_, ti=ti: e.matmul(pb.t[:], lhsT=Y_.t[:, kc, ti * 128:(ti + 1) * 128], rhs=Wo.t[:, kc, j * 512:(j + 1) * 512], start=(kc == 0), stop=(kc == 15)), reads=[Y_.b, Wo.b], writes=[pb.b])
                    c.op("dve", lambda e, pb=pb, j=j, X_=X_, R_=R_: e.scalar_tensor_tensor(out=R_.t[:, j * 512:(j + 1) * 512], in0=X_.t[:, j * 512:(j + 1) * 512], scalar=ALPHA, in1=pb.t[:], op0=ALU.mult, op1=ALU.add), reads=[X_.b, pb.b], writes=[R_.b])
                ln_tile(k, R_, lnp, S, ti, tt, h1_dram[tt * 128:(tt + 1) * 128, :], H_, hT32, True, k.gates[layer], rw, rb)
            c.dma("sp", lambda e, H_=H_, tt0=tt0, ng=ng: e.dma_start(out=h1T_dram[:, :, tt0 * 128:(tt0 + ng) * 128].rearrange("kc p t -> p kc t"), in_=H_.t[:, :, 0:ng * 128]), reads=[H_.b])
        c.flush()


def phase_moe(k, layer, supers, hT_dram, h1_dram, out_dram, out_row_of_tile, outT_dram):
    nc, c = k.nc, k.c
    wg, wu, wd = k.moe_wg[layer], k.moe_wu[layer], k.moe_wd[layer]
    gates_sb = k.gates[layer]
    SMAX = max(len(s) for s in supers) * 128
    with ExitStack() as st:
        hT = k.tile(st, "mhT", [128, 16, SMAX], BF16)
        acc = k.tile(st, "macc", [128, SMAX // 128, 2048], F32)
        hid = k.tile(st, "mhid", [128, 8, SMAX], BF16)
        NR = 4 if SMAX <= 768 else 2
        Wg = [k.tile(st, f"mWg{i}", [128, 16, 128], BF16) for i in range(NR)]
        Wu = [k.tile(st, f"mWu{i}", [128, 16, 128], BF16) for i in range(NR)]
        Wd = [k.tile(st, f"mWd{i}", [128, 2, 2048], BF16) for i in range(4)]
        sgt = [k.tile(st, f"msg{i}", [128, 512], BF16) for i in range(2)]
        xr = [k.tile(st, f"mxr{i}", [128, 2048], F32) for i in range(1)]
        hst = k.tile(st, "mhst", [128, 16, 128], BF16)
        lnp = load_ln_params(k, st, layer, 1)
        S = ln_scratch(k, st, router=False)
        wq = 0
        pi = 0
        for tiles in supers:
            ns = len(tiles) * 128
            tt0 = tiles[0]
            c.dma("sp", lambda e, tt0=tt0, ns=ns: e.dma_start(out=hT.t[:, :, 0:ns], in_=hT_dram[:, :, tt0 * 128:tt0 * 128 + ns].rearrange("kc p t -> p kc t")), writes=[hT.b])
            ttl = tok_tiles(ns)
            for ex in range(16):
                for fc in range(8):
                    G_, U_ = Wg[wq % NR], Wu[wq % NR]
                    wq += 1
                    c.dma("pool", lambda e, G_=G_, ex=ex, fc=fc: e.dma_start(out=G_.t[:], in_=wg[ex].rearrange("(kc p) f -> p kc f", p=128)[:, :, fc * 128:(fc + 1) * 128]), writes=[G_.b])
                    c.dma("pool", lambda e, U_=U_, ex=ex, fc=fc: e.dma_start(out=U_.t[:], in_=wu[ex].rearrange("(kc p) f -> p kc f", p=128)[:, :, fc * 128:(fc + 1) * 128]), writes=[U_.b])
                    for (t0, n) in ttl:
                        pg = k.ps[(pi % 2) * 2]
                        pu = k.ps[(pi % 2) * 2 + 1]
                        sg_ = sgt[pi % 2]
                        pi += 1
                        for kc in range(16):
                            c.op("pe", lambda e, pg=pg, G_=G_, kc=kc, t0=t0, n=n: e.matmul(pg.t[:, 0:n], lhsT=G_.t[:, kc, :], rhs=hT.t[:, kc, t0:t0 + n], start=(kc == 0), stop=(kc == 15)), reads=[G_.b, hT.b], writes=[pg.b])
                        for kc in range(16):
                            c.op("pe", lambda e, pu=pu, U_=U_, kc=kc, t0=t0, n=n: e.matmul(pu.t[:, 0:n], lhsT=U_.t[:, kc, :], rhs=hT.t[:, kc, t0:t0 + n], start=(kc == 0), stop=(kc == 15)), reads=[U_.b, hT.b], writes=[pu.b])
                        c.op("act", lambda e, pg=pg, sg_=sg_, n=n: e.activation(out=sg_.t[:, 0:n], in_=pg.t[:, 0:n], func=AF.Silu), reads=[pg.b], writes=[sg_.b])
                        c.op("dve", lambda e, pu=pu, sg_=sg_, fc=fc, t0=t0, n=n: e.tensor_tensor(out=hid.t[:, fc, t0:t0 + n], in0=sg_.t[:, 0:n], in1=pu.t[:, 0:n], op=ALU.mult), reads=[sg_.b, pu.b], writes=[hid.b])
                for pr in range(4):
                    D_ = Wd[pr]
                    c.dma("pool", lambda e, D_=D_, ex=ex, pr=pr: e.dma_start(out=D_.t[:], in_=wd[ex][pr * 256:(pr + 1) * 256, :].rearrange("(fc p) n -> p fc n", p=128)), writes=[D_.b])
                for lt in range(len(tiles)):
                    tt = tiles[lt]
                    for j in range(4):
                        po = k.ps[4 + (lt * 4 + j) % 4]
                        for fc in range(8):
                            D_ = Wd[fc // 2]
                            c.op("pe", lambda e, po=po, fc=fc, D_=D_, lt=lt, j=j: e.matmul(po.t[:], lhsT=hid.t[:, fc, lt * 128:(lt + 1) * 128], rhs=D_.t[:, fc % 2, j * 512:(j + 1) * 512], start=(fc == 0), stop=(fc == 7)), reads=[hid.b, D_.b], writes=[po.b])
                        if ex == 0:
                            c.op("dve", lambda e, po=po, lt=lt, j=j, tt=tt, ex=ex: e.tensor_scalar(out=acc.t[:, lt, j * 512:(j + 1) * 512], in0=po.t[:], scalar1=gates_sb.t[:, tt, ex:ex + 1], scalar2=None, op0=ALU.mult), reads=[po.b, gates_sb.b, acc.b], writes=[acc.b])
                        else:
                            c.op("dve", lambda e, po=po, lt=lt, j=j, tt=tt, ex=ex: e.scalar_tensor_tensor(out=acc.t[:, lt, j * 512:(j + 1) * 512], in0=po.t[:], scalar=gates_sb.t[:, tt, ex:ex + 1], in1=acc.t[:, lt, j * 512:(j + 1) * 512], op0=ALU.mult, op1=ALU.add), reads=[po.b, gates_sb.b, acc.b], writes=[acc.b])
            for lt in range(len(tiles)):
                tt = tiles[lt]
                X_ = xr[0]
                c.dma("sp", lambda e, X_=X_, tt=tt: e.dma_start(out=X_.t[:], in_=h1_dram[tt * 128:(tt + 1) * 128, :]), writes=[X_.b])
                c.op("dve", lambda e, X_=X_, lt=lt: e.scalar_tensor_tensor(out=X_.t[:], in0=X_.t[:], scalar=ALPHA, in1=acc.t[:, lt, :], op0=ALU.mult, op1=ALU.add), reads=[X_.b, acc.b], writes=[X_.b])
                orow = out_row_of_tile(tt)
                dst = out_dram[orow:orow + 128, :] if orow is not None else k.dummy_out[0:128, :]
                ln_tile(k, X_, lnp, S, 0, tt, dst, hst, None, False, None, None, None)
                if outT_dram is not None:
                    c.dma("sp", lambda e, tt=tt: e.dma_start(out=outT_dram[:, :, tt * 128:(tt + 1) * 128].rearrange("kc p t -> p kc t"), in_=hst.t[:, :, 0:128]), reads=[hst.b])
        c.flush()


def phase_pool(k):
    nc, c = k.nc, k.c
    with ExitStack() as st:
        hT = k.tile(st, "phT", [128, 16, NEXT], BF16)
        Wi = [k.tile(st, f"pWi{i}", [128, 16, 512], BF16) for i in range(2)]
        Wgp = [k.tile(st, f"pWg{i}", [128, 4, 512], BF16) for i in range(2)]
        hp = [k.tile(st, f"php{i}", [128, NEXT], F32) for i in range(2)]
        sa = [k.tile(st, f"psa{i}", [128, NEXT], F32) for i in range(2)]
        sb_ = [k.tile(st, f"psb{i}", [128, NEXT], F32) for i in range(2)]
        pl = k.tile(st, "ppl", [128, 4, 2048], BF16)
        ysg = [k.tile(st, f"pys{i}", [128, 2048], BF16) for i in range(2)]
        hm = k.tile(st, "phm", [128, 1], F32)
        ic = k.tile(st, "pic", [128, 64], F32)
        sc = k.tile(st, "psc", [128, 16], F32)
        c.dma("sp", lambda e: e.dma_start(out=hT.t[:], in_=k.h2T.rearrange("kc p t -> p kc t")), writes=[hT.b])
        c.dma("sp", lambda e: e.dma_start(out=hm.t[:], in_=k.halo_mask), writes=[hm.b])
        c.dma("sp", lambda e: e.dma_start(out=ic.t[:], in_=k.invcnt), writes=[ic.b])
        c.dma("sp", lambda e: e.dma_start(out=sc.t[:], in_=k.c_scale.rearrange("(cc p) -> p cc", p=128), allow_slow_non_contiguous=True), writes=[sc.b])
        pi = 0
        for g in range(4):
            w = (2, 4, 8, 16)[g]
            W_ = Wi[g % 2]
            Wg_ = Wgp[g % 2]
            c.dma("pool", lambda e, W_=W_, g=g: e.dma_start(out=W_.t[:], in_=k.c_w_in.rearrange("(kc p) n -> p kc n", p=128)[:, :, g * 512:(g + 1) * 512]), writes=[W_.b])
            c.dma("pool", lambda e, Wg_=Wg_, g=g: e.dma_start(out=Wg_.t[:], in_=k.c_w_group[g].rearrange("(kc p) n -> p kc n", p=128)), writes=[Wg_.b])
            for sub in range(4):
                H_ = hp[sub % 2]
                for (t0, n) in tok_tiles(NEXT):
                    pb = k.ps[pi % 4]
                    pi += 1
                    for kc in range(16):
                        c.op("pe", lambda e, pb=pb, kc=kc, W_=W_, sub=sub, t0=t0, n=n: e.matmul(pb.t[:, 0:n], lhsT=W_.t[:, kc, sub * 128:(sub + 1) * 128], rhs=hT.t[:, kc, t0:t0 + n], start=(kc == 0), stop=(kc == 15)), reads=[W_.b, hT.b], writes=[pb.b])
                    k.evac(H_.t[:, t0:t0 + n], pb.t[:, 0:n], reads=[pb.b], writes=[H_.b])
                c.op("dve", lambda e, H_=H_: e.tensor_scalar(out=H_.t[:, 0:128], in0=H_.t[:, 0:128], scalar1=hm.t[:, 0:1], scalar2=None, op0=ALU.mult), reads=[H_.b, hm.b], writes=[H_.b])
                cur = H_
                step = 1
                pp = [sa[sub % 2], sb_[sub % 2]]
                ii = 0
                while step < w:
                    nxt = pp[ii % 2]
                    ii += 1
                    eng = "pool" if ii % 2 == 0 else "dve"
                    c.op(eng, lambda e, cur=cur, nxt=nxt, step=step: e.tensor_copy(out=nxt.t[:, 0:step], in_=cur.t[:, 0:step]), reads=[cur.b], writes=[nxt.b])
                    c.op(eng, lambda e, cur=cur, nxt=nxt, step=step: e.tensor_tensor(out=nxt.t[:, step:NEXT], in0=cur.t[:, step:NEXT], in1=cur.t[:, 0:NEXT - step], op=ALU.add), reads=[cur.b, nxt.b], writes=[nxt.b])
                    cur = nxt
                    step *= 2
                c.op("dve", lambda e, cur=cur, H_=H_, sub=sub, w=w: e.scalar_tensor_tensor(out=pl.t[:, sub, :], in0=cur.t[:, 128:NEXT], scalar=1.0 / w, in1=H_.t[:, 128:NEXT], op0=ALU.mult, op1=ALU.subtract), reads=[cur.b, H_.b], writes=[pl.b])
                c.op("dve", lambda e, cur=cur, g=g: e.tensor_tensor(out=cur.t[:, 128:144], in0=cur.t[:, 128:144], in1=ic.t[:, g * 16:(g + 1) * 16], op=ALU.mult), reads=[cur.b, ic.b, pl.b], writes=[cur.b])
                c.op("dve", lambda e, cur=cur, H_=H_, sub=sub: e.tensor_tensor(out=pl.t[:, sub, 0:16], in0=cur.t[:, 128:144], in1=H_.t[:, 128:144], op=ALU.subtract), reads=[cur.b, H_.b], writes=[pl.b])
            for oc in range(4):
                Y_ = ysg[oc % 2]
                cc = g * 4 + oc
                for (t0, n) in tok_tiles(2048):
                    pb = k.ps[4 + pi % 4]
                    pi += 1
                    for kc in range(4):
                        c.op("pe", lambda e, pb=pb, kc=kc, Wg_=Wg_, oc=oc, t0=t0, n=n: e.matmul(pb.t[:, 0:n], lhsT=Wg_.t[:, kc, oc * 128:(oc + 1) * 128], rhs=pl.t[:, kc, t0:t0 + n], start=(kc == 0), stop=(kc == 3)), reads=[Wg_.b, pl.b], writes=[pb.b])
                    c.op("dve", lambda e, pb=pb, Y_=Y_, cc=cc, t0=t0, n=n: e.tensor_scalar(out=Y_.t[:, t0:t0 + n], in0=pb.t[:, 0:n], scalar1=sc.t[:, cc:cc + 1], scalar2=None, op0=ALU.mult), reads=[pb.b, sc.b], writes=[Y_.b])
                c.dma("sp", lambda e, Y_=Y_, cc=cc: e.dma_start(out=k.y1T[cc], in_=Y_.t[:]), reads=[Y_.b])
        c.flush()


def build(debug=False, stop_after=None, small_moe=False):
    nc = bass.Bass("TRN2", target_bir_lowering=False)
    k = K(nc, debug)

    def din(name, shape):
        return nc.dram_tensor(name, shape, F32, kind="ExternalInput").ap()

    def scr(name, shape, dt):
        kind = "ExternalOutput" if (debug and name in DEBUG_OUTS) else "Internal"
        return nc.dram_tensor(name, shape, dt, kind=kind).ap()

    k.x_loc = din("x_loc", [4096, 2048])
    k.kbias = din("kbias", [32, 128])
    k.halo_mask = din("halo_mask", [128, 1])
    k.invcnt = din("invcnt", [128, 64])
    k.w_in = din("ab_w_in", [2048, 5120])
    k.lam_re = din("ab_lambda_re", [32, 64])
    k.lam_im = din("ab_lambda_im", [32, 64])
    k.log_dt = din("ab_log_dt", [32])
    k.b_re = din("ab_b_re", [32, 64, 16])
    k.b_im = din("ab_b_im", [32, 64, 16])
    k.c_re = din("ab_c_re", [32, 16, 64])
    k.c_im = din("ab_c_im", [32, 16, 64])
    k.ab_d = din("ab_d", [512])
    k.w_glu = din("ab_w_glu", [512, 512])
    k.b_glu = din("ab_b_glu", [512])
    k.ab_w_out = din("ab_w_out", [2048, 2048])
    k.c_w_in = din("c_w_in", [2048, 2048])
    k.c_w_group = din("c_w_group", [4, 512, 512])
    k.c_scale = din("c_scale", [2048])
    k.c_w_out = din("c_w_out", [2048, 2048])
    k.ln_g = din("ln_g", [2, 2, 2048])
    k.ln_b = din("ln_b", [2, 2, 2048])
    k.router_w = din("router_w", [2048, 16])
    k.router_b = din("router_b", [16])
    if not small_moe:
        k.moe_wg = din("moe_w_gate", [2, 16, 2048, 1024])
        k.moe_wu = din("moe_w_up", [2, 16, 2048, 1024])
        k.moe_wd = din("moe_w_down", [2, 16, 1024, 2048])
    out = nc.dram_tensor("out", [2048, 2048], F32, kind="ExternalOutput").ap()

    k.uT_all = scr("uT", [4, 128, 4096], BF16)
    k.uT = [k.uT_all[i] for i in range(4)]
    qT_all = scr("qT", [12, 128, NEXT], BF16)
    k.qT = [qT_all[i] for i in range(12)]
    kT_all = scr("kT", [12, 128, 4096], BF16)
    k.kT = [kT_all[i] for i in range(12)]
    k.vS = scr("vS", [4096, 1536], BF16)
    k.yT_all = scr("yT", [16, 128, NEXT], BF16)
    k.yT = [k.yT_all[i] for i in range(16)]
    k.vT = scr("vT", [4, 128, NEXT], F32)
    k.h1 = scr("h1", [NEXT, 2048], F32)
    k.h1T = scr("h1T", [16, 128, NEXT], BF16)
    k.h2 = scr("h2", [NEXT, 2048], F32)
    k.h2T = scr("h2T", [16, 128, NEXT], BF16)
    k.y1T_all = scr("y1T", [16, 128, 2048], BF16)
    k.y1T = [k.y1T_all[i] for i in range(16)]
    k.h3 = scr("h3", [2048, 2048], F32)
    k.h3T = scr("h3T", [16, 128, 2048], BF16)
    k.dummy_out = scr("dummy_o", [128, 2048], F32)

    with nc.allow_low_precision("bf16 matmul operands, fp32 accumulation"), nc.allow_non_contiguous_dma(reason="small parameter gathers"):
        setup_consts(k)
        phases = [
            ("inproj0", lambda: (setattr(k, "act_copy_ok", True), phase_inproj0(k), setattr(k, "act_copy_ok", False))),
            ("s5", lambda: phase_s5(k)),
            ("ln0", lambda: phase_outproj_ln(k, 0, NEXT, k.yT_all, k.ab_w_out, k.x_loc, HIST, k.h1, k.h1T)),
            ("moe0", lambda: phase_moe(k, 0, [[0, 1, 2, 3, 4], list(range(5, 11)), list(range(11, 17))], k.h1T, k.h1, k.h2, lambda tt: tt * 128, k.h2T)),
            ("pool", lambda: phase_pool(k)),
            ("ln1", lambda: phase_outproj_ln(k, 1, 2048, k.y1T_all, k.c_w_out, k.h2, 128, k.h3, k.h3T)),
            ("moe1", lambda: phase_moe(k, 1, [list(range(0, 8)), list(range(8, 16))], k.h3T, k.h3, out, lambda tt: tt * 128, None)),
        ]
        for name, fn in phases:
            if stop_after == 'consts':
                break
            fn()
            if stop_after == name:
                break
        k.c.flush()
    return nc, k


DEBUG_OUTS = set()


def make_in_maps(inputs):
    x = np.ascontiguousarray(inputs["x"], dtype=np.float32)
    sq = lambda a: np.ascontiguousarray(np.asarray(a, dtype=np.float32)[0])
    shared = {
        "ab_w_in": sq(inputs["ab_w_in"]), "ab_lambda_re": sq(inputs["ab_lambda_re"]), "ab_lambda_im": sq(inputs["ab_lambda_im"]),
        "ab_log_dt": sq(inputs["ab_log_dt"]), "ab_b_re": sq(inputs["ab_b_re"]), "ab_b_im": sq(inputs["ab_b_im"]),
        "ab_c_re": sq(inputs["ab_c_re"]), "ab_c_im": sq(inputs["ab_c_im"]), "ab_d": sq(inputs["ab_d"]),
        "ab_w_glu": sq(inputs["ab_w_glu"]), "ab_b_glu": sq(inputs["ab_b_glu"]), "ab_w_out": sq(inputs["ab_w_out"]),
        "c_w_in": sq(inputs["c_w_in"]), "c_w_group": sq(inputs["c_w_group"]), "c_scale": sq(inputs["c_scale"]),
        "c_w_out": sq(inputs["c_w_out"]),
        "ln_g": np.ascontiguousarray(inputs["ln_g"], dtype=np.float32), "ln_b": np.ascontiguousarray(inputs["ln_b"], dtype=np.float32),
        "router_w": np.ascontiguousarray(inputs["router_w"], dtype=np.float32), "router_b": np.ascontiguousarray(inputs["router_b"], dtype=np.float32),
        "moe_w_gate": np.ascontiguousarray(inputs["moe_w_gate"], dtype=np.float32),
        "moe_w_up": np.ascontiguousarray(inputs["moe_w_up"], dtype=np.float32),
        "moe_w_down": np.ascontiguousarray(inputs["moe_w_down"], dtype=np.float32),
    }
    windows = (2, 4, 8, 16)
    in_maps = []
    for core in range(8):
        b, p = core // 2, core % 2
        m = dict(shared)
        if p == 1:
            m["x_loc"] = x[b]
            kb = np.zeros((32, 128), np.float32)
            hm = np.ones((128, 1), np.float32)
            ic = np.stack([np.full(16, 1.0 / w, np.float32) for w in windows])
        else:
            xl = np.zeros((4096, 2048), np.float32)
            xl[2048:] = x[b, :2048]
            m["x_loc"] = xl
            kb = np.zeros((32, 128), np.float32)
            kb[:16] = -30000.0
            hm = np.zeros((128, 1), np.float32)
            ic = np.stack([1.0 / np.minimum(np.arange(16) + 1, w).astype(np.float32) for w in windows])
        m["kbias"] = kb
        m["halo_mask"] = hm
        m["invcnt"] = np.ascontiguousarray(np.broadcast_to(ic.reshape(1, 64), (128, 64)), dtype=np.float32)
        in_maps.append(m)
    return in_maps


def kernel(**inputs):
    nc, _ = build()
    in_maps = make_in_maps(inputs)
    res = run_bass_kernel_spmd(nc, in_maps, core_ids=list(range(8)))
    outp = np.empty((4, 4096, 2048), np.float32)
    for core in range(8):
        b, p = core // 2, core % 2
        outp[b, p * 2048:(p + 1) * 2048] = res.results[core]["out"]
    return outp
```

```python
from contextlib import ExitStack
import numpy as np
import concourse.bass as bass
import concourse.mybir as mybir
from concourse.bass_utils import run_bass_kernel_spmd

F32 = mybir.dt.float32
BF16 = mybir.dt.bfloat16
I32 = mybir.dt.int32
AF = mybir.ActivationFunctionType
ALU = mybir.AluOpType
AX = mybir.AxisListType

NDMA = 24
NOSELF = ("pe",)
MAXFLY = 6
ALPHA = 4.0 ** 0.25
LN_EPS = 1e-5
HIST = 1920
NEXT = 2176
LC = 256
TWO_PI = float(2 * np.pi)


class Buf:
    __slots__ = ("name", "lw", "rd")

    def __init__(self, name):
        self.name = name
        self.lw = None
        self.rd = []


class Op:
    __slots__ = ("eng", "fn", "reads", "writes", "dma", "deps", "has_dep", "ms", "sem_idx", "sem_val")

    def __init__(self, eng, fn, reads, writes, dma):
        self.eng = eng
        self.fn = fn
        self.reads = reads
        self.writes = writes
        self.dma = dma
        self.deps = ()
        self.has_dep = False
        self.ms = 0
        self.sem_idx = -1
        self.sem_val = 0


class Ctx:
    ENG = ("pe", "act", "dve", "pool", "sp")

    def __init__(self, nc):
        self.nc = nc
        self.e = {"pe": nc.tensor, "act": nc.scalar, "dve": nc.vector, "pool": nc.gpsimd, "sp": nc.sync}
        self.ops = []
        self.bufs = []
        self.dma_sems = [nc.alloc_semaphore(f"dmas{i}") for i in range(NDMA)]
        self.dma_counts = [0] * NDMA
        self.dma_rr = 0
        self.bar = nc.alloc_semaphore("bar")
        self.nbar = 0
        self.waited = {e: {} for e in self.ENG}
        self.n_inst = 0
        self.inflight = {}

    def buf(self, name):
        b = Buf(name)
        self.bufs.append(b)
        return b

    def op(self, eng, fn, reads=(), writes=()):
        self.ops.append(Op(eng, fn, tuple(reads), tuple(writes), False))

    def dma(self, eng, fn, reads=(), writes=()):
        self.ops.append(Op(eng, fn, tuple(reads), tuple(writes), True))

    def _wait(self, E, key, sem, val):
        w = self.waited[E]
        if w.get(key, 0) < val:
            self.e[E].wait_ge(sem, val)
            w[key] = val
            self.n_inst += 1

    def flush(self):
        nc = self.nc
        ops = self.ops
        if not ops:
            return
        for b in self.bufs:
            b.lw = None
            b.rd = []
        last_on = {}
        for i, op in enumerate(ops):
            deps = set()
            for b in op.reads:
                if b.lw is not None:
                    deps.add(b.lw)
            for b in op.writes:
                if b.lw is not None:
                    deps.add(b.lw)
                deps.update(b.rd)
            deps.discard(i)
            if op.eng in NOSELF and not op.dma:
                deps = {d for d in deps if ops[d].dma or ops[d].eng != op.eng}
            op.deps = sorted(deps)
            for d in op.deps:
                ops[d].has_dep = True
            for b in op.reads:
                if not op.dma:
                    b.rd = [r for r in b.rd if ops[r].dma or ops[r].eng != op.eng]
                b.rd.append(i)
            for b in op.writes:
                b.lw = i
                b.rd = []
            if not op.dma:
                last_on[op.eng] = i
        for e, i in last_on.items():
            ops[i].has_dep = True
        sem = {e: nc.alloc_semaphore(f"ph{self.nbar}_{e}") for e in self.ENG if e != "sp"}
        cnt = {e: 0 for e in self.ENG}
        dma_used = set()
        for op in ops:
            E = op.eng
            for d in op.deps:
                D = ops[d]
                if D.dma:
                    self._wait(E, ("d", D.sem_idx), self.dma_sems[D.sem_idx], D.sem_val)
                else:
                    self._wait(E, ("e", D.eng), sem[D.eng], D.ms)
            if op.dma:
                fl = self.inflight.setdefault(E, [])
                if len(fl) >= MAXFLY:
                    pk, pv = fl[len(fl) - MAXFLY]
                    self._wait(E, ("d", pk), self.dma_sems[pk], pv)
                k = self.dma_rr
                self.dma_rr = (self.dma_rr + 1) % NDMA
                if self.dma_counts[k] > 0:
                    self._wait(E, ("d", k), self.dma_sems[k], self.dma_counts[k])
                self.dma_counts[k] += 16
                op.sem_idx = k
                op.sem_val = self.dma_counts[k]
                dma_used.add(k)
                fl.append((k, op.sem_val))
                ins = op.fn(self.e[E])
                ins.then_inc(self.dma_sems[k], 16)
            else:
                ins = op.fn(self.e[E])
                if op.has_dep:
                    cnt[E] += 1
                    op.ms = cnt[E]
                    ins.then_inc(sem[E], 1)
            self.n_inst += 1
        for k in sorted(dma_used):
            self._wait("sp", ("d", k), self.dma_sems[k], self.dma_counts[k])
        for e in self.ENG:
            if e != "sp" and cnt[e] > 0:
                self._wait("sp", ("e", e), sem[e], cnt[e])
        self.nbar += 1
        self.e["sp"].sem_inc(self.bar, 1)
        for e in self.ENG:
            if e != "sp":
                self.e[e].wait_ge(self.bar, self.nbar)
        self.ops = []
        self.waited = {e: {k: v for k, v in self.waited[e].items() if k[0] == "d"} for e in self.ENG}


class TB:
    __slots__ = ("t", "b")

    def __init__(self, t, b):
        self.t = t
        self.b = b


class K:
    def __init__(self, nc, debug):
        self.nc = nc
        self.c = Ctx(nc)
        self.debug = debug
        self.ev = 0
        self.uid = 0
        self.act_copy_ok = False

    def tile(self, st, name, shape, dt):
        self.uid += 1
        t = st.enter_context(self.nc.sbuf_tensor(f"{name}_{self.uid}", shape, dt))
        return TB(t, self.c.buf(name))

    def gtile(self, name, shape, dt):
        t = self.nc.alloc_sbuf_tensor(name, shape, dt)
        return TB(t, self.c.buf(name))

    def evac(self, out, in_, reads, writes, eng=None):
        if eng is None:
            eng = "act" if (self.ev % 2 == 0 and self.act_copy_ok) else "dve"
            self.ev += 1
        if eng == "act":
            self.c.op("act", lambda e: e.activation(out=out, in_=in_, func=AF.Copy), reads=reads, writes=writes)
        else:
            self.c.op(eng, lambda e: e.tensor_copy(out=out, in_=in_), reads=reads, writes=writes)


def tok_tiles(n, w=512):
    out = []
    t = 0
    while t < n:
        m = min(w, n - t)
        out.append((t, m))
        t += m
    return out


def setup_consts(k):
    nc, c = k.nc, k.c
    k.ident = k.gtile("ident", [128, 128], F32)
    k.identb = k.gtile("identb", [128, 128], BF16)
    k.triu = k.gtile("triu", [128, 128], BF16)
    k.strl = k.gtile("strl", [128, 128], BF16)
    k.cmask = k.gtile("cmask", [128, 4, 512], BF16)
    k.kb_sb = k.gtile("kb_sb", [128, 32], F32)
    k.eps = k.gtile("eps", [128, 1], F32)
    k.gates = [k.gtile(f"gates{i}", [128, 17, 16], F32) for i in range(2)]
    k.ps = [TB(nc.alloc_psum_tensor(f"ps{i}", [128, 512], F32), c.buf(f"ps{i}")) for i in range(8)]
    with ExitStack() as st:
        tmp = k.tile(st, "ctmp", [128, 512], F32)
        c.op("pool", lambda e: e.memset(k.ident.t[:], 0.0), writes=[k.ident.b])
        c.op("pool", lambda e: e.memset(k.eps.t[:], LN_EPS), writes=[k.eps.b])
        c.op("pool", lambda e: e.affine_select(out=k.ident.t[:], in_=k.ident.t[:], pattern=[[-1, 128]], compare_op=ALU.not_equal, fill=1.0, base=0, channel_multiplier=1), reads=[k.ident.b], writes=[k.ident.b])
        c.op("dve", lambda e: e.tensor_copy(out=k.identb.t[:], in_=k.ident.t[:]), reads=[k.ident.b], writes=[k.identb.b])
        c.op("pool", lambda e: e.memset(tmp.t[:, 0:128], 1.0), writes=[tmp.b])
        c.op("pool", lambda e: e.affine_select(out=tmp.t[:, 0:128], in_=tmp.t[:, 0:128], pattern=[[-1, 128]], compare_op=ALU.is_ge, fill=0.0, base=0, channel_multiplier=1), reads=[tmp.b], writes=[tmp.b])
        c.op("dve", lambda e: e.tensor_copy(out=k.triu.t[:], in_=tmp.t[:, 0:128]), reads=[tmp.b], writes=[k.triu.b])
        c.op("dve", lambda e: e.tensor_scalar(out=k.strl.t[:], in0=tmp.t[:, 0:128], scalar1=-1.0, scalar2=1.0, op0=ALU.mult, op1=ALU.add), reads=[tmp.b], writes=[k.strl.b])
        for m in range(4):
            c.op("pool", lambda e: e.memset(tmp.t[:], 1.0), reads=[tmp.b], writes=[tmp.b])
            c.op("pool", lambda e, m=m: e.affine_select(out=tmp.t[:], in_=tmp.t[:], pattern=[[1, 512]], compare_op=ALU.is_gt, fill=0.0, base=-128 * m, channel_multiplier=-1), reads=[tmp.b], writes=[tmp.b])
            c.op("dve", lambda e, m=m: e.tensor_copy(out=k.cmask.t[:, m, :], in_=tmp.t[:]), reads=[tmp.b], writes=[k.cmask.b])
        c.dma("sp", lambda e: e.dma_start(out=k.kb_sb.t[:], in_=k.kbias.rearrange("kb p -> p kb"), allow_slow_non_contiguous=True), writes=[k.kb_sb.b])
        c.flush()


def phase_inproj0(k):
    nc, c = k.nc, k.c
    with ExitStack() as st:
        xT = k.tile(st, "xT", [128, 16, NEXT], BF16)
        xs = [k.tile(st, f"xs{i}", [128, 2048], F32) for i in range(2)]
        Wb = [k.tile(st, f"Wb{i}", [128, 16, 512], BF16) for i in range(2)]
        stg = [k.tile(st, f"stg{i}", [128, NEXT], BF16) for i in range(2)]
        vst = [k.tile(st, f"vst{i}", [128, 512], BF16) for i in range(3)]
        wi = si = vi = pi = 0
        for (tok0, ntok, blocks) in ((0, HIST, [0, 4, 5, 6, 7, 8, 9]), (HIST, NEXT, list(range(10)))):
            for tt in range(ntok // 128):
                x_ = xs[tt % 2]
                r0 = tok0 + tt * 128
                c.dma("sp", lambda e, x_=x_, r0=r0: e.dma_start(out=x_.t[:], in_=k.x_loc[r0:r0 + 128, :]), writes=[x_.b])
                for g in range(4):
                    pb = k.ps[g]
                    for j in range(4):
                        kc = 4 * g + j
                        c.op("pe", lambda e, pb=pb, j=j, kc=kc, x_=x_: e.transpose(pb.t[:, j * 128:(j + 1) * 128], x_.t[:, kc * 128:(kc + 1) * 128], k.ident.t[:]), reads=[x_.b, k.ident.b], writes=[pb.b])
                    k.evac(xT.t[:, 4 * g:4 * g + 4, tt * 128:(tt + 1) * 128], pb.t[:].rearrange("p (a b) -> p a b", a=4), reads=[pb.b], writes=[xT.b])
            for blk in blocks:
                W_ = Wb[wi % 2]
                wi += 1
                c.dma("pool", lambda e, W_=W_, blk=blk: e.dma_start(out=W_.t[:], in_=k.w_in.rearrange("(kc p) n -> p kc n", p=128)[:, :, blk * 512:(blk + 1) * 512]), writes=[W_.b])
                if blk < 7:
                    for sub in range(4):
                        s_ = stg[si % 2]
                        si += 1
                        for (t0, n) in tok_tiles(ntok):
                            pb = k.ps[4 + pi % 4]
                            pi += 1
                            for kc in range(16):
                                c.op("pe", lambda e, pb=pb, kc=kc, W_=W_, sub=sub, t0=t0, n=n: e.matmul(pb.t[:, 0:n], lhsT=W_.t[:, kc, sub * 128:(sub + 1) * 128], rhs=xT.t[:, kc, t0:t0 + n], start=(kc == 0), stop=(kc == 15)), reads=[W_.b, xT.b], writes=[pb.b])
                            k.evac(s_.t[:, t0:t0 + n], pb.t[:, 0:n], reads=[pb.b], writes=[s_.b])
                        if blk == 0:
                            dst = k.uT[sub][:, tok0:tok0 + ntok]
                        elif blk < 4:
                            dst = k.qT[(blk - 1) * 4 + sub][:, 0:ntok]
                        else:
                            dst = k.kT[(blk - 4) * 4 + sub][:, tok0:tok0 + ntok]
                        c.dma("sp", lambda e, dst=dst, s_=s_, ntok=ntok: e.dma_start(out=dst, in_=s_.t[:, 0:ntok]), reads=[s_.b])
                else:
                    for tt in range(ntok // 128):
                        pb = k.ps[4 + pi % 4]
                        pi += 1
                        v_ = vst[vi % 3]
                        vi += 1
                        for kc in range(16):
                            c.op("pe", lambda e, pb=pb, kc=kc, W_=W_, tt=tt: e.matmul(pb.t[:], lhsT=xT.t[:, kc, tt * 128:(tt + 1) * 128], rhs=W_.t[:, kc, :], start=(kc == 0), stop=(kc == 15)), reads=[W_.b, xT.b], writes=[pb.b])
                        k.evac(v_.t[:], pb.t[:], reads=[pb.b], writes=[v_.b])
                        r0 = tok0 + tt * 128
                        c.dma("sp", lambda e, v_=v_, r0=r0, blk=blk: e.dma_start(out=k.vS[r0:r0 + 128, (blk - 7) * 512:(blk - 6) * 512], in_=v_.t[:]), reads=[v_.b])
        c.flush()


def range_reduce_sin(k, out, ang, tmp_i, tmp_f, bufs_r, bufs_w, eng="dve"):
    c = k.c
    c.op(eng, lambda e: e.tensor_scalar(out=tmp_i, in0=ang, scalar1=1.0 / TWO_PI, scalar2=None, op0=ALU.mult), reads=bufs_r, writes=bufs_w)
    c.op(eng, lambda e: e.tensor_copy(out=tmp_f, in_=tmp_i), reads=bufs_w, writes=bufs_w)
    c.op(eng, lambda e: e.scalar_tensor_tensor(out=tmp_f, in0=tmp_f, scalar=-TWO_PI, in1=ang, op0=ALU.mult, op1=ALU.add), reads=list(bufs_r) + list(bufs_w), writes=bufs_w)
    c.op("act", lambda e: e.activation(out=out, in_=tmp_f, func=AF.Sin), reads=bufs_w, writes=bufs_w)


def phase_s5(k):
    nc, c = k.nc, k.c
    NCH = 4096 // LC
    first_ext_chunk = HIST // LC
    with ExitStack() as st:
        uT = k.tile(st, "s5uT", [128, 4, 4096], BF16)
        cosT = k.tile(st, "cosT", [128, 16, LC + 1], F32)
        sinT = k.tile(st, "sinT", [128, 16, LC + 1], F32)
        BT = k.tile(st, "BT", [128, 32, 128], BF16)
        CT = k.tile(st, "CT", [128, 32, 128], BF16)
        par = k.tile(st, "s5par", [128, 12, 16], F32)
        zin = [k.tile(st, f"zin{i}", [128, 2], F32) for i in range(16)]
        dcol = k.tile(st, "dcol", [128, 4], F32)
        bglu = k.tile(st, "bglu", [128, 4], F32)
        wglu = k.tile(st, "wglu", [128, 4, 512], BF16)
        iot = k.tile(st, "iot", [128, LC + 1], F32)
        LR, LI, DT, TH, MAG, LBR, LBI, CR, CI, T0, T1, T2 = range(12)
        c.dma("sp", lambda e: e.dma_start(out=par.t[:, LR, :], in_=k.lam_re.rearrange("(gp gl) n -> (gl n) gp", gl=2), allow_slow_non_contiguous=True), writes=[par.b])
        c.dma("sp", lambda e: e.dma_start(out=par.t[:, LI, :], in_=k.lam_im.rearrange("(gp gl) n -> (gl n) gp", gl=2), allow_slow_non_contiguous=True), writes=[par.b])
        for gl in range(2):
            c.dma("sp", lambda e, gl=gl: e.dma_start(out=par.t[gl * 64:(gl + 1) * 64, DT, :], in_=k.log_dt.rearrange("(gp gl) -> gp gl", gl=2)[:, gl].partition_broadcast(64)), writes=[par.b])
        c.dma("sp", lambda e: e.dma_start(out=dcol.t[:], in_=k.ab_d.rearrange("(ct p) -> p ct", p=128), allow_slow_non_contiguous=True), writes=[dcol.b])
        c.dma("sp", lambda e: e.dma_start(out=bglu.t[:], in_=k.b_glu.rearrange("(ct p) -> p ct", p=128), allow_slow_non_contiguous=True), writes=[bglu.b])
        c.dma("pool", lambda e: e.dma_start(out=wglu.t[:], in_=k.w_glu.rearrange("(kc p) n -> p kc n", p=128)), writes=[wglu.b])
        c.dma("sp", lambda e: e.dma_start(out=uT.t[:], in_=k.uT_all.rearrange("ct p t -> p ct t")), writes=[uT.b])
        ioti = k.tile(st, "ioti", [128, LC + 1], I32)
        c.op("pool", lambda e: e.iota(ioti.t[:], pattern=[[1, LC + 1]], base=0, channel_multiplier=0), writes=[ioti.b])
        c.op("dve", lambda e: e.tensor_copy(out=iot.t[:], in_=ioti.t[:]), reads=[ioti.b], writes=[iot.b])
        for z_ in zin:
            c.op("pool", lambda e, z_=z_: e.memset(z_.t[:], 0.0), writes=[z_.b])
        P = lambda i: par.t[:, i, :]
        c.op("act", lambda e: e.activation(out=P(DT), in_=P(DT), func=AF.Exp), reads=[par.b], writes=[par.b])
        c.op("dve", lambda e: e.tensor_tensor(out=P(TH), in0=P(LI), in1=P(DT), op=ALU.mult), reads=[par.b], writes=[par.b])
        c.op("dve", lambda e: e.tensor_tensor(out=P(T0), in0=P(LR), in1=P(DT), op=ALU.mult), reads=[par.b], writes=[par.b])
        c.op("act", lambda e: e.activation(out=P(MAG), in_=P(T0), func=AF.Exp), reads=[par.b], writes=[par.b])
        import os
        s5stop = int(os.environ.get("S5STOP", "9"))
        if s5stop == 1:
            c.flush()
            return
        with ExitStack() as st2:
            ang = k.tile(st2, "ang", [128, LC + 1], F32)
            ti = k.tile(st2, "ti", [128, LC + 1], I32)
            tf = k.tile(st2, "tf", [128, LC + 1], F32)
            for gp in range(16):
                c.op("dve", lambda e, gp=gp: e.tensor_scalar(out=ang.t[:], in0=iot.t[:], scalar1=par.t[:, TH, gp:gp + 1], scalar2=None, op0=ALU.mult), reads=[iot.b, par.b], writes=[ang.b])
                range_reduce_sin(k, sinT.t[:, gp, :], ang.t[:], ti.t[:], tf.t[:], [ang.b], [ti.b, tf.b, sinT.b])
                c.op("dve", lambda e: e.tensor_scalar(out=ang.t[:], in0=ang.t[:], scalar1=float(np.pi / 2), scalar2=None, op0=ALU.add), reads=[ang.b, ti.b, tf.b], writes=[ang.b])
                range_reduce_sin(k, cosT.t[:, gp, :], ang.t[:], ti.t[:], tf.t[:], [ang.b], [ti.b, tf.b, cosT.b])
            if s5stop == 2:
                c.flush()
                return
            c.op("dve", lambda e: e.tensor_tensor(out=P(LBR), in0=P(MAG), in1=cosT.t[:, :, 1], op=ALU.mult), reads=[par.b, cosT.b], writes=[par.b])
            c.op("dve", lambda e: e.tensor_tensor(out=P(LBI), in0=P(MAG), in1=sinT.t[:, :, 1], op=ALU.mult), reads=[par.b, sinT.b], writes=[par.b])
            c.op("dve", lambda e: e.tensor_tensor(out=P(T0), in0=P(LR), in1=P(LR), op=ALU.mult), reads=[par.b], writes=[par.b])
            c.op("dve", lambda e: e.tensor_tensor(out=P(T1), in0=P(LI), in1=P(LI), op=ALU.mult), reads=[par.b], writes=[par.b])
            c.op("dve", lambda e: e.tensor_tensor(out=P(T0), in0=P(T0), in1=P(T1), op=ALU.add), reads=[par.b], writes=[par.b])
            c.op("dve", lambda e: e.reciprocal(out=P(T2), in_=P(T0)), reads=[par.b], writes=[par.b])
            c.op("dve", lambda e: e.tensor_scalar(out=P(LBR), in0=P(LBR), scalar1=-1.0, scalar2=None, op0=ALU.add), reads=[par.b], writes=[par.b])
            c.op("dve", lambda e: e.tensor_tensor(out=P(T0), in0=P(LBR), in1=P(LR), op=ALU.mult), reads=[par.b], writes=[par.b])
            c.op("dve", lambda e: e.tensor_tensor(out=P(T1), in0=P(LBI), in1=P(LI), op=ALU.mult), reads=[par.b], writes=[par.b])
            c.op("dve", lambda e: e.tensor_tensor(out=P(T0), in0=P(T0), in1=P(T1), op=ALU.add), reads=[par.b], writes=[par.b])
            c.op("dve", lambda e: e.tensor_tensor(out=P(CR), in0=P(T0), in1=P(T2), op=ALU.mult), reads=[par.b], writes=[par.b])
            c.op("dve", lambda e: e.tensor_tensor(out=P(T0), in0=P(LBI), in1=P(LR), op=ALU.mult), reads=[par.b], writes=[par.b])
            c.op("dve", lambda e: e.tensor_tensor(out=P(T1), in0=P(LBR), in1=P(LI), op=ALU.mult), reads=[par.b], writes=[par.b])
            c.op("dve", lambda e: e.tensor_tensor(out=P(T0), in0=P(T0), in1=P(T1), op=ALU.subtract), reads=[par.b], writes=[par.b])
            c.op("dve", lambda e: e.tensor_tensor(out=P(CI), in0=P(T0), in1=P(T2), op=ALU.mult), reads=[par.b], writes=[par.b])
            if s5stop == 3:
                c.flush()
                return
            zr = [k.tile(st2, f"zr{i}", [128, 128], F32) for i in range(2)]
            zi = [k.tile(st2, f"zi{i}", [128, 128], F32) for i in range(2)]
            yr = [k.tile(st2, f"yr{i}", [128, 128], F32) for i in range(2)]
            yi = [k.tile(st2, f"yi{i}", [128, 128], F32) for i in range(2)]
            bb = [k.tile(st2, f"bb{i}", [128, 2, 128], F32) for i in range(2)]
            for gp in range(16):
                i = gp % 2
                Zr, Zi, Yr, Yi, Bb = zr[i], zi[i], yr[i], yi[i], bb[i]
                for T_ in (Zr, Zi, Yr, Yi):
                    c.op("pool", lambda e, T_=T_: e.memset(T_.t[:], 0.0), writes=[T_.b])
                for gl in range(2):
                    g = 2 * gp + gl
                    c0 = (g % 8) * 16
                    c.dma("sp", lambda e, Zr=Zr, g=g, gl=gl, c0=c0: e.dma_start(out=Zr.t[gl * 64:(gl + 1) * 64, c0:c0 + 16], in_=k.b_re[g]), reads=[Zr.b], writes=[Zr.b])
                    c.dma("sp", lambda e, Zi=Zi, g=g, gl=gl, c0=c0: e.dma_start(out=Zi.t[gl * 64:(gl + 1) * 64, c0:c0 + 16], in_=k.b_im[g]), reads=[Zi.b], writes=[Zi.b])
                    c.dma("sp", lambda e, Yr=Yr, g=g, gl=gl, c0=c0: e.dma_start(out=Yr.t[c0:c0 + 16, gl * 64:(gl + 1) * 64], in_=k.c_re[g]), reads=[Yr.b], writes=[Yr.b])
                    c.dma("sp", lambda e, Yi=Yi, g=g, gl=gl, c0=c0: e.dma_start(out=Yi.t[c0:c0 + 16, gl * 64:(gl + 1) * 64], in_=k.c_im[g]), reads=[Yi.b], writes=[Yi.b])
                cr = par.t[:, CR, gp:gp + 1]
                ci = par.t[:, CI, gp:gp + 1]
                c.op("dve", lambda e, Bb=Bb, Zi=Zi, ci=ci: e.tensor_scalar(out=Bb.t[:, 0, :], in0=Zi.t[:], scalar1=ci, scalar2=None, op0=ALU.mult), reads=[Zi.b, par.b], writes=[Bb.b])
                c.op("dve", lambda e, Bb=Bb, Zr=Zr, cr=cr: e.scalar_tensor_tensor(out=Bb.t[:, 0, :], in0=Zr.t[:], scalar=cr, in1=Bb.t[:, 0, :], op0=ALU.mult, op1=ALU.subtract), reads=[Zr.b, par.b, Bb.b], writes=[Bb.b])
                c.op("dve", lambda e, Bb=Bb, Zr=Zr, ci=ci: e.tensor_scalar(out=Bb.t[:, 1, :], in0=Zr.t[:], scalar1=ci, scalar2=None, op0=ALU.mult), reads=[Zr.b, par.b], writes=[Bb.b])
                c.op("dve", lambda e, Bb=Bb, Zi=Zi, cr=cr: e.scalar_tensor_tensor(out=Bb.t[:, 1, :], in0=Zi.t[:], scalar=cr, in1=Bb.t[:, 1, :], op0=ALU.mult, op1=ALU.add), reads=[Zi.b, par.b, Bb.b], writes=[Bb.b])
                if s5stop == 5:
                    continue
                pb = k.ps[gp % 2]
                for part in range(2):
                    c.op("pe", lambda e, pb=pb, Bb=Bb, part=part: e.transpose(pb.t[:, part * 128:(part + 1) * 128], Bb.t[:, part, :], k.ident.t[:]), reads=[Bb.b, k.ident.b], writes=[pb.b])
                c.op("pe", lambda e, pb=pb, Yr=Yr: e.transpose(pb.t[:, 256:384], Yr.t[:], k.ident.t[:]), reads=[Yr.b, k.ident.b], writes=[pb.b])
                c.op("pe", lambda e, pb=pb, Yi=Yi: e.transpose(pb.t[:, 384:512], Yi.t[:], k.ident.t[:]), reads=[Yi.b, k.ident.b], writes=[pb.b])
                if s5stop == 6:
                    continue
                c.op("dve", lambda e, pb=pb, gp=gp: e.tensor_copy(out=BT.t[:, 2 * gp:2 * gp + 2, :], in_=pb.t[:, 0:256].rearrange("p (a b) -> p a b", a=2)), reads=[pb.b], writes=[BT.b])
                if s5stop == 7:
                    continue
                c.op("dve", lambda e, pb=pb, gp=gp: e.tensor_copy(out=CT.t[:, 2 * gp, :], in_=pb.t[:, 256:384]), reads=[pb.b], writes=[CT.b])
                c.op("dve", lambda e, pb=pb, gp=gp: e.tensor_scalar(out=CT.t[:, 2 * gp + 1, :], in0=pb.t[:, 384:512], scalar1=-1.0, scalar2=None, op0=ALU.mult), reads=[pb.b], writes=[CT.b])
            c.flush()
        if s5stop in (4, 5, 6, 7):
            return
        with ExitStack() as st3:
            NW = 2
            cre = [k.tile(st3, f"cre{i}", [128, LC], F32) for i in range(NW)]
            cim = [k.tile(st3, f"cim{i}", [128, LC], F32) for i in range(NW)]
            t1 = [k.tile(st3, f"t1_{i}", [128, LC], F32) for i in range(NW)]
            t2 = [k.tile(st3, f"t2_{i}", [128, LC], F32) for i in range(NW)]
            zre = [k.tile(st3, f"zre{i}", [128, LC], F32) for i in range(NW)]
            zim = [k.tile(st3, f"zim{i}", [128, LC], F32) for i in range(NW)]
            sre = [k.tile(st3, f"sre{i}", [128, LC], BF16) for i in range(8)]
            sim = [k.tile(st3, f"sim{i}", [128, LC], BF16) for i in range(8)]
            ctmp = [k.tile(st3, f"cz{i}", [128, 2], F32) for i in range(NW)]
            vv = [k.tile(st3, f"vv{i}", [128, LC], F32) for i in range(4)]
            py7 = TB(k.ps[7].t, c.buf("ps7a"))
            pg7 = TB(k.ps[7].t, c.buf("ps7b"))

            def s5_gen():
                wk = 0
                for ch in range(NCH):
                    t0 = ch * LC
                    need_y = ch >= first_ext_chunk
                    for ct in range(4):
                        py = py7
                        for q in range(4):
                            gp = 4 * ct + q
                            yield
                            w = wk % NW
                            wk += 1
                            eA = "dve"
                            pb = k.ps[6]
                            for part in range(2):
                                c.op("pe", lambda e, pb=pb, gp=gp, part=part, ct=ct, t0=t0: e.matmul(pb.t[:, part * LC:(part + 1) * LC], lhsT=BT.t[:, 2 * gp + part, :], rhs=uT.t[:, ct, t0:t0 + LC], start=True, stop=True), reads=[BT.b, uT.b], writes=[pb.b])
                            bre = pb.t[:, 0:LC]
                            bim = pb.t[:, LC:2 * LC]
                            cs = cosT.t[:, gp, 0:LC]
                            sn = sinT.t[:, gp, 0:LC]
                            c.op("dve", lambda e, w=w, bre=bre, cs=cs: e.tensor_tensor(out=t1[w].t[:], in0=bre, in1=cs, op=ALU.mult), reads=[pb.b, cosT.b], writes=[t1[w].b])
                            c.op("dve", lambda e, w=w, bim=bim, sn=sn: e.tensor_tensor(out=t2[w].t[:], in0=bim, in1=sn, op=ALU.mult), reads=[pb.b, sinT.b], writes=[t2[w].b])
                            c.op(eA, lambda e, w=w: e.tensor_tensor(out=cre[w].t[:], in0=t1[w].t[:], in1=t2[w].t[:], op=ALU.add), reads=[t1[w].b, t2[w].b], writes=[cre[w].b])
                            c.op("dve", lambda e, w=w, bim=bim, cs=cs: e.tensor_tensor(out=t1[w].t[:], in0=bim, in1=cs, op=ALU.mult), reads=[pb.b, cosT.b, cre[w].b], writes=[t1[w].b])
                            c.op("dve", lambda e, w=w, bre=bre, sn=sn: e.tensor_tensor(out=t2[w].t[:], in0=bre, in1=sn, op=ALU.mult), reads=[pb.b, sinT.b, cre[w].b], writes=[t2[w].b])
                            c.op(eA, lambda e, w=w: e.tensor_tensor(out=cim[w].t[:], in0=t1[w].t[:], in1=t2[w].t[:], op=ALU.subtract), reads=[t1[w].b, t2[w].b], writes=[cim[w].b])
                            c.op("dve", lambda e, w=w, gp=gp: e.tensor_tensor_scan(out=zre[w].t[:], data0=par.t[:, MAG, gp:gp + 1].broadcast_to([128, LC]), data1=cre[w].t[:], initial=zin[gp].t[:, 0:1], op0=ALU.mult, op1=ALU.add), reads=[par.b, cre[w].b, zin[gp].b], writes=[zre[w].b])
                            c.op("dve", lambda e, w=w, gp=gp: e.tensor_tensor_scan(out=zim[w].t[:], data0=par.t[:, MAG, gp:gp + 1].broadcast_to([128, LC]), data1=cim[w].t[:], initial=zin[gp].t[:, 1:2], op0=ALU.mult, op1=ALU.add), reads=[par.b, cim[w].b, zin[gp].b], writes=[zim[w].b])
                            cL = cosT.t[:, gp, LC:LC + 1]
                            sL = sinT.t[:, gp, LC:LC + 1]
                            c.op(eA, lambda e, w=w, sL=sL: e.tensor_scalar(out=ctmp[w].t[:, 0:1], in0=zim[w].t[:, LC - 1:LC], scalar1=sL, scalar2=None, op0=ALU.mult), reads=[zim[w].b, sinT.b], writes=[ctmp[w].b])
                            c.op(eA, lambda e, w=w, sL=sL: e.tensor_scalar(out=ctmp[w].t[:, 1:2], in0=zre[w].t[:, LC - 1:LC], scalar1=sL, scalar2=None, op0=ALU.mult), reads=[zre[w].b, sinT.b], writes=[ctmp[w].b])
                            c.op("dve", lambda e, w=w, cL=cL, gp=gp: e.scalar_tensor_tensor(out=zin[gp].t[:, 0:1], in0=zre[w].t[:, LC - 1:LC], scalar=cL, in1=ctmp[w].t[:, 0:1], op0=ALU.mult, op1=ALU.subtract), reads=[zre[w].b, cosT.b, ctmp[w].b], writes=[zin[gp].b])
                            c.op("dve", lambda e, w=w, cL=cL, gp=gp: e.scalar_tensor_tensor(out=zin[gp].t[:, 1:2], in0=zim[w].t[:, LC - 1:LC], scalar=cL, in1=ctmp[w].t[:, 1:2], op0=ALU.mult, op1=ALU.add), reads=[zim[w].b, cosT.b, ctmp[w].b], writes=[zin[gp].b])
                            if not need_y:
                                continue
                            s8 = (4 * ct + q) % 8
                            eB = "dve"
                            c.op(eB, lambda e, w=w, cs=cs: e.tensor_tensor(out=t1[w].t[:], in0=zre[w].t[:], in1=cs, op=ALU.mult), reads=[zre[w].b, cosT.b, cim[w].b], writes=[t1[w].b])
                            c.op(eB, lambda e, w=w, sn=sn: e.tensor_tensor(out=t2[w].t[:], in0=zim[w].t[:], in1=sn, op=ALU.mult), reads=[zim[w].b, sinT.b, cim[w].b], writes=[t2[w].b])
                            c.op(eB, lambda e, w=w, s8=s8: e.tensor_tensor(out=sre[s8].t[:], in0=t1[w].t[:], in1=t2[w].t[:], op=ALU.subtract), reads=[t1[w].b, t2[w].b], writes=[sre[s8].b])
                            c.op(eB, lambda e, w=w, sn=sn: e.tensor_tensor(out=t1[w].t[:], in0=zre[w].t[:], in1=sn, op=ALU.mult), reads=[zre[w].b, sinT.b, sre[s8].b], writes=[t1[w].b])
                            c.op(eB, lambda e, w=w, cs=cs: e.tensor_tensor(out=t2[w].t[:], in0=zim[w].t[:], in1=cs, op=ALU.mult), reads=[zim[w].b, cosT.b, sre[s8].b], writes=[t2[w].b])
                            c.op(eB, lambda e, w=w, s8=s8: e.tensor_tensor(out=sim[s8].t[:], in0=t1[w].t[:], in1=t2[w].t[:], op=ALU.add), reads=[t1[w].b, t2[w].b], writes=[sim[s8].b])
                            c.op("pe", lambda e, py=py, gp=gp, s8=s8, q=q: e.matmul(py.t[:, 0:LC], lhsT=CT.t[:, 2 * gp, :], rhs=sre[s8].t[:], start=(q == 0), stop=False), reads=[CT.b, sre[s8].b], writes=[py.b])
                            c.op("pe", lambda e, py=py, gp=gp, s8=s8, q=q: e.matmul(py.t[:, 0:LC], lhsT=CT.t[:, 2 * gp + 1, :], rhs=sim[s8].t[:], start=False, stop=(q == 3)), reads=[CT.b, sim[s8].b], writes=[py.b])
                        if need_y:
                            V_ = vv[(ch * 4 + ct) % len(vv)]
                            lo = max(t0, HIST)
                            c.op("dve", lambda e, V_=V_, py=py, ct=ct, t0=t0: e.scalar_tensor_tensor(out=V_.t[:], in0=uT.t[:, ct, t0:t0 + LC], scalar=dcol.t[:, ct:ct + 1], in1=py.t[:, 0:LC], op0=ALU.mult, op1=ALU.add), reads=[uT.b, dcol.b, py.b], writes=[V_.b])
                            c.dma("sp", lambda e, V_=V_, ct=ct, lo=lo, t0=t0: e.dma_start(out=k.vT[ct][:, lo - HIST:t0 + LC - HIST], in_=V_.t[:, lo - t0:LC]), reads=[V_.b])

            scale = 1.0 / float(np.sqrt(128.0))
            TA = [attn_alloc(k, st3, "a"), attn_alloc(k, st3, "b")]
            gens = [attn_stream(k, list(range(0, 6)), [k.ps[0], k.ps[1], k.ps[2]], TA[0], scale),
                    attn_stream(k, list(range(6, 12)), [k.ps[3], k.ps[4], k.ps[5]], TA[1], scale),
                    s5_gen()]
            alive = [True, True, True]
            step = 0
            while any(alive):
                for gi, g in enumerate(gens):
                    if not alive[gi]:
                        continue
                    if gi == 2 and step % 3 != 0 and (alive[0] or alive[1]):
                        continue
                    try:
                        next(g)
                    except StopIteration:
                        alive[gi] = False
                step += 1
            c.flush()
        with ExitStack() as st4:
            vts = k.tile(st4, "vts", [128, 4, NEXT], F32)
            gtb = k.tile(st4, "gtb", [128, 4, NEXT], BF16)
            sgs = [k.tile(st4, f"sgs{i}", [128, 512], F32) for i in range(2)]
            yos = [k.tile(st4, f"yos{i}", [128, NEXT], BF16) for i in range(2)]
            c.dma("sp", lambda e: e.dma_start(out=vts.t[:], in_=k.vT.rearrange("ct p t -> p ct t")), writes=[vts.b])
            for ct in range(4):
                c.op("act", lambda e, ct=ct: e.activation(out=gtb.t[:, ct, :], in_=vts.t[:, ct, :], func=AF.Gelu), reads=[vts.b], writes=[gtb.b])
            pj = 0
            for ot in range(4):
                Y_ = yos[ot % 2]
                for (t0, n) in tok_tiles(NEXT):
                    pg = k.ps[pj % 4]
                    S_ = sgs[pj % 2]
                    pj += 1
                    for kc in range(4):
                        c.op("pe", lambda e, pg=pg, kc=kc, ot=ot, t0=t0, n=n: e.matmul(pg.t[:, 0:n], lhsT=wglu.t[:, kc, ot * 128:(ot + 1) * 128], rhs=gtb.t[:, kc, t0:t0 + n], start=(kc == 0), stop=(kc == 3)), reads=[wglu.b, gtb.b], writes=[pg.b])
                    c.op("act", lambda e, S_=S_, pg=pg, ot=ot, n=n: e.activation(out=S_.t[:, 0:n], in_=pg.t[:, 0:n], func=AF.Sigmoid, bias=bglu.t[:, ot:ot + 1]), reads=[pg.b, bglu.b], writes=[S_.b])
                    c.op("dve", lambda e, S_=S_, Y_=Y_, ot=ot, t0=t0, n=n: e.tensor_tensor(out=Y_.t[:, t0:t0 + n], in0=S_.t[:, 0:n], in1=gtb.t[:, ot, t0:t0 + n], op=ALU.mult), reads=[S_.b, gtb.b], writes=[Y_.b])
                c.dma("sp", lambda e, Y_=Y_, ot=ot: e.dma_start(out=k.yT[ot], in_=Y_.t[:]), reads=[Y_.b])
            c.flush()


def attn_alloc(k, st, sfx):
    T = {}
    T["kT"] = k.tile(st, f"kTs{sfx}", [128, 4096], BF16)
    T["qT"] = k.tile(st, f"qTs{sfx}", [128, NEXT], BF16)
    T["V"] = k.tile(st, f"Vs{sfx}", [128, 32, 128], BF16)
    T["e"] = [k.tile(st, f"e_sb{sfx}{i}", [128, 512], F32) for i in range(3)]
    T["L"] = [k.tile(st, f"L_sb{sfx}{i}", [128, 512], BF16) for i in range(3)]
    T["g"] = [k.tile(st, f"g_sb{sfx}{i}", [128, 512], F32) for i in range(2)]
    T["w"] = [k.tile(st, f"w_sb{sfx}{i}", [128, 512], BF16) for i in range(3)]
    T["yb"] = k.tile(st, f"yb{sfx}", [128, 512], BF16)
    return T


def attn_qtile_gen(k, h, q0e, nq, banks, T, scale):
    c = k.c
    kT_, qT_, V_ = T["kT"], T["qT"], T["V"]
    e_sb, L_sb, g_sb, w_sb, Y_ = T["e"], T["L"], T["g"], T["w"], T["yb"]
    ps_s, ps_cs, ps_o = banks
    q0 = HIST + q0e
    kb_max = (q0 + nq) // 128 - 1
    kbs = list(range(kb_max, -1, -1))
    n = len(kbs)

    def diag_m(kb):
        return (kb * 128 - q0) // 128 if kb * 128 >= q0 else None

    def emit_S(i):
        kb = kbs[i]
        E_ = e_sb[i % len(e_sb)]
        c.op("pe", lambda e: e.matmul(ps_s.t[:, 0:nq], lhsT=kT_.t[:, kb * 128:(kb + 1) * 128], rhs=qT_.t[:, q0e:q0e + nq], start=True, stop=True), reads=[kT_.b, qT_.b], writes=[ps_s.b])
        c.op("act", lambda e: e.activation(out=E_.t[:, 0:nq], in_=ps_s.t[:, 0:nq], func=AF.Exp, scale=scale, bias=k.kb_sb.t[:, kb:kb + 1]), reads=[ps_s.b, k.kb_sb.b], writes=[E_.b])

    def emit_L(i):
        kb = kbs[i]
        E_, L_ = e_sb[i % len(e_sb)], L_sb[i % len(L_sb)]
        c.op("act", lambda e: e.activation(out=L_.t[:, 0:nq], in_=E_.t[:, 0:nq], func=AF.Ln, bias=1.0), reads=[E_.b], writes=[L_.b])
        m = diag_m(kb)
        if m is not None:
            c.op("pool", lambda e: e.tensor_tensor(out=L_.t[:, 0:nq], in0=L_.t[:, 0:nq], in1=k.cmask.t[:, m, 0:nq], op=ALU.mult), reads=[L_.b, k.cmask.b], writes=[L_.b])

    def emit_WV(i):
        kb = kbs[i]
        W_ = w_sb[i % len(w_sb)]
        c.op("pe", lambda e: e.matmul(ps_o.t[:, 0:nq], lhsT=V_.t[:, kb, :], rhs=W_.t[:, 0:nq], start=(i == 0), stop=(i == n - 1)), reads=[V_.b, W_.b], writes=[ps_o.b])

    def emit_strict(i):
        L_ = L_sb[i % len(L_sb)]
        c.op("pe", lambda e: e.matmul(ps_cs.t[:, 0:nq], lhsT=k.strl.t[:], rhs=L_.t[:, 0:nq], start=False, stop=True, skip_group_check=True), reads=[k.strl.b, L_.b], writes=[ps_cs.b])

    emit_S(0)
    emit_L(0)
    for i in range(n):
        kb = kbs[i]
        if i + 1 < n:
            emit_S(i + 1)
        if i > 0:
            emit_strict(i - 1)
        E_, L_, G_, W_ = e_sb[i % len(e_sb)], L_sb[i % len(L_sb)], g_sb[i % len(g_sb)], w_sb[i % len(w_sb)]
        c.op("pe", lambda e, L_=L_, i=i: e.matmul(ps_cs.t[:, 0:nq], lhsT=k.triu.t[:], rhs=L_.t[:, 0:nq], start=(i == 0), stop=True, skip_group_check=True), reads=[k.triu.b, L_.b], writes=[ps_cs.b])
        c.op("act", lambda e, G_=G_: e.activation(out=G_.t[:, 0:nq], in_=ps_cs.t[:, 0:nq], func=AF.Exp, scale=-1.0), reads=[ps_cs.b], writes=[G_.b])
        if i + 1 < n:
            emit_L(i + 1)
        if i > 0:
            emit_WV(i - 1)
        c.op("pool", lambda e, E_=E_, G_=G_, W_=W_: e.tensor_tensor(out=W_.t[:, 0:nq], in0=E_.t[:, 0:nq], in1=G_.t[:, 0:nq], op=ALU.mult), reads=[E_.b, G_.b], writes=[W_.b])
        m = diag_m(kb)
        if m is not None:
            c.op("pool", lambda e, W_=W_, m=m: e.tensor_tensor(out=W_.t[:, 0:nq], in0=W_.t[:, 0:nq], in1=k.cmask.t[:, m, 0:nq], op=ALU.mult), reads=[W_.b, k.cmask.b], writes=[W_.b])
        yield
    emit_WV(n - 1)
    k.evac(Y_.t[:, 0:nq], ps_o.t[:, 0:nq], reads=[ps_o.b], writes=[Y_.b], eng="dve")
    c.dma("sp", lambda e: e.dma_start(out=k.yT[4 + h][:, q0e:q0e + nq], in_=Y_.t[:, 0:nq]), reads=[Y_.b])


def attn_stream(k, heads, banks, T, scale):
    c = k.c
    QT = [(0, 128), (128, 512), (640, 512), (1152, 512), (1664, 512)]
    kT_, qT_, V_ = T["kT"], T["qT"], T["V"]
    for h in heads:
        c.dma("sp", lambda e, h=h: e.dma_start(out=kT_.t[:], in_=k.kT[h]), writes=[kT_.b])
        c.dma("sp", lambda e, h=h: e.dma_start(out=qT_.t[:], in_=k.qT[h]), writes=[qT_.b])
        c.dma("sp", lambda e, h=h: e.dma_start(out=V_.t[:], in_=k.vS[:, h * 128:(h + 1) * 128].rearrange("(kb p) d -> p kb d", p=128)), writes=[V_.b])
        for (q0e, nq) in QT:
            yield from attn_qtile_gen(k, h, q0e, nq, banks, T, scale)


def ln_tile(k, r, lnp, S, sbk, tt, res_out_dram, hT_stage, hT32, want_router, gates_sb, rw, rb, do_transpose=True):
    nc, c = k.nc, k.c
    gB, bB = lnp
    stats, mv, sm = S["stats"], S["mv"], S.get("sm")
    for j in range(4):
        c.op("dve", lambda e, j=j: e.bn_stats(out=stats.t[:, j, :], in_=r.t[:, j * 512:(j + 1) * 512]), reads=[r.b], writes=[stats.b])
    c.op("dve", lambda e: e.bn_aggr(out=mv.t[:], in_=stats.t[:].rearrange("p a b -> p (a b)")), reads=[stats.b], writes=[mv.b])
    c.op("act", lambda e: e.activation(out=mv.t[:, 1:2], in_=mv.t[:, 1:2], func=AF.Sqrt, bias=k.eps.t[:, 0:1]), reads=[mv.b, k.eps.b], writes=[mv.b])
    c.op("dve", lambda e: e.reciprocal(out=mv.t[:, 1:2], in_=mv.t[:, 1:2]), reads=[mv.b], writes=[mv.b])
    c.op("dve", lambda e: e.tensor_scalar(out=r.t[:], in0=r.t[:], scalar1=mv.t[:, 0:1], scalar2=mv.t[:, 1:2], op0=ALU.subtract, op1=ALU.mult), reads=[r.b, mv.b], writes=[r.b])
    c.op("pool", lambda e: e.tensor_tensor(out=r.t[:], in0=r.t[:], in1=gB.t[:], op=ALU.mult), reads=[r.b, gB.b], writes=[r.b])
    c.op("dve", lambda e: e.tensor_tensor(out=r.t[:], in0=r.t[:], in1=bB.t[:], op=ALU.add), reads=[r.b, bB.b], writes=[r.b])
    c.dma("sp", lambda e: e.dma_start(out=res_out_dram, in_=r.t[:]), reads=[r.b])
    if not do_transpose:
        return
    cb = sbk * 128
    for g in range(4):
        pb = k.ps[g]
        for j in range(4):
            kc = 4 * g + j
            c.op("pe", lambda e, pb=pb, j=j, kc=kc: e.transpose(pb.t[:, j * 128:(j + 1) * 128], r.t[:, kc * 128:(kc + 1) * 128], k.ident.t[:]), reads=[r.b, k.ident.b], writes=[pb.b])
        src = pb.t[:].rearrange("p (a b) -> p a b", a=4)
        c.op("dve", lambda e, g=g, src=src: e.tensor_copy(out=hT_stage.t[:, 4 * g:4 * g + 4, cb:cb + 128], in_=src), reads=[pb.b], writes=[hT_stage.b])
        if want_router:
            c.op("dve", lambda e, g=g, src=src: e.tensor_copy(out=hT32.t[:, 4 * g:4 * g + 4, :], in_=src), reads=[pb.b], writes=[hT32.b])
    if not want_router:
        return
    pl = k.ps[7]
    hhi, hlo = S["hhi"], S["hlo"]
    rwhi, rwlo = rw
    c.op("dve", lambda e: e.tensor_copy(out=hhi.t[:], in_=hT32.t[:]), reads=[hT32.b], writes=[hhi.b])
    c.op("dve", lambda e: e.tensor_tensor(out=hT32.t[:], in0=hT32.t[:], in1=hhi.t[:], op=ALU.subtract), reads=[hT32.b, hhi.b], writes=[hT32.b])
    c.op("dve", lambda e: e.tensor_copy(out=hlo.t[:], in_=hT32.t[:]), reads=[hT32.b], writes=[hlo.b])
    trip = [(hhi, rwhi), (hlo, rwhi), (hhi, rwlo)]
    for pi3, (ha, wa) in enumerate(trip):
        for kc in range(16):
            c.op("pe", lambda e, kc=kc, ha=ha, wa=wa, pi3=pi3: e.matmul(pl.t[:, 0:16], lhsT=ha.t[:, kc, :], rhs=wa.t[:, kc, :], start=(pi3 == 0 and kc == 0), stop=(pi3 == 2 and kc == 15)), reads=[ha.b, wa.b], writes=[pl.b])
    class _V:
        def __init__(self, t):
            self.t = t
        def __getitem__(self, key):
            a, sl = key
            return self.t[:, sl]
    lg = _V(sm.t)
    LG, PR, MXi, GS, T4, IG, PM, OH1, PM2, OH2, GT = [slice(i * 16, (i + 1) * 16) for i in range(11)]
    mx = lambda a, b: sm.t[:, 32 + a:32 + b]
    smb = [sm.b]
    o = lambda fn, extra=(): c.op("dve", fn, reads=smb + list(extra), writes=smb)
    o(lambda e: e.tensor_tensor(out=lg[:, LG], in0=pl.t[:, 0:16], in1=rb.t[:], op=ALU.add), extra=[pl.b, rb.b])
    o(lambda e: e.tensor_reduce(out=mx(0, 1), in_=lg[:, LG], axis=AX.X, op=ALU.max))
    o(lambda e: e.tensor_scalar(out=lg[:, PR], in0=lg[:, LG], scalar1=mx(0, 1), scalar2=None, op0=ALU.subtract))
    c.op("act", lambda e: e.activation(out=lg[:, PR], in_=lg[:, PR], func=AF.Exp), reads=smb, writes=smb)
    p4 = lg[:, PR].rearrange("p (g e) -> p g e", e=4)
    t4 = lg[:, T4].rearrange("p (g e) -> p g e", e=4)
    gs = lg[:, GS]
    pairs = [(0, 1), (0, 2), (0, 3), (1, 2), (1, 3), (2, 3)]
    for pi_, (a, b) in enumerate(pairs):
        if pi_ == 0:
            o(lambda e, a=a, b=b: e.tensor_tensor(out=gs[:, 0:4], in0=p4[:, :, a], in1=p4[:, :, b], op=ALU.add))
        else:
            o(lambda e, a=a, b=b: e.tensor_tensor(out=gs[:, 4:8], in0=p4[:, :, a], in1=p4[:, :, b], op=ALU.add))
            o(lambda e: e.tensor_tensor(out=gs[:, 0:4], in0=gs[:, 0:4], in1=gs[:, 4:8], op=ALU.max))
    o(lambda e: e.tensor_reduce(out=gs[:, 8:9], in_=gs[:, 0:4], axis=AX.X, op=ALU.max))
    o(lambda e: e.tensor_scalar(out=gs[:, 12:16], in0=gs[:, 0:4], scalar1=gs[:, 8:9], scalar2=None, op0=ALU.is_ge))
    ig = lg[:, IG].rearrange("p (g e) -> p g e", e=4)
    for e4 in range(4):
        o(lambda e, e4=e4: e.tensor_copy(out=ig[:, :, e4], in_=gs[:, 12:16]))
    o(lambda e: e.tensor_tensor(out=lg[:, PM], in0=lg[:, PR], in1=lg[:, IG], op=ALU.mult))
    o(lambda e: e.tensor_tensor(out=lg[:, PM], in0=lg[:, PM], in1=lg[:, IG], op=ALU.add))
    o(lambda e: e.tensor_scalar(out=lg[:, PM], in0=lg[:, PM], scalar1=-1.0, scalar2=None, op0=ALU.add))
    o(lambda e: e.tensor_reduce(out=mx(1, 2), in_=lg[:, PM], axis=AX.X, op=ALU.max))
    o(lambda e: e.tensor_scalar(out=lg[:, OH1], in0=lg[:, PM], scalar1=mx(1, 2), scalar2=None, op0=ALU.is_ge))
    o(lambda e: e.scalar_tensor_tensor(out=lg[:, PM2], in0=lg[:, OH1], scalar=-4.0, in1=lg[:, PM], op0=ALU.mult, op1=ALU.add))
    o(lambda e: e.tensor_reduce(out=mx(2, 3), in_=lg[:, PM2], axis=AX.X, op=ALU.max))
    o(lambda e: e.tensor_scalar(out=lg[:, OH2], in0=lg[:, PM2], scalar1=mx(2, 3), scalar2=None, op0=ALU.is_ge))
    o(lambda e: e.tensor_tensor(out=mx(3, 4), in0=mx(1, 2), in1=mx(2, 3), op=ALU.add))
    o(lambda e: e.reciprocal(out=mx(3, 4), in_=mx(3, 4)))
    o(lambda e: e.tensor_scalar(out=mx(4, 6), in0=mx(1, 3), scalar1=mx(3, 4), scalar2=None, op0=ALU.mult))
    o(lambda e: e.tensor_scalar(out=lg[:, GT], in0=lg[:, OH1], scalar1=mx(4, 5), scalar2=None, op0=ALU.mult))
    c.op("dve", lambda e: e.scalar_tensor_tensor(out=gates_sb.t[:, tt, :], in0=lg[:, OH2], scalar=mx(5, 6), in1=lg[:, GT], op0=ALU.mult, op1=ALU.add), reads=smb, writes=smb + [gates_sb.b])


def load_ln_params(k, st, idx_l, idx_j):
    c = k.c
    gB = k.tile(st, "gB", [128, 2048], F32)
    bB = k.tile(st, "bB", [128, 2048], F32)
    c.dma("sp", lambda e: e.dma_start(out=gB.t[:], in_=k.ln_g[idx_l, idx_j].partition_broadcast(128)), writes=[gB.b])
    c.dma("sp", lambda e: e.dma_start(out=bB.t[:], in_=k.ln_b[idx_l, idx_j].partition_broadcast(128)), writes=[bB.b])
    return gB, bB


def ln_scratch(k, st, router=True):
    if not router:
        return {"stats": k.tile(st, "stats", [128, 4, 6], F32), "mv": k.tile(st, "mv", [128, 2], F32)}
    return {"stats": k.tile(st, "stats", [128, 4, 6], F32), "mv": k.tile(st, "mv", [128, 2], F32), "sm": k.tile(st, "sm", [128, 11 * 16], F32),
            "hhi": k.tile(st, "hhi", [128, 16, 128], BF16), "hlo": k.tile(st, "hlo", [128, 16, 128], BF16)}


def load_router(k, st):
    c = k.c
    rw = k.tile(st, "rw", [128, 16, 16], F32)
    rb = k.tile(st, "rb", [128, 16], F32)
    c.dma("sp", lambda e: e.dma_start(out=rw.t[:], in_=k.router_w.rearrange("(kc p) n -> p kc n", p=128)), writes=[rw.b])
    c.dma("sp", lambda e: e.dma_start(out=rb.t[:], in_=k.router_b.partition_broadcast(128)), writes=[rb.b])
    rwhi = k.tile(st, "rwhi", [128, 16, 16], BF16)
    rwlo = k.tile(st, "rwlo", [128, 16, 16], BF16)
    c.op("dve", lambda e: e.tensor_copy(out=rwhi.t[:], in_=rw.t[:]), reads=[rw.b], writes=[rwhi.b])
    c.op("dve", lambda e: e.tensor_tensor(out=rw.t[:], in0=rw.t[:], in1=rwhi.t[:], op=ALU.subtract), reads=[rw.b, rwhi.b], writes=[rw.b])
    c.op("dve", lambda e: e.tensor_copy(out=rwlo.t[:], in_=rw.t[:]), reads=[rw.b], writes=[rwlo.b])
    return (rwhi, rwlo), rb


def phase_outproj_ln(k, layer, ntok, yT_dram, w_out, res_dram, res_row0, h1_dram, h1T_dram):
    nc, c = k.nc, k.c
    ntt = ntok // 128
    with ExitStack() as st:
        Wo = k.tile(st, "Wo", [128, 16, 2048], BF16)
        yTs = [k.tile(st, f"yTs{i}", [128, 16, 512], BF16) for i in range(2)]
        xr = [k.tile(st, f"xr{i}", [128, 2048], F32) for i in range(2)]
        rr = [k.tile(st, f"rr{i}", [128, 2048], F32) for i in range(2)]
        hst = [k.tile(st, f"hst{i}", [128, 16, 512], BF16) for i in range(2)]
        hT32 = k.tile(st, "hT32", [128, 16, 128], F32)
        lnp = load_ln_params(k, st, layer, 0)
        S = ln_scratch(k, st)
        rw, rb = load_router(k, st)
        for half in range(2):
            c.dma("pool", lambda e, half=half: e.dma_start(out=Wo.t[:, :, half * 1024:(half + 1) * 1024], in_=w_out.rearrange("(kc p) n -> p kc n", p=128)[:, :, half * 1024:(half + 1) * 1024]), reads=[Wo.b], writes=[Wo.b])
        groups = []
        t = 0
        if ntt % 4 == 1:
            groups.append((0, 1))
            t = 1
        while t < ntt:
            groups.append((t, 4))
            t += 4
        for gi, (tt0, ng) in enumerate(groups):
            Y_ = yTs[gi % 2]
            H_ = hst[gi % 2]
            c.dma("sp", lambda e, Y_=Y_, tt0=tt0, ng=ng: e.dma_start(out=Y_.t[:, :, 0:ng * 128], in_=yT_dram[:, :, tt0 * 128:(tt0 + ng) * 128].rearrange("kc p t -> p kc t")), writes=[Y_.b])
            for ti in range(ng):
                tt = tt0 + ti
                X_ = xr[tt % 2]
                R_ = rr[tt % 2]
                r0 = res_row0 + tt * 128
                c.dma("sp", lambda e, X_=X_, r0=r0: e.dma_start(out=X_.t[:], in_=res_dram[r0:r0 + 128, :]), writes=[X_.b])
                for j in range(4):
                    pb = k.ps[4 + j % 2]
                    for kc in range(16):
                        c.op("pe", lambda e, pb=pb, kc=kc, j=j, Y_=Y_, ti=ti: e.matmul(pb.t[:], lhsT=Y_.t[:, kc, ti * 128:(ti + 1) * 128], rhs=Wo.t[:, kc, j * 512:(j + 1) * 512], start=(kc == 0), stop=(kc == 15)), reads=[Y_.b, Wo.b], writes=[pb.b])
                    c.op("dve", lambda e, pb=pb, j=j, X_=X_, R_=R_: e.scalar_tensor_tensor(out=R_.t[:, j * 512:(j + 1) * 512], in0=X_.t[:, j * 512:(j + 1) * 512], scalar=ALPHA, in1=pb.t[:], op0=ALU.mult, op1=ALU.add), reads=[X_.b, pb.b], writes=[R_.b])
                ln_tile(k, R_, lnp, S, ti, tt, h1_dram[tt * 128:(tt + 1) * 128, :], H_, hT32, True, k.gates[layer], rw, rb)
            c.dma("sp", lambda e, H_=H_, tt0=tt0, ng=ng: e.dma_start(out=h1T_dram[:, :, tt0 * 128:(tt0 + ng) * 128].rearrange("kc p t -> p kc t"), in_=H_.t[:, :, 0:ng * 128]), reads=[H_.b])
        c.flush()


def phase_moe(k, layer, supers, hT_dram, h1_dram, out_dram, out_row_of_tile, outT_dram):
    nc, c = k.nc, k.c
    wg, wu, wd = k.moe_wg[layer], k.moe_wu[layer], k.moe_wd[layer]
    gates_sb = k.gates[layer]
    SMAX = max(len(s) for s in supers) * 128
    with ExitStack() as st:
        hT = k.tile(st, "mhT", [128, 16, SMAX], BF16)
        acc = k.tile(st, "macc", [128, SMAX // 128, 2048], F32)
        hid = k.tile(st, "mhid", [128, 8, SMAX], BF16)
        NR = 4 if SMAX <= 768 else 2
        Wg = [k.tile(st, f"mWg{i}", [128, 16, 128], BF16) for i in range(NR)]
        Wu = [k.tile(st, f"mWu{i}", [128, 16, 128], BF16) for i in range(NR)]
        Wd = [k.tile(st, f"mWd{i}", [128, 2, 2048], BF16) for i in range(4)]
        sgt = [k.tile(st, f"msg{i}", [128, 512], BF16) for i in range(2)]
        xr = [k.tile(st, f"mxr{i}", [128, 2048], F32) for i in range(2)]
        lnp = load_ln_params(k, st, layer, 1)
        S = ln_scratch(k, st, router=False)
        wq = 0
        pi = 0
        pending = None

        def ln_stage(tiles_):
            for lt in range(len(tiles_)):
                tt = tiles_[lt]
                X_ = xr[lt % len(xr)]
                c.dma("sp", lambda e, X_=X_, tt=tt: e.dma_start(out=X_.t[:], in_=h1_dram[tt * 128:(tt + 1) * 128, :]), writes=[X_.b])
                c.op("dve", lambda e, X_=X_, lt=lt: e.scalar_tensor_tensor(out=X_.t[:], in0=X_.t[:], scalar=ALPHA, in1=acc.t[:, lt, :], op0=ALU.mult, op1=ALU.add), reads=[X_.b, acc.b], writes=[X_.b])
                orow = out_row_of_tile(tt)
                ln_tile(k, X_, lnp, S, 0, tt, out_dram[orow:orow + 128, :], None, None, False, None, None, None, do_transpose=False)
                yield

        for tiles in supers:
            ns = len(tiles) * 128
            tt0 = tiles[0]
            c.dma("sp", lambda e, tt0=tt0, ns=ns: e.dma_start(out=hT.t[:, :, 0:ns], in_=hT_dram[:, :, tt0 * 128:tt0 * 128 + ns].rearrange("kc p t -> p kc t")), writes=[hT.b])
            ttl = tok_tiles(ns)
            for ex in range(16):
                for fc in range(8):
                    G_, U_ = Wg[wq % NR], Wu[wq % NR]
                    wq += 1
                    c.dma("pool", lambda e, G_=G_, ex=ex, fc=fc: e.dma_start(out=G_.t[:], in_=wg[ex].rearrange("(kc p) f -> p kc f", p=128)[:, :, fc * 128:(fc + 1) * 128]), writes=[G_.b])
                    c.dma("pool", lambda e, U_=U_, ex=ex, fc=fc: e.dma_start(out=U_.t[:], in_=wu[ex].rearrange("(kc p) f -> p kc f", p=128)[:, :, fc * 128:(fc + 1) * 128]), writes=[U_.b])
                    for (t0, n) in ttl:
                        pg = k.ps[(pi % 2) * 2]
                        pu = k.ps[(pi % 2) * 2 + 1]
                        sg_ = sgt[pi % 2]
                        pi += 1
                        for kc in range(16):
                            c.op("pe", lambda e, pg=pg, G_=G_, kc=kc, t0=t0, n=n: e.matmul(pg.t[:, 0:n], lhsT=G_.t[:, kc, :], rhs=hT.t[:, kc, t0:t0 + n], start=(kc == 0), stop=(kc == 15)), reads=[G_.b, hT.b], writes=[pg.b])
                        for kc in range(16):
                            c.op("pe", lambda e, pu=pu, U_=U_, kc=kc, t0=t0, n=n: e.matmul(pu.t[:, 0:n], lhsT=U_.t[:, kc, :], rhs=hT.t[:, kc, t0:t0 + n], start=(kc == 0), stop=(kc == 15)), reads=[U_.b, hT.b], writes=[pu.b])
                        c.op("act", lambda e, pg=pg, sg_=sg_, n=n: e.activation(out=sg_.t[:, 0:n], in_=pg.t[:, 0:n], func=AF.Silu), reads=[pg.b], writes=[sg_.b])
                        c.op("dve", lambda e, pu=pu, sg_=sg_, fc=fc, t0=t0, n=n: e.tensor_tensor(out=hid.t[:, fc, t0:t0 + n], in0=sg_.t[:, 0:n], in1=pu.t[:, 0:n], op=ALU.mult), reads=[sg_.b, pu.b], writes=[hid.b])
                        if ex == 0 and pending is not None:
                            next(pending, None)
                if ex == 0 and pending is not None:
                    for _ in pending:
                        pass
                    pending = None
                for pr in range(4):
                    D_ = Wd[pr]
                    c.dma("pool", lambda e, D_=D_, ex=ex, pr=pr: e.dma_start(out=D_.t[:], in_=wd[ex][pr * 256:(pr + 1) * 256, :].rearrange("(fc p) n -> p fc n", p=128)), writes=[D_.b])
                for lt in range(len(tiles)):
                    tt = tiles[lt]
                    for j in range(4):
                        po = k.ps[4 + (lt * 4 + j) % 4]
                        for fc in range(8):
                            D_ = Wd[fc // 2]
                            c.op("pe", lambda e, po=po, fc=fc, D_=D_, lt=lt, j=j: e.matmul(po.t[:], lhsT=hid.t[:, fc, lt * 128:(lt + 1) * 128], rhs=D_.t[:, fc % 2, j * 512:(j + 1) * 512], start=(fc == 0), stop=(fc == 7)), reads=[hid.b, D_.b], writes=[po.b])
                        if ex == 0:
                            c.op("dve", lambda e, po=po, lt=lt, j=j, tt=tt, ex=ex: e.tensor_scalar(out=acc.t[:, lt, j * 512:(j + 1) * 512], in0=po.t[:], scalar1=gates_sb.t[:, tt, ex:ex + 1], scalar2=None, op0=ALU.mult), reads=[po.b, gates_sb.b, acc.b], writes=[acc.b])
                        else:
                            c.op("dve", lambda e, po=po, lt=lt, j=j, tt=tt, ex=ex: e.scalar_tensor_tensor(out=acc.t[:, lt, j * 512:(j + 1) * 512], in0=po.t[:], scalar=gates_sb.t[:, tt, ex:ex + 1], in1=acc.t[:, lt, j * 512:(j + 1) * 512], op0=ALU.mult, op1=ALU.add), reads=[po.b, gates_sb.b, acc.b], writes=[acc.b])
            pending = ln_stage(tiles)
        for _ in pending:
            pass
        c.flush()


def phase_pool(k):
    nc, c = k.nc, k.c
    with ExitStack() as st:
        hT = k.tile(st, "phT", [128, 16, NEXT], BF16)
        Wi = [k.tile(st, f"pWi{i}", [128, 16, 512], BF16) for i in range(2)]
        Wgp = [k.tile(st, f"pWg{i}", [128, 4, 512], BF16) for i in range(2)]
        hp = [k.tile(st, f"php{i}", [128, NEXT], F32) for i in range(2)]
        sa = [k.tile(st, f"psa{i}", [128, NEXT], F32) for i in range(1)]
        sb_ = [k.tile(st, f"psb{i}", [128, NEXT], F32) for i in range(1)]
        pl = k.tile(st, "ppl", [128, 4, 2048], BF16)
        ysg = [k.tile(st, f"pys{i}", [128, 2048], BF16) for i in range(2)]
        hm = k.tile(st, "phm", [128, 1], F32)
        ic = k.tile(st, "pic", [128, 64], F32)
        sc = k.tile(st, "psc", [128, 16], F32)
        xs = [k.tile(st, f"pxs{i}", [128, 2048], F32) for i in range(2)]
        for tt in range(NEXT // 128):
            x_ = xs[tt % 2]
            c.dma("sp", lambda e, x_=x_, tt=tt: e.dma_start(out=x_.t[:], in_=k.h2[tt * 128:(tt + 1) * 128, :]), writes=[x_.b])
            for g4 in range(4):
                pb = k.ps[g4]
                for j in range(4):
                    kc = 4 * g4 + j
                    c.op("pe", lambda e, pb=pb, j=j, kc=kc, x_=x_: e.transpose(pb.t[:, j * 128:(j + 1) * 128], x_.t[:, kc * 128:(kc + 1) * 128], k.ident.t[:]), reads=[x_.b, k.ident.b], writes=[pb.b])
                c.op("dve", lambda e, pb=pb, g4=g4, tt=tt: e.tensor_copy(out=hT.t[:, 4 * g4:4 * g4 + 4, tt * 128:(tt + 1) * 128], in_=pb.t[:].rearrange("p (a b) -> p a b", a=4)), reads=[pb.b], writes=[hT.b])
        c.dma("sp", lambda e: e.dma_start(out=hm.t[:], in_=k.halo_mask), writes=[hm.b])
        c.dma("sp", lambda e: e.dma_start(out=ic.t[:], in_=k.invcnt), writes=[ic.b])
        c.dma("sp", lambda e: e.dma_start(out=sc.t[:], in_=k.c_scale.rearrange("(cc p) -> p cc", p=128), allow_slow_non_contiguous=True), writes=[sc.b])
        pi = 0
        for g in range(4):
            w = (2, 4, 8, 16)[g]
            W_ = Wi[g % 2]
            Wg_ = Wgp[g % 2]
            c.dma("pool", lambda e, W_=W_, g=g: e.dma_start(out=W_.t[:], in_=k.c_w_in.rearrange("(kc p) n -> p kc n", p=128)[:, :, g * 512:(g + 1) * 512]), writes=[W_.b])
            c.dma("pool", lambda e, Wg_=Wg_, g=g: e.dma_start(out=Wg_.t[:], in_=k.c_w_group[g].rearrange("(kc p) n -> p kc n", p=128)), writes=[Wg_.b])
            for sub in range(4):
                H_ = hp[sub % 2]
                for (t0, n) in tok_tiles(NEXT):
                    pb = k.ps[pi % 4]
                    pi += 1
                    for kc in range(16):
                        c.op("pe", lambda e, pb=pb, kc=kc, W_=W_, sub=sub, t0=t0, n=n: e.matmul(pb.t[:, 0:n], lhsT=W_.t[:, kc, sub * 128:(sub + 1) * 128], rhs=hT.t[:, kc, t0:t0 + n], start=(kc == 0), stop=(kc == 15)), reads=[W_.b, hT.b], writes=[pb.b])
                    k.evac(H_.t[:, t0:t0 + n], pb.t[:, 0:n], reads=[pb.b], writes=[H_.b])
                c.op("dve", lambda e, H_=H_: e.tensor_scalar(out=H_.t[:, 0:128], in0=H_.t[:, 0:128], scalar1=hm.t[:, 0:1], scalar2=None, op0=ALU.mult), reads=[H_.b, hm.b], writes=[H_.b])
                cur = H_
                step = 1
                pp = [sa[0], sb_[0]]
                ii = 0
                while step < w:
                    nxt = pp[ii % 2]
                    ii += 1
                    eng = "pool" if ii % 2 == 0 else "dve"
                    c.op(eng, lambda e, cur=cur, nxt=nxt, step=step: e.tensor_copy(out=nxt.t[:, 0:step], in_=cur.t[:, 0:step]), reads=[cur.b], writes=[nxt.b])
                    c.op(eng, lambda e, cur=cur, nxt=nxt, step=step: e.tensor_tensor(out=nxt.t[:, step:NEXT], in0=cur.t[:, step:NEXT], in1=cur.t[:, 0:NEXT - step], op=ALU.add), reads=[cur.b, nxt.b], writes=[nxt.b])
                    cur = nxt
                    step *= 2
                c.op("dve", lambda e, cur=cur, H_=H_, sub=sub, w=w: e.scalar_tensor_tensor(out=pl.t[:, sub, :], in0=cur.t[:, 128:NEXT], scalar=1.0 / w, in1=H_.t[:, 128:NEXT], op0=ALU.mult, op1=ALU.subtract), reads=[cur.b, H_.b], writes=[pl.b])
                c.op("dve", lambda e, cur=cur, g=g: e.tensor_tensor(out=cur.t[:, 128:144], in0=cur.t[:, 128:144], in1=ic.t[:, g * 16:(g + 1) * 16], op=ALU.mult), reads=[cur.b, ic.b, pl.b], writes=[cur.b])
                c.op("dve", lambda e, cur=cur, H_=H_, sub=sub: e.tensor_tensor(out=pl.t[:, sub, 0:16], in0=cur.t[:, 128:144], in1=H_.t[:, 128:144], op=ALU.subtract), reads=[cur.b, H_.b], writes=[pl.b])
            for oc in range(4):
                Y_ = ysg[oc % 2]
                cc = g * 4 + oc
                for (t0, n) in tok_tiles(2048):
                    pb = k.ps[4 + pi % 4]
                    pi += 1
                    for kc in range(4):
                        c.op("pe", lambda e, pb=pb, kc=kc, Wg_=Wg_, oc=oc, t0=t0, n=n: e.matmul(pb.t[:, 0:n], lhsT=Wg_.t[:, kc, oc * 128:(oc + 1) * 128], rhs=pl.t[:, kc, t0:t0 + n], start=(kc == 0), stop=(kc == 3)), reads=[Wg_.b, pl.b], writes=[pb.b])
                    c.op("dve", lambda e, pb=pb, Y_=Y_, cc=cc, t0=t0, n=n: e.tensor_scalar(out=Y_.t[:, t0:t0 + n], in0=pb.t[:, 0:n], scalar1=sc.t[:, cc:cc + 1], scalar2=None, op0=ALU.mult), reads=[pb.b, sc.b], writes=[Y_.b])
                c.dma("sp", lambda e, Y_=Y_, cc=cc: e.dma_start(out=k.y1T[cc], in_=Y_.t[:]), reads=[Y_.b])
        c.flush()


def build(debug=False, stop_after=None, small_moe=False):
    nc = bass.Bass("TRN2", target_bir_lowering=False)
    k = K(nc, debug)

    def din(name, shape):
        return nc.dram_tensor(name, shape, F32, kind="ExternalInput").ap()

    def scr(name, shape, dt):
        kind = "ExternalOutput" if (debug and name in DEBUG_OUTS) else "Internal"
        return nc.dram_tensor(name, shape, dt, kind=kind).ap()

    k.x_loc = din("x_loc", [4096, 2048])
    k.kbias = din("kbias", [32, 128])
    k.halo_mask = din("halo_mask", [128, 1])
    k.invcnt = din("invcnt", [128, 64])
    k.w_in = din("ab_w_in", [2048, 5120])
    k.lam_re = din("ab_lambda_re", [32, 64])
    k.lam_im = din("ab_lambda_im", [32, 64])
    k.log_dt = din("ab_log_dt", [32])
    k.b_re = din("ab_b_re", [32, 64, 16])
    k.b_im = din("ab_b_im", [32, 64, 16])
    k.c_re = din("ab_c_re", [32, 16, 64])
    k.c_im = din("ab_c_im", [32, 16, 64])
    k.ab_d = din("ab_d", [512])
    k.w_glu = din("ab_w_glu", [512, 512])
    k.b_glu = din("ab_b_glu", [512])
    k.ab_w_out = din("ab_w_out", [2048, 2048])
    k.c_w_in = din("c_w_in", [2048, 2048])
    k.c_w_group = din("c_w_group", [4, 512, 512])
    k.c_scale = din("c_scale", [2048])
    k.c_w_out = din("c_w_out", [2048, 2048])
    k.ln_g = din("ln_g", [2, 2, 2048])
    k.ln_b = din("ln_b", [2, 2, 2048])
    k.router_w = din("router_w", [2048, 16])
    k.router_b = din("router_b", [16])
    if not small_moe:
        k.moe_wg = din("moe_w_gate", [2, 16, 2048, 1024])
        k.moe_wu = din("moe_w_up", [2, 16, 2048, 1024])
        k.moe_wd = din("moe_w_down", [2, 16, 1024, 2048])
    out = nc.dram_tensor("out", [2048, 2048], F32, kind="ExternalOutput").ap()

    k.uT_all = scr("uT", [4, 128, 4096], BF16)
    k.uT = [k.uT_all[i] for i in range(4)]
    qT_all = scr("qT", [12, 128, NEXT], BF16)
    k.qT = [qT_all[i] for i in range(12)]
    kT_all = scr("kT", [12, 128, 4096], BF16)
    k.kT = [kT_all[i] for i in range(12)]
    k.vS = scr("vS", [4096, 1536], BF16)
    k.yT_all = scr("yT", [16, 128, NEXT], BF16)
    k.yT = [k.yT_all[i] for i in range(16)]
    k.vT = scr("vT", [4, 128, NEXT], F32)
    k.h1 = scr("h1", [NEXT, 2048], F32)
    k.h1T = scr("h1T", [16, 128, NEXT], BF16)
    k.h2 = scr("h2", [NEXT, 2048], F32)
    k.h2T = scr("h2T", [16, 128, NEXT], BF16)
    k.y1T_all = scr("y1T", [16, 128, 2048], BF16)
    k.y1T = [k.y1T_all[i] for i in range(16)]
    k.h3 = scr("h3", [2048, 2048], F32)
    k.h3T = scr("h3T", [16, 128, 2048], BF16)
    k.dummy_out = scr("dummy_o", [128, 2048], F32)

    with nc.allow_low_precision("bf16 matmul operands, fp32 accumulation"), nc.allow_non_contiguous_dma(reason="small parameter gathers"):
        setup_consts(k)
        phases = [
            ("inproj0", lambda: (setattr(k, "act_copy_ok", True), phase_inproj0(k), setattr(k, "act_copy_ok", False))),
            ("s5", lambda: phase_s5(k)),
            ("ln0", lambda: phase_outproj_ln(k, 0, NEXT, k.yT_all, k.ab_w_out, k.x_loc, HIST, k.h1, k.h1T)),
            ("moe0", lambda: phase_moe(k, 0, [[0, 1, 2, 3, 4], list(range(5, 11)), list(range(11, 17))], k.h1T, k.h1, k.h2, lambda tt: tt * 128, k.h2T)),
            ("pool", lambda: phase_pool(k)),
            ("ln1", lambda: phase_outproj_ln(k, 1, 2048, k.y1T_all, k.c_w_out, k.h2, 128, k.h3, k.h3T)),
            ("moe1", lambda: phase_moe(k, 1, [list(range(0, 8)), list(range(8, 16))], k.h3T, k.h3, out, lambda tt: tt * 128, None)),
        ]
        for name, fn in phases:
            if stop_after == 'consts':
                break
            fn()
            if stop_after == name:
                break
        k.c.flush()
    return nc, k


DEBUG_OUTS = set()


def make_in_maps(inputs):
    x = np.ascontiguousarray(inputs["x"], dtype=np.float32)
    sq = lambda a: np.ascontiguousarray(np.asarray(a, dtype=np.float32)[0])
    shared = {
        "ab_w_in": sq(inputs["ab_w_in"]), "ab_lambda_re": sq(inputs["ab_lambda_re"]), "ab_lambda_im": sq(inputs["ab_lambda_im"]),
        "ab_log_dt": sq(inputs["ab_log_dt"]), "ab_b_re": sq(inputs["ab_b_re"]), "ab_b_im": sq(inputs["ab_b_im"]),
        "ab_c_re": sq(inputs["ab_c_re"]), "ab_c_im": sq(inputs["ab_c_im"]), "ab_d": sq(inputs["ab_d"]),
        "ab_w_glu": sq(inputs["ab_w_glu"]), "ab_b_glu": sq(inputs["ab_b_glu"]), "ab_w_out": sq(inputs["ab_w_out"]),
        "c_w_in": sq(inputs["c_w_in"]), "c_w_group": sq(inputs["c_w_group"]), "c_scale": sq(inputs["c_scale"]),
        "c_w_out": sq(inputs["c_w_out"]),
        "ln_g": np.ascontiguousarray(inputs["ln_g"], dtype=np.float32), "ln_b": np.ascontiguousarray(inputs["ln_b"], dtype=np.float32),
        "router_w": np.ascontiguousarray(inputs["router_w"], dtype=np.float32), "router_b": np.ascontiguousarray(inputs["router_b"], dtype=np.float32),
        "moe_w_gate": np.ascontiguousarray(inputs["moe_w_gate"], dtype=np.float32),
        "moe_w_up": np.ascontiguousarray(inputs["moe_w_up"], dtype=np.float32),
        "moe_w_down": np.ascontiguousarray(inputs["moe_w_down"], dtype=np.float32),
    }
    windows = (2, 4, 8, 16)
    in_maps = []
    for core in range(8):
        b, p = core // 2, core % 2
        m = dict(shared)
        if p == 1:
            m["x_loc"] = x[b]
            kb = np.zeros((32, 128), np.float32)
            hm = np.ones((128, 1), np.float32)
            ic = np.stack([np.full(16, 1.0 / w, np.float32) for w in windows])
        else:
            xl = np.zeros((4096, 2048), np.float32)
            xl[2048:] = x[b, :2048]
            m["x_loc"] = xl
            kb = np.zeros((32, 128), np.float32)
            kb[:16] = -30000.0
            hm = np.zeros((128, 1), np.float32)
            ic = np.stack([1.0 / np.minimum(np.arange(16) + 1, w).astype(np.float32) for w in windows])
        m["kbias"] = kb
        m["halo_mask"] = hm
        m["invcnt"] = np.ascontiguousarray(np.broadcast_to(ic.reshape(1, 64), (128, 64)), dtype=np.float32)
        in_maps.append(m)
    return in_maps


def kernel(**inputs):
    nc, _ = build()
    in_maps = make_in_maps(inputs)
    res = run_bass_kernel_spmd(nc, in_maps, core_ids=list(range(8)))
    outp = np.empty((4, 4096, 2048), np.float32)
    for core in range(8):
        b, p = core // 2, core % 2
        outp[b, p * 2048:(p + 1) * 2048] = res.results[core]["out"]
    return outp
```
